# Optimizing a Trainium2 kernel written in Bass

```python
import math
import jax
import jax.numpy as jnp
from jax import lax
import numpy as np

D_MODEL = 2048
BATCH = 4
SEQ = 4096
DEPTH = 4

CHUNK = 64
LN_EPS = 1e-5
RET_HEADS = 8
RET_DK = D_MODEL // 16
RET_DV = D_MODEL // 8
RET_QK = RET_HEADS * RET_DK
RET_V = RET_HEADS * RET_DV
ROPE_BASE = 10000.0
SSD_DI = D_MODEL
SSD_P = 64
SSD_HEADS = SSD_DI // SSD_P
SSD_G = 4
SSD_N = 128
SSD_CONV = 4
SSD_XBC = SSD_DI + 2 * SSD_G * SSD_N
HG_DK = 128
HG_HEADS = D_MODEL // HG_DK
HG_DV = D_MODEL // HG_HEADS
HG_W = HG_HEADS * HG_DK
HG_V = HG_HEADS * HG_DV
FFN_DIM = ((8 * D_MODEL // 3 + 255) // 256) * 256
N_EXPERTS = 8
TOP_K = 2
MOE_BLOCK = 256

EV_SIZES = (RET_QK, RET_QK, RET_V, RET_V, SSD_DI, SSD_XBC, SSD_HEADS)
EV_IN = 2 * RET_QK + 2 * RET_V + SSD_DI + SSD_XBC + SSD_HEADS
OD_SIZES = (HG_W, HG_W, HG_V, HG_V)
OD_IN = 2 * HG_W + 2 * HG_V

kernel_name = "hybrid_retention_ssd_hgrn2_moe_encoder"

F32 = jnp.float32


def _split(t, sizes):
    return jnp.split(t, [int(v) for v in np.cumsum(sizes)[:-1]], axis=-1)


def layer_norm(x, g, b):
    x32 = x.astype(F32)
    mu = jnp.mean(x32, axis=-1, keepdims=True)
    xc = x32 - mu
    var = jnp.mean(xc * xc, axis=-1, keepdims=True)
    return (xc * lax.rsqrt(var + LN_EPS) * g + b).astype(x.dtype)


def group_layer_norm(t, w, groups):
    shp = t.shape
    t32 = t.astype(F32).reshape(*shp[:-1], groups, shp[-1] // groups)
    tc = t32 - jnp.mean(t32, axis=-1, keepdims=True)
    t32 = tc * lax.rsqrt(jnp.mean(tc * tc, axis=-1, keepdims=True) + LN_EPS)
    return (t32.reshape(shp) * w).astype(t.dtype)


def group_rms_norm(t, w, groups):
    shp = t.shape
    t32 = t.astype(F32).reshape(*shp[:-1], groups, shp[-1] // groups)
    t32 = t32 * lax.rsqrt(jnp.mean(t32 * t32, axis=-1, keepdims=True) + LN_EPS)
    return (t32.reshape(shp) * w).astype(t.dtype)


def rotary(t):
    s, dh = t.shape[1], t.shape[-1]
    inv_freq = ROPE_BASE ** (-jnp.arange(0, dh, 2, dtype=F32) / dh)
    ang = jnp.arange(s, dtype=F32)[:, None] * inv_freq[None, :]
    cos = jnp.cos(ang)[None, :, None, :]
    sin = jnp.sin(ang)[None, :, None, :]
    t1, t2 = jnp.split(t.astype(F32), 2, axis=-1)
    return jnp.concatenate([t1 * cos - t2 * sin, t1 * sin + t2 * cos], axis=-1).astype(t.dtype)


def _to_chunks(t):
    b, s = t.shape[:2]
    return jnp.moveaxis(t.reshape(b, s // CHUNK, CHUNK, *t.shape[2:]), 1, 0)


def _from_chunks(t):
    nc, b = t.shape[:2]
    return jnp.moveaxis(t, 0, 1).reshape(b, nc * CHUNK, *t.shape[3:])


def scalar_decay_scan(q, k, v, log_a):
    b, _, h, n = q.shape
    p = v.shape[-1]
    causal = jnp.tril(jnp.ones((CHUNK, CHUNK), dtype=bool))

    def step(state, inp):
        qc, kc, vc, ac = inp
        cum = jnp.cumsum(ac.astype(F32), axis=1)
        cum_h = jnp.swapaxes(cum, 1, 2)
        diff = cum_h[:, :, :, None] - cum_h[:, :, None, :]
        decay = jnp.exp(jnp.where(causal, diff, -jnp.inf))
        scores = jnp.einsum("bthn,bshn->bhts", qc, kc) * decay
        y = jnp.einsum("bhts,bshp->bthp", scores, vc)
        y = y + jnp.einsum("bthn,bhnp->bthp", qc * jnp.exp(cum)[..., None], state)
        last = cum[:, -1]
        k_dec = kc * jnp.exp(last[:, None, :] - cum)[..., None]
        state = jnp.exp(last)[:, :, None, None] * state + jnp.einsum("bshn,bshp->bhnp", k_dec, vc)
        return state, y

    state0 = jnp.zeros((b, h, n, p), F32)
    _, ys = lax.scan(step, state0, (_to_chunks(q), _to_chunks(k), _to_chunks(v), _to_chunks(log_a)))
    return _from_chunks(ys).astype(v.dtype)


def gated_decay_scan(q, k, v, log_f):
    b, _, h, dk = q.shape
    dv = v.shape[-1]
    causal = jnp.tril(jnp.ones((CHUNK, CHUNK), dtype=bool))[None, :, :, None, None]

    def step(state, inp):
        qc, kc, vc, fc = inp
        cum = jnp.cumsum(fc.astype(F32), axis=1)
        diff = cum[:, :, None] - cum[:, None, :]
        decay = jnp.exp(jnp.where(causal, diff, -jnp.inf))
        scores = jnp.einsum("bthk,bshk,btshk->bhts", qc, kc, decay)
        y = jnp.einsum("bhts,bshv->bthv", scores, vc)
        y = y + jnp.einsum("bthk,bhkv->bthv", qc * jnp.exp(cum), state)
        last = cum[:, -1]
        k_dec = kc * jnp.exp(last[:, None] - cum)
        state = jnp.exp(last)[..., None] * state + jnp.einsum("bshk,bshv->bhkv", k_dec, vc)
        return state, y

    state0 = jnp.zeros((b, h, dk, dv), F32)
    _, ys = lax.scan(step, state0, (_to_chunks(q), _to_chunks(k), _to_chunks(v), _to_chunks(log_f)))
    return _from_chunks(ys).astype(v.dtype)


def causal_depthwise_conv(x, w, bias):
    c = x.shape[-1]
    y = lax.conv_general_dilated(
        x, w[:, None, :], window_strides=(1,), padding=[(SSD_CONV - 1, 0)],
        dimension_numbers=("NWC", "WIO", "NWC"), feature_group_count=c)
    return y + bias


def retention_ssd_mixer(x, w_in, ret_norm_w, conv_w, conv_b, dt_bias, a_log, d_skip, ssd_norm_w, w_out):
    b, s, _ = x.shape
    q, k, v, g, z, xbc, dt_raw = _split(x @ w_in, EV_SIZES)
    q = rotary(q.reshape(b, s, RET_HEADS, RET_DK))
    k = rotary(k.reshape(b, s, RET_HEADS, RET_DK)) * (RET_DK ** -0.5)
    v = v.reshape(b, s, RET_HEADS, RET_DV)
    log_gamma = jnp.log1p(-jnp.exp2(-5.0 - jnp.arange(RET_HEADS, dtype=F32)))
    ret = scalar_decay_scan(q, k, v, jnp.broadcast_to(log_gamma, (b, s, RET_HEADS)))
    ret = group_layer_norm(ret.reshape(b, s, RET_V), ret_norm_w, RET_HEADS) * jax.nn.silu(g)
    xbc = jax.nn.silu(causal_depthwise_conv(xbc, conv_w, conv_b))
    xs, bm, cm = _split(xbc, (SSD_DI, SSD_G * SSD_N, SSD_G * SSD_N))
    xs = xs.reshape(b, s, SSD_HEADS, SSD_P)
    heads_per_group = SSD_HEADS // SSD_G
    bm = jnp.repeat(bm.reshape(b, s, SSD_G, SSD_N), heads_per_group, axis=2)
    cm = jnp.repeat(cm.reshape(b, s, SSD_G, SSD_N), heads_per_group, axis=2)
    dt = jax.nn.softplus(dt_raw.astype(F32) + dt_bias.astype(F32))
    log_a = -dt * jnp.exp(a_log.astype(F32))
    y = scalar_decay_scan(cm, bm, xs * dt[..., None].astype(xs.dtype), log_a)
    y = y + d_skip[:, None] * xs
    y = group_rms_norm(y.reshape(b, s, SSD_DI) * jax.nn.silu(z), ssd_norm_w, SSD_G)
    return jnp.concatenate([ret, y], axis=-1) @ w_out


def hgrn2_mixer(x, w_in, lower_bound, norm_w, w_out):
    b, s, _ = x.shape
    q, f_raw, i, g = _split(x @ w_in, OD_SIZES)
    lb = lower_bound.reshape(HG_HEADS, HG_DK)
    f_raw = f_raw.reshape(b, s, HG_HEADS, HG_DK).astype(F32)
    log_f = jnp.log(lb + (1.0 - lb) * jax.nn.sigmoid(f_raw))
    k = ((1.0 - lb) * jax.nn.sigmoid(-f_raw)).astype(x.dtype)
    q = jax.nn.silu(q.reshape(b, s, HG_HEADS, HG_DK))
    i = i.reshape(b, s, HG_HEADS, HG_DV)
    o = gated_decay_scan(q, k, i, log_f)
    o = group_rms_norm(o.reshape(b, s, HG_V), norm_w, HG_HEADS) * jax.nn.silu(g)
    return o @ w_out


def swiglu(x, w1, w3, w2):
    return (jax.nn.silu(x @ w1) * (x @ w3)) @ w2


def moe_swiglu(x, w_router, w1, w3, w2):
    b, s, d = x.shape
    xt = x.reshape(-1, d)
    n = xt.shape[0]
    logits = (xt @ w_router).astype(F32)
    top_logits, top_idx = lax.top_k(logits, TOP_K)
    gates = jax.nn.softmax(top_logits, axis=-1)
    n_assign = n * TOP_K
    expert_flat = top_idx.reshape(-1)
    token_flat = jnp.arange(n_assign, dtype=jnp.int32) // TOP_K
    gate_flat = gates.reshape(-1)
    order = jnp.argsort(expert_flat)
    sorted_expert = expert_flat[order]
    counts = jnp.zeros((N_EXPERTS,), jnp.int32).at[expert_flat].add(1)
    padded = (counts + MOE_BLOCK - 1) // MOE_BLOCK * MOE_BLOCK
    start_sorted = jnp.cumsum(counts) - counts
    padded_end = jnp.cumsum(padded)
    start_padded = padded_end - padded
    rank = jnp.arange(n_assign, dtype=jnp.int32) - start_sorted[sorted_expert]
    dest = start_padded[sorted_expert] + rank
    n_rows = n_assign + N_EXPERTS * MOE_BLOCK
    n_blocks = n_rows // MOE_BLOCK
    row_token = jnp.zeros((n_rows,), jnp.int32).at[dest].set(token_flat[order])
    row_gate = jnp.zeros((n_rows,), F32).at[dest].set(gate_flat[order])
    block_start = jnp.arange(n_blocks, dtype=jnp.int32) * MOE_BLOCK
    block_expert = jnp.minimum(jnp.searchsorted(padded_end, block_start, side="right"), N_EXPERTS - 1)
    xr = xt[row_token].reshape(n_blocks, MOE_BLOCK, d)

    def expert_block(args):
        xb, e = args
        return (jax.nn.silu(xb @ w1[e]) * (xb @ w3[e])) @ w2[e]

    yr = lax.map(expert_block, (xr, block_expert)).reshape(n_rows, d)
    out = jax.ops.segment_sum(yr * row_gate[:, None].astype(yr.dtype), row_token, num_segments=n)
    return out.reshape(b, s, d)


def setup_inputs(seed: int = 0) -> dict:
    key = jax.random.key(seed)
    k = jax.random.split(key, 30)
    ne, no = (DEPTH + 1) // 2, DEPTH // 2
    beta = (8.0 * DEPTH) ** -0.25

    def nrm(i, shape, scale):
        return scale * jax.random.normal(k[i], shape, F32)

    def gain(i, shape):
        return 1.0 + nrm(i, shape, 0.02)

    dt = jnp.exp(jax.random.uniform(k[5], (ne, SSD_HEADS), F32, math.log(1e-3), math.log(1e-1)))
    return {
        "x": nrm(0, (BATCH, SEQ, D_MODEL), 1.0),
        "ev_w_in": nrm(1, (ne, D_MODEL, EV_IN), D_MODEL ** -0.5),
        "ev_ret_norm_w": gain(2, (ne, RET_V)),
        "ev_conv_w": nrm(3, (ne, SSD_CONV, SSD_XBC), SSD_CONV ** -0.5),
        "ev_conv_b": nrm(4, (ne, SSD_XBC), 0.02),
        "ev_dt_bias": dt + jnp.log(-jnp.expm1(-dt)),
        "ev_a_log": jnp.log(jax.random.uniform(k[6], (ne, SSD_HEADS), F32, 1.0, 16.0)),
        "ev_d_skip": 1.0 + nrm(7, (ne, SSD_HEADS), 0.1),
        "ev_ssd_norm_w": gain(8, (ne, SSD_DI)),
        "ev_w_out": nrm(9, (ne, RET_V + SSD_DI, D_MODEL), beta * (RET_V + SSD_DI) ** -0.5),
        "ev_ln1_g": gain(10, (ne, D_MODEL)),
        "ev_ln1_b": nrm(11, (ne, D_MODEL), 0.02),
        "ffn_w1": nrm(12, (ne, D_MODEL, FFN_DIM), D_MODEL ** -0.5),
        "ffn_w3": nrm(13, (ne, D_MODEL, FFN_DIM), D_MODEL ** -0.5),
        "ffn_w2": nrm(14, (ne, FFN_DIM, D_MODEL), beta * FFN_DIM ** -0.5),
        "ev_ln2_g": gain(15, (ne, D_MODEL)),
        "ev_ln2_b": nrm(16, (ne, D_MODEL), 0.02),
        "od_w_in": nrm(17, (no, D_MODEL, OD_IN), D_MODEL ** -0.5),
        "hg_lb_logits": nrm(18, (DEPTH, HG_W), 0.5),
        "od_hg_norm_w": gain(19, (no, HG_V)),
        "od_w_out": nrm(20, (no, HG_V, D_MODEL), beta * HG_V ** -0.5),
        "od_ln1_g": gain(21, (no, D_MODEL)),
        "od_ln1_b": nrm(22, (no, D_MODEL), 0.02),
        "moe_router": nrm(23, (no, D_MODEL, N_EXPERTS), D_MODEL ** -0.5),
        "moe_w1": nrm(24, (no, N_EXPERTS, D_MODEL, FFN_DIM), D_MODEL ** -0.5),
        "moe_w3": nrm(25, (no, N_EXPERTS, D_MODEL, FFN_DIM), D_MODEL ** -0.5),
        "moe_w2": nrm(26, (no, N_EXPERTS, FFN_DIM, D_MODEL), beta * FFN_DIM ** -0.5),
        "od_ln2_g": gain(27, (no, D_MODEL)),
        "od_ln2_b": nrm(28, (no, D_MODEL), 0.02),
    }


def reference(x, ev_w_in, ev_ret_norm_w, ev_conv_w, ev_conv_b, ev_dt_bias, ev_a_log, ev_d_skip,
              ev_ssd_norm_w, ev_w_out, ev_ln1_g, ev_ln1_b, ffn_w1, ffn_w3, ffn_w2, ev_ln2_g, ev_ln2_b,
              od_w_in, hg_lb_logits, od_hg_norm_w, od_w_out, od_ln1_g, od_ln1_b, moe_router,
              moe_w1, moe_w3, moe_w2, od_ln2_g, od_ln2_b):
    alpha = (2.0 * DEPTH) ** 0.25
    lb_cum = jnp.cumsum(jax.nn.softmax(hg_lb_logits.astype(F32), axis=0), axis=0)
    lower_bounds = lb_cum - lb_cum[0]
    for layer in range(DEPTH):
        j = layer // 2
        if layer % 2 == 0:
            mix = retention_ssd_mixer(x, ev_w_in[j], ev_ret_norm_w[j], ev_conv_w[j], ev_conv_b[j],
                                      ev_dt_bias[j], ev_a_log[j], ev_d_skip[j], ev_ssd_norm_w[j],
                                      ev_w_out[j])
            x = layer_norm(alpha * x + mix, ev_ln1_g[j], ev_ln1_b[j])
            x = layer_norm(alpha * x + swiglu(x, ffn_w1[j], ffn_w3[j], ffn_w2[j]), ev_ln2_g[j], ev_ln2_b[j])
        else:
            mix = hgrn2_mixer(x, od_w_in[j], lower_bounds[layer], od_hg_norm_w[j], od_w_out[j])
            x = layer_norm(alpha * x + mix, od_ln1_g[j], od_ln1_b[j])
            ffn = moe_swiglu(x, moe_router[j], moe_w1[j], moe_w3[j], moe_w2[j])
            x = layer_norm(alpha * x + ffn, od_ln2_g[j], od_ln2_b[j])
    return x
```

```python
import math
from contextlib import ExitStack

import numpy as np
import concourse.bass as bass
import concourse.mybir as mybir
from concourse.alu_op_type import AluOpType as ALU
from concourse.bass_utils import run_bass_kernel_spmd

F32 = mybir.dt.float32
BF16 = mybir.dt.bfloat16
I32 = mybir.dt.int32
AF = mybir.ActivationFunctionType
AX = mybir.AxisListType

D = 2048
DEPTH = 4
LN_EPS = 1e-5
ALPHA = (2.0 * DEPTH) ** 0.25
FFN = 5632
NEXP = 8
EV_IN = 11296
SEM_LIMIT = 2000000


class Sem:
    def __init__(self, ctx, name):
        self.h = ctx.es.enter_context(ctx.nc.semaphore(name))
        self.v = 0
        ctx.sems.append(self)


class Buf:
    def __init__(self, ctx, t, name):
        self.ctx = ctx
        self.t = t
        self.name = name
        self.w = {}
        self.r = {}
        self.dsem = None

    def __getitem__(self, idx):
        return self.t[idx]

    def dma_sem(self):
        if self.dsem is None or self.dsem.v >= SEM_LIMIT:
            pool = self.ctx.sem_pool
            self.dsem = pool.pop() if pool else Sem(self.ctx, None)
            self.ctx.dma_bufs.append(self)
        return self.dsem


class Q:
    def __init__(self, ctx, name, eng, self_sync=True):
        self.ctx = ctx
        self.name = name
        self.eng = eng
        self.sem = None
        self.seen = {}
        self.self_sync = self_sync

    def _need(self, sem, val, pend):
        if not self.self_sync and sem is self.sem:
            return
        if self.seen.get(sem, 0) >= val or pend.get(sem, 0) >= val:
            return
        pend[sem] = val

    def _wait(self, sem, val):
        pend = {}
        self._need(sem, val, pend)
        self._emit(pend, None)

    def _emit(self, pend, keep_last):
        items = list(pend.items())
        ret = None
        if keep_last and items:
            ret = items.pop()
        for s, v in items:
            self.eng.wait_ge(s.h, v)
            self.seen[s] = v
        if ret is not None:
            self.seen[ret[0]] = ret[1]
        return ret

    def deps(self, reads, writes, keep_last=False):
        pend = {}
        for b in reads:
            for s, v in b.w.items():
                self._need(s, v, pend)
        for b in writes:
            for s, v in b.w.items():
                self._need(s, v, pend)
            for s, v in b.r.items():
                self._need(s, v, pend)
        return self._emit(pend, keep_last)

    def mark(self, tok, reads, writes):
        s, v = tok
        for b in reads:
            if b.r.get(s, 0) < v:
                b.r[s] = v
        for b in writes:
            if b.w.get(s, 0) < v:
                b.w[s] = v

    def op(self, ins_fn, reads=(), writes=(), single=None):
        if single is None:
            single = self.self_sync
        w = self.deps(reads, writes, keep_last=single)
        self.ctx._first = None
        ins = ins_fn()
        if w is not None:
            ins._wait_ge(w[0].h, w[1])
        if self.sem is None or self.sem.v >= SEM_LIMIT:
            self.sem = Sem(self.ctx, None)
        self.sem.v += 1
        ins.then_inc(self.sem.h, 1)
        self.mark((self.sem, self.sem.v), reads, writes)

    def dma(self, pairs, sb, reads=(), writes=(), **kw):
        w = self.deps(reads, writes, keep_last=True)
        s = sb.dma_sem()
        for out_ap, in_ap in pairs:
            s.v += 16
            ins = self.eng.dma_start(out=out_ap, in_=in_ap, **kw)
            if w is not None:
                ins._wait_ge(w[0].h, w[1])
                w = None
            ins.then_inc(s.h, 16)
        self.mark((s, s.v), reads, writes)


class Ctx:
    def __init__(self, nc, es):
        self.nc = nc
        self.es = es
        self.sems = []
        self.sem_pool = []
        self.dma_bufs = []
        self._first = None
        self.pe = Q(self, "pe", nc.tensor, self_sync=False)
        self.dve = Q(self, "dve", nc.vector)
        self.act = Q(self, "act", nc.scalar)
        self.pool = Q(self, "pool", nc.gpsimd)
        self.sp = Q(self, "sp", nc.sync)
        self.qs = [self.pe, self.dve, self.act, self.pool, self.sp]
        self.uid = 0

    def mm(self, *a, **k):
        ins = self.nc.tensor.matmul(*a, **k)
        if self._first is None:
            self._first = ins
        return ins

    def tr(self, *a, **k):
        ins = self.nc.tensor.transpose(*a, **k)
        if self._first is None:
            self._first = ins
        return ins

    def name(self, base):
        self.uid += 1
        return f"{base}_{self.uid}"

    def sbuf(self, es, shape, dt, name="sb"):
        t = es.enter_context(self.nc.sbuf_tensor(self.name(name), list(shape), dt))
        return Buf(self, t, name)

    def psum(self, es, shape, dt, name="ps"):
        t = es.enter_context(self.nc.psum_tensor(self.name(name), list(shape), dt))
        return Buf(self, t, name)

    def dram(self, name, shape, dt, kind="Internal"):
        t = self.nc.dram_tensor(name, list(shape), dt, kind=kind)
        return Buf(self, t.ap(), name)

    def barrier(self, qs=None):
        for q in (qs or self.qs):
            for s in self.sems:
                if s.v > 0:
                    q._wait(s, s.v)
        for b in self.dma_bufs:
            if b.dsem is not None and b.dsem.v < SEM_LIMIT:
                self.sem_pool.append(b.dsem)
            b.dsem = None
        self.dma_bufs = []


def gemm(ctx, A, K, T, Ws, C, epi_tile, epi_block, TS, WCB, n_par=1, acc=None, ps_bufs=None,
         a_row0=0, w_row0=0, keep_a=None):
    nc = ctx.nc
    KC = K // 128
    NT = TS // 512
    nW = len(Ws)
    with ExitStack() as es:
        a_sb = keep_a if keep_a is not None else ctx.sbuf(es, [128, KC, TS], BF16, "a_sb")
        w_sb = [[ctx.sbuf(es, [128, KC, WCB], BF16, "w_sb") for _ in range(2)] for _ in range(nW)]
        if ps_bufs is None:
            ps_bufs = [[ctx.psum(es, [128, 512], F32, "ps") for _ in range(2)] for _ in range(nW)]
        pi = 0
        wi = 0
        for st in range(T // TS):
            t0 = st * TS
            A_v = A.t[a_row0:a_row0 + K, t0:t0 + TS].rearrange("(kc p) t -> p kc t", p=128)
            g = 4 if KC % 4 == 0 else (2 if KC % 2 == 0 else 1)
            ctx.sp.dma([(a_sb[:, k0:k0 + g, :], A_v[:, k0:k0 + g, :]) for k0 in range(0, KC, g)],
                       a_sb, reads=[A], writes=[a_sb])
            for c0 in range(0, C, WCB):
                cw = min(WCB, C - c0)
                slot = wi % 2
                wi += 1
                for j, W in enumerate(Ws):
                    W_v = W[w_row0:w_row0 + K, c0:c0 + cw].rearrange("(kc p) c -> p kc c", p=128)
                    wb = w_sb[j][slot]
                    h = KC // 2 if KC % 2 == 0 else KC
                    ctx.pool.dma([(wb[:, k0:k0 + h, 0:cw], W_v[:, k0:k0 + h, :]) for k0 in range(0, KC, h)],
                                 wb, reads=[], writes=[wb])
                for m0 in range(0, cw, 128):
                    rows = min(128, cw - m0)
                    cb0 = c0 + m0
                    for nt in range(NT):
                        pss = []
                        for j in range(nW):
                            ps = ps_bufs[j][pi % 2]
                            wb = w_sb[j][slot]

                            def mm(ps=ps, wb=wb):
                                ins = None
                                for kc in range(KC):
                                    ins = ctx.mm(ps[0:rows, :], lhsT=wb[:, kc, m0:m0 + rows],
                                                           rhs=a_sb[:, kc, nt * 512:(nt + 1) * 512],
                                                           start=(kc == 0), stop=(kc == KC - 1))
                                return ins
                            ctx.pe.op(mm, reads=[wb, a_sb], writes=[ps])
                            pss.append(ps)
                        pi += 1
                        epi_tile(pss, cb0, rows, st, nt, t0)
                    if epi_block is not None:
                        epi_block(cb0, rows, st, t0)
        ctx.barrier()


class Stager:
    def __init__(self, ctx, es, TS, dt, n=3, name="stg"):
        self.ctx = ctx
        self.bufs = [ctx.sbuf(es, [128, TS], dt, name) for _ in range(n)]
        self.i = 0

    def cur(self):
        return self.bufs[self.i % len(self.bufs)]

    def next(self):
        self.i += 1


def make_plain_epi(ctx, colmap, stagers, flip=[0]):
    def epi_tile(pss, cb0, rows, st, nt, t0):
        dst, r0, func, key = colmap(cb0)
        sb = stagers[key].cur()
        ps = pss[0]
        sl = slice(nt * 512, (nt + 1) * 512)
        if func is None and flip[0] % 2 == 0:
            ctx.dve.op(lambda: ctx.nc.vector.tensor_copy(out=sb[0:rows, sl], in_=ps[0:rows, :]), reads=[ps], writes=[sb])
        else:
            f = AF.Copy if func is None else func
            ctx.act.op(lambda: ctx.nc.scalar.activation(out=sb[0:rows, sl], in_=ps[0:rows, :], func=f), reads=[ps], writes=[sb])
        flip[0] += 1

    def epi_block(cb0, rows, st, t0):
        dst, r0, func, key = colmap(cb0)
        stg = stagers[key]
        sb = stg.cur()
        TS = sb.t.shape[1]
        ctx.sp.dma([(dst.t[r0:r0 + rows, t0:t0 + TS], sb[0:rows, :])], sb, reads=[sb], writes=[dst])
        stg.next()

    return epi_tile, epi_block


def make_consts(ctx, es):
    nc = ctx.nc
    c = {}
    ones_f = ctx.sbuf(es, [128, 128], F32, "ones_f")
    ctx.pool.op(lambda: nc.gpsimd.memset(ones_f[:], 1.0), writes=[ones_f])
    ones_b = ctx.sbuf(es, [128, 128], BF16, "ones_b")
    ctx.pool.op(lambda: nc.gpsimd.memset(ones_b[:], 1.0), writes=[ones_b])
    ident_f = ctx.sbuf(es, [128, 128], F32, "ident_f")
    ctx.pool.op(lambda: nc.gpsimd.affine_select(out=ident_f[:], in_=ones_f[:], pattern=[[-1, 128]],
                                                compare_op=ALU.is_equal, fill=0.0, base=0, channel_multiplier=1),
                reads=[ones_f], writes=[ident_f])
    ident_b = ctx.sbuf(es, [128, 128], BF16, "ident_b")
    ctx.dve.op(lambda: nc.vector.tensor_copy(out=ident_b[:], in_=ident_f[:]), reads=[ident_f], writes=[ident_b])
    c["ones_f"], c["ones_b"], c["ident_f"], c["ident_b"] = ones_f, ones_b, ident_f, ident_b
    return c


def group_norm_tile(ctx, consts, wk, r, out, NB, gsz, center, nfeat, W=512):
    nc = ctx.nc
    rb, rsq, ps1, ps2, mean, rstd, tmp = wk["rb"], wk["rsq"], wk["ps1"], wk["ps2"], wk["mean"], wk["rstd"], wk["tmp"]
    ones_b = consts["ones_b"]
    inv = 1.0 / nfeat
    if center:
        ctx.act.op(lambda: nc.scalar.activation(out=rb[:, 0:NB, 0:W], in_=r[:, 0:NB, 0:W], func=AF.Copy), reads=[r], writes=[rb])
    ctx.pool.op(lambda: nc.gpsimd.tensor_tensor(out=rsq[:, 0:NB, 0:W], in0=r[:, 0:NB, 0:W], in1=r[:, 0:NB, 0:W], op=ALU.mult),
                reads=[r], writes=[rsq])
    for g0 in range(0, NB, gsz):
        if center:
            def mm1():
                ins = None
                for b in range(g0, g0 + gsz):
                    ins = ctx.mm(ps1[:, 0:W], lhsT=ones_b[:], rhs=rb[:, b, 0:W], start=(b == g0), stop=(b == g0 + gsz - 1))
                return ins
            ctx.pe.op(mm1, reads=[ones_b, rb], writes=[ps1])

        def mm2():
            ins = None
            for b in range(g0, g0 + gsz):
                ins = ctx.mm(ps2[:, 0:W], lhsT=ones_b[:], rhs=rsq[:, b, 0:W], start=(b == g0), stop=(b == g0 + gsz - 1))
            return ins
        ctx.pe.op(mm2, reads=[ones_b, rsq], writes=[ps2])
        if center:
            ctx.act.op(lambda: nc.scalar.activation(out=mean[:, 0:W], in_=ps1[:, 0:W], func=AF.Copy, scale=inv), reads=[ps1], writes=[mean])
            ctx.dve.op(lambda: nc.vector.tensor_tensor(out=tmp[:, 0:W], in0=mean[:, 0:W], in1=mean[:, 0:W], op=ALU.mult), reads=[mean], writes=[tmp])
            ctx.dve.op(lambda: nc.vector.scalar_tensor_tensor(out=tmp[:, 0:W], in0=ps2[:, 0:W], scalar=inv, in1=tmp[:, 0:W],
                                                              op0=ALU.mult, op1=ALU.subtract), reads=[ps2, tmp], writes=[tmp])
            ctx.dve.op(lambda: nc.vector.tensor_scalar(out=tmp[:, 0:W], in0=tmp[:, 0:W], scalar1=0.0, scalar2=LN_EPS, op0=ALU.max, op1=ALU.add),
                       reads=[tmp], writes=[tmp])
        else:
            ctx.dve.op(lambda: nc.vector.tensor_scalar(out=tmp[:, 0:W], in0=ps2[:, 0:W], scalar1=inv, scalar2=LN_EPS, op0=ALU.mult, op1=ALU.add),
                       reads=[ps2], writes=[tmp])
        ctx.act.op(lambda: nc.scalar.activation(out=tmp[:, 0:W], in_=tmp[:, 0:W], func=AF.Sqrt), reads=[tmp], writes=[tmp])
        ctx.dve.op(lambda: nc.vector.reciprocal(out=rstd[:, 0:W], in_=tmp[:, 0:W]), reads=[tmp], writes=[rstd])
        for b in range(g0, g0 + gsz):
            q = ctx.dve if b % 2 == 0 else ctx.pool
            e = nc.vector if b % 2 == 0 else nc.gpsimd
            if center:
                q.op(lambda e=e, b=b: e.tensor_tensor(out=out[:, b, 0:W], in0=r[:, b, 0:W], in1=mean[:, 0:W], op=ALU.subtract), reads=[r, mean], writes=[out])
                q.op(lambda e=e, b=b: e.tensor_tensor(out=out[:, b, 0:W], in0=out[:, b, 0:W], in1=rstd[:, 0:W], op=ALU.mult), reads=[out, rstd], writes=[out])
            else:
                q.op(lambda e=e, b=b: e.tensor_tensor(out=out[:, b, 0:W], in0=r[:, b, 0:W], in1=rstd[:, 0:W], op=ALU.mult), reads=[r, rstd], writes=[out])


def norm_work(ctx, es, NB, W=512):
    return {
        "rb": ctx.sbuf(es, [128, NB, W], BF16, "n_rb"),
        "rsq": ctx.sbuf(es, [128, NB, W], BF16, "n_rsq"),
        "ps1": ctx.psum(es, [128, 512], F32, "n_ps1"),
        "ps2": ctx.psum(es, [128, 512], F32, "n_ps2"),
        "mean": ctx.sbuf(es, [128, W], F32, "n_mean"),
        "rstd": ctx.sbuf(es, [128, W], F32, "n_rstd"),
        "tmp": ctx.sbuf(es, [128, W], F32, "n_tmp"),
    }


def ln_phase(ctx, consts, XT, M, XB, gvec, bvec, T, out_final=None):
    nc = ctx.nc
    NB = D // 128
    with ExitStack() as es:
        wk = norm_work(ctx, es, NB)
        gb = ctx.sbuf(es, [128, 2, NB], F32, "ln_gb")
        ctx.sp.dma([(gb[:, 0, :], gvec.rearrange("(b p) -> p b", p=128)), (gb[:, 1, :], bvec.rearrange("(b p) -> p b", p=128))],
                   gb, writes=[gb], allow_slow_non_contiguous=True)
        xs = [ctx.sbuf(es, [128, NB, 512], F32, "ln_x") for _ in range(2)]
        ms = [ctx.sbuf(es, [128, NB, 512], F32, "ln_m") for _ in range(2)]
        ob = [ctx.sbuf(es, [128, NB, 512], BF16, "ln_ob") for _ in range(2)]
        for nt in range(T // 512):
            x, m, o = xs[nt % 2], ms[nt % 2], ob[nt % 2]
            tsl = slice(nt * 512, (nt + 1) * 512)
            ctx.sp.dma([(x[:, :, :], XT.t[:, tsl].rearrange("(b p) t -> p b t", p=128))], x, reads=[XT], writes=[x])
            ctx.sp.dma([(m[:, :, :], M.t[:, tsl].rearrange("(b p) t -> p b t", p=128))], m, reads=[M], writes=[m])
            ctx.dve.op(lambda: nc.vector.scalar_tensor_tensor(out=m[:, :, :], in0=x[:, :, :], scalar=ALPHA, in1=m[:, :, :], op0=ALU.mult, op1=ALU.add),
                       reads=[x, m], writes=[m])
            group_norm_tile(ctx, consts, wk, m, x, NB, NB, True, D)
            for b in range(NB):
                q = ctx.dve if b % 2 == 0 else ctx.pool
                e = nc.vector if b % 2 == 0 else nc.gpsimd
                q.op(lambda e=e, b=b: e.tensor_scalar(out=x[:, b, :], in0=x[:, b, :], scalar1=gb[:, 0, b:b + 1], scalar2=gb[:, 1, b:b + 1],
                                                     op0=ALU.mult, op1=ALU.add), reads=[x, gb], writes=[x])
            if out_final is not None:
                ctx.sp.dma([(out_final.t[:, tsl].rearrange("(b p) t -> p b t", p=128), x[:, :, :])], x, reads=[x], writes=[out_final])
            else:
                ctx.act.op(lambda: nc.scalar.activation(out=o[:, :, :], in_=x[:, :, :], func=AF.Copy), reads=[x], writes=[o])
                ctx.sp.dma([(XT.t[:, tsl].rearrange("(b p) t -> p b t", p=128), x[:, :, :])], x, reads=[x], writes=[XT])
                ctx.sp.dma([(XB.t[:, tsl].rearrange("(b p) t -> p b t", p=128), o[:, :, :])], o, reads=[o], writes=[XB])
        ctx.barrier()


def swiglu_up(ctx, XB, T, W1s, W3s, H, GB=None):
    nc = ctx.nc
    TS = min(T, 2048)
    with ExitStack() as es:
        stg = Stager(ctx, es, TS, BF16, 3, "h_stg")
        tmps = [ctx.sbuf(es, [128, 512], F32, "sw_tmp") for _ in range(3)]
        gts = [ctx.sbuf(es, [128, TS], F32, "gate_t") for _ in range(2)] if GB is not None else None
        ti = [0]
        for e, (W1, W3) in enumerate(zip(W1s, W3s)):
            cur_g = [None]

            def epi_tile(pss, cb0, rows, st, nt, t0, e=e):
                tmp = tmps[ti[0] % 3]
                ti[0] += 1
                sb = stg.cur()
                sl = slice(nt * 512, (nt + 1) * 512)
                ctx.act.op(lambda: nc.scalar.activation(out=tmp[:, :], in_=pss[0][:, :], func=AF.Silu), reads=[pss[0]], writes=[tmp])
                if GB is not None:
                    if nt == 0 and cb0 == 0:
                        g = gts[(e + st) % 2]
                        ctx.sp.dma([(g[:, :], GB.t[e, t0:t0 + TS].partition_broadcast(128))], g, reads=[GB], writes=[g])
                        cur_g[0] = g
                    g = cur_g[0]
                    ctx.dve.op(lambda: nc.vector.tensor_tensor(out=tmp[:, :], in0=tmp[:, :], in1=pss[1][:, :], op=ALU.mult), reads=[tmp, pss[1]], writes=[tmp])
                    ctx.dve.op(lambda: nc.vector.tensor_tensor(out=sb[:, sl], in0=tmp[:, :], in1=g[:, sl], op=ALU.mult), reads=[tmp, g], writes=[sb])
                else:
                    ctx.dve.op(lambda: nc.vector.tensor_tensor(out=sb[:, sl], in0=tmp[:, :], in1=pss[1][:, :], op=ALU.mult), reads=[tmp, pss[1]], writes=[sb])

            def epi_block(cb0, rows, st, t0, e=e):
                sb = stg.cur()
                ctx.sp.dma([(H[e].t[cb0:cb0 + rows, t0:t0 + TS], sb[0:rows, :])], sb, reads=[sb], writes=[H[e]])
                stg.next()

            gemm(ctx, XB, D, T, [W1, W3], FFN, epi_tile, epi_block, TS, 256)


def cast_dram(ctx, dst, src):
    ctx.pool.dma([(dst.t[:, :], src.t[:, :])], dst, reads=[src], writes=[dst])


def ffn_down(ctx, H, T, W2, M, K=FFN):
    with ExitStack() as es:
        stg = Stager(ctx, es, 512, F32, 4, "m_stg")
        et, eb = make_plain_epi(ctx, lambda cb0: (M, cb0, None, "m"), {"m": stg})
        gemm(ctx, H, K, T, [W2], D, et, eb, 512, 256)


def finish(ctx, bufs):
    ctx.barrier()


def all_gather(ctx, dst, src, n_cores):
    q = ctx.pool
    q.deps([src], [dst])
    if getattr(ctx, "cc_sem", None) is None or ctx.cc_sem.v >= SEM_LIMIT:
        ctx.cc_sem = Sem(ctx, None)
    s = ctx.cc_sem
    s.v += 1
    ctx.nc.gpsimd.collective_compute("AllGather", ALU.bypass, replica_groups=[list(range(n_cores))],
                                     ins=[src.t.opt()], outs=[dst.t.opt()]).then_inc(s.h)
    q.mark((s, s.v), [src], [dst])


SEG = 512
CH = 32
NCH = SEG // CH


def gla_scan(ctx, consts, T, nheads, dv, Y, seg_setup, prologue, nslots=3, scalar_decay=False):
    nc = ctx.nc
    nvb = (dv + 127) // 128
    dvb = min(dv, 128)
    G = 2
    with ExitStack() as es:
        m0 = ctx.sbuf(es, [128, NCH, CH], F32, "m0")
        ctx.pool.op(lambda: nc.gpsimd.memset(m0[:, :, :], 1.0), writes=[m0])
        ctx.pool.op(lambda: nc.gpsimd.memset(m0[:, :, 0:1], 0.0), writes=[m0])
        cm = ctx.sbuf(es, [CH, NCH, CH], F32, "cm")
        ctx.pool.op(lambda: nc.gpsimd.memset(cm[:, :, :], 1.0), writes=[cm])
        ctx.pool.op(lambda: nc.gpsimd.affine_select(out=cm[:, :, :], in_=cm[:, :, :], pattern=[[0, NCH], [1, CH]],
                                                    compare_op=ALU.is_ge, fill=0.0, base=0, channel_multiplier=-1),
                    reads=[cm], writes=[cm])
        if scalar_decay:
            idm = ctx.sbuf(es, [CH, NCH, CH], F32, "idm")
            ctx.pool.op(lambda: nc.gpsimd.memset(idm[:, :, :], 1.0), writes=[idm])
            ctx.pool.op(lambda: nc.gpsimd.affine_select(out=idm[:, :, :], in_=idm[:, :, :], pattern=[[0, NCH], [1, CH]],
                                                        compare_op=ALU.is_equal, fill=0.0, base=0, channel_multiplier=-1), reads=[idm], writes=[idm])
            ngm = ctx.sbuf(es, [CH, NCH, CH], F32, "ngm")
            ctx.pool.op(lambda: nc.gpsimd.memset(ngm[:, :, :], 0.0), writes=[ngm])
            ctx.pool.op(lambda: nc.gpsimd.affine_select(out=ngm[:, :, :], in_=ngm[:, :, :], pattern=[[0, NCH], [1, CH]],
                                                        compare_op=ALU.is_ge, fill=-1.0e30, base=0, channel_multiplier=-1), reads=[ngm], writes=[ngm])
        S = ctx.sbuf(es, [128, nheads, dv], F32, "S")
        Sb = ctx.sbuf(es, [128, nheads, dv], BF16, "Sb")
        Sh = [Buf(ctx, S.t, "S_h") for _ in range(nheads)]
        Sbh = [Buf(ctx, Sb.t, "Sb_h") for _ in range(nheads)]
        ctx.pool.op(lambda: nc.gpsimd.memset(S[:, :, :], 0.0), writes=Sh)
        ctx.pool.op(lambda: nc.gpsimd.memset(Sb[:, :, :], 0.0), writes=Sbh)
        slots = []
        for i in range(nslots):
            sl = {
                "qf": ctx.sbuf(es, [128, SEG], F32, "qf"), "kf": ctx.sbuf(es, [128, SEG], F32, "kf"),
                "gf": ctx.sbuf(es, [128, SEG], F32, "gf"), "cum": ctx.sbuf(es, [128, NCH, CH], F32, "cum"),
                "t1": ctx.sbuf(es, [128, NCH, CH], F32, "t1"), "e": ctx.sbuf(es, [128, NCH, CH], F32, "e"),
                "qt": ctx.sbuf(es, [128, SEG], BF16, "qt"), "kt": ctx.sbuf(es, [128, SEG], BF16, "kt"),
                "qh": ctx.sbuf(es, [128, SEG], BF16, "qh"), "kd": ctx.sbuf(es, [128, SEG], BF16, "kd"),
                "dcol": ctx.sbuf(es, [128, NCH], F32, "dcol"),
                "dm": ctx.sbuf(es, [CH, NCH, CH], F32, "dm"), "col": ctx.sbuf(es, [CH, NCH], F32, "col"),
                "vT": [ctx.sbuf(es, [dvb, SEG], BF16, "vT") for _ in range(nvb)],
                "vtok": ctx.sbuf(es, [CH, NCH, dv], BF16, "vtok"), "kdtok": ctx.sbuf(es, [CH, NCH, 128], BF16, "kdtok"),
                "sc": ctx.sbuf(es, [CH, SEG], BF16, "sc"),
                "ysb": [ctx.sbuf(es, [dvb, SEG], F32, "ysb") for _ in range(nvb)],
                "aux": [ctx.sbuf(es, [128, SEG], F32, "aux") for _ in range(2)],
                "auxb": [ctx.sbuf(es, [128, SEG], BF16, "auxb") for _ in range(2)],
            }
            slots.append(sl)
        ps_sc = [ctx.psum(es, [CH, SEG], F32, "ps_sc") for _ in range(G)]
        ps_y = [[ctx.psum(es, [dvb, SEG], F32, "ps_y") for _ in range(nvb)] for _ in range(G)]
        ps_tr = ctx.psum(es, [CH, 1024], BF16, "ps_tr")
        n_u = G if (2 + G * nvb + 1 + G) <= 8 else 1
        ps_u = [ctx.psum(es, [128, 512], F32, "ps_u") for _ in range(n_u)]
        ps_misc = ps_u[0]
        ident_b = consts["ident_b"]
        si = 0
        for seg in range(T // SEG):
            t0 = seg * SEG
            seg_setup(seg, t0)
            for h0 in range(0, nheads, G):
                hs = list(range(h0, min(h0 + G, nheads)))
                sls = []
                for h in hs:
                    sl = slots[si % nslots]
                    si += 1
                    sls.append(sl)
                    prologue(h, seg, t0, sl, ps_misc)
                    qf, kf, gf, cum, t1, e = sl["qf"], sl["kf"], sl["gf"], sl["cum"], sl["t1"], sl["e"]
                    cumf = cum.t.rearrange("p c t -> p (c t)")
                    ctx.dve.op(lambda: nc.vector.tensor_tensor_scan(out=cumf, data0=m0.t.rearrange("p c t -> p (c t)"), data1=gf[:, :],
                                                                    initial=0.0, op0=ALU.mult, op1=ALU.add), reads=[m0, gf], writes=[cum])
                    if scalar_decay:
                        dm, col = sl["dm"], sl["col"]
                        ctx.dve.op(lambda: nc.vector.tensor_tensor(out=dm[:, :, :], in0=cum[0:CH, :, :], in1=idm[:, :, :], op=ALU.mult), reads=[cum, idm], writes=[dm])
                        ctx.dve.op(lambda: nc.vector.tensor_reduce(out=col[:, :], in_=dm[:, :, :], axis=AX.X, op=ALU.add), reads=[dm], writes=[col])
                        ctx.dve.op(lambda: nc.vector.tensor_tensor(out=dm[:, :, :], in0=cum[0:CH, :, :], in1=col[:, :].unsqueeze(2).to_broadcast([CH, NCH, CH]),
                                                                   op=ALU.subtract), reads=[cum, col], writes=[dm])
                        ctx.dve.op(lambda: nc.vector.tensor_tensor(out=dm[:, :, :], in0=dm[:, :, :], in1=ngm[:, :, :], op=ALU.min), reads=[dm, ngm], writes=[dm])
                        ctx.act.op(lambda: nc.scalar.activation(out=dm[:, :, :], in_=dm[:, :, :], func=AF.Exp), reads=[dm], writes=[dm])
                        ctx.act.op(lambda: nc.scalar.activation(out=sl["qt"][:, :], in_=qf[:, :], func=AF.Copy), reads=[qf], writes=[sl["qt"]])
                        ctx.pool.op(lambda: nc.gpsimd.tensor_copy(out=sl["kt"][:, :], in_=kf[:, :]), reads=[kf], writes=[sl["kt"]])
                    else:
                        ctx.dve.op(lambda: nc.vector.tensor_tensor(out=t1[:, :, :], in0=cum[:, :, :], in1=cum[:, :, 15:16].to_broadcast([128, NCH, CH]),
                                                                   op=ALU.subtract), reads=[cum], writes=[t1])
                        ctx.act.op(lambda: nc.scalar.activation(out=e[:, :, :], in_=t1[:, :, :], func=AF.Exp), reads=[t1], writes=[e])
                        ctx.dve.op(lambda: nc.vector.tensor_tensor(out=sl["qt"][:, :], in0=qf[:, :], in1=e.t.rearrange("p c t -> p (c t)"), op=ALU.mult),
                                   reads=[qf, e], writes=[sl["qt"]])
                        ctx.act.op(lambda: nc.scalar.activation(out=e[:, :, :], in_=t1[:, :, :], func=AF.Exp, scale=-1.0), reads=[t1], writes=[e])
                        ctx.pool.op(lambda: nc.gpsimd.tensor_tensor(out=sl["kt"][:, :], in0=kf[:, :], in1=e.t.rearrange("p c t -> p (c t)"), op=ALU.mult),
                                    reads=[kf, e], writes=[sl["kt"]])
                    ctx.act.op(lambda: nc.scalar.activation(out=e[:, :, :], in_=cum[:, :, :], func=AF.Exp), reads=[cum], writes=[e])
                    ctx.dve.op(lambda: nc.vector.tensor_tensor(out=sl["qh"][:, :], in0=qf[:, :], in1=e.t.rearrange("p c t -> p (c t)"), op=ALU.mult),
                               reads=[qf, e], writes=[sl["qh"]])
                    ctx.act.op(lambda: nc.scalar.activation(out=sl["dcol"][:, :], in_=cum[:, :, CH - 1], func=AF.Exp), reads=[cum], writes=[sl["dcol"]])
                    ctx.pool.op(lambda: nc.gpsimd.tensor_tensor(out=t1[:, :, :], in0=cum[:, :, :], in1=cum[:, :, CH - 1:CH].to_broadcast([128, NCH, CH]),
                                                                op=ALU.subtract), reads=[cum], writes=[t1])
                    ctx.act.op(lambda: nc.scalar.activation(out=e[:, :, :], in_=t1[:, :, :], func=AF.Exp, scale=-1.0), reads=[t1], writes=[e])
                    ctx.pool.op(lambda: nc.gpsimd.tensor_tensor(out=sl["kd"][:, :], in0=kf[:, :], in1=e.t.rearrange("p c t -> p (c t)"), op=ALU.mult),
                                reads=[kf, e], writes=[sl["kd"]])
                    per = 1024 // dv
                    for c0 in range(0, NCH, per):
                        def trv(c0=c0):
                            ins = None
                            for c in range(c0, c0 + per):
                                for vb in range(nvb):
                                    o0 = (c - c0) * dv + vb * 128
                                    ins = ctx.tr(out=ps_tr[0:CH, o0:o0 + dvb], in_=sl["vT"][vb][0:dvb, c * CH:(c + 1) * CH],
                                                              identity=ident_b[0:dvb, 0:dvb])
                            return ins
                        ctx.pe.op(trv, reads=sl["vT"] + [ident_b], writes=[ps_tr])
                        ctx.act.op(lambda c0=c0: nc.scalar.activation(out=sl["vtok"][:, c0:c0 + per, :], in_=ps_tr.t[0:CH, 0:per * dv].rearrange("p (c d) -> p c d", d=dv),
                                                                      func=AF.Copy), reads=[ps_tr], writes=[sl["vtok"]])
                    for c0 in range(0, NCH, 8):
                        def trk(c0=c0):
                            ins = None
                            for c in range(c0, c0 + 8):
                                ins = ctx.tr(out=ps_tr[0:CH, (c - c0) * 128:(c - c0 + 1) * 128], in_=sl["kd"][:, c * CH:(c + 1) * CH],
                                                          identity=ident_b[:, :])
                            return ins
                        ctx.pe.op(trk, reads=[sl["kd"], ident_b], writes=[ps_tr])
                        ctx.dve.op(lambda c0=c0: nc.vector.tensor_copy(out=sl["kdtok"][:, c0:c0 + 8, :], in_=ps_tr.t[0:CH, 0:1024].rearrange("p (c d) -> p c d", d=128)),
                                   reads=[ps_tr], writes=[sl["kdtok"]])
                for gi, h in enumerate(hs):
                    sl = sls[gi]

                    def scm(sl=sl, gi=gi):
                        ins = None
                        for c in range(NCH):
                            cs = slice(c * CH, (c + 1) * CH)
                            ins = ctx.mm(ps_sc[gi][0:CH, cs], lhsT=sl["kt"][:, cs], rhs=sl["qt"][:, cs], start=True, stop=True)
                        return ins
                    ctx.pe.op(scm, reads=[sl["kt"], sl["qt"]], writes=[ps_sc[gi]])
                    mkb = sl["dm"] if scalar_decay else cm
                    ctx.dve.op(lambda sl=sl, gi=gi, mkb=mkb: nc.vector.tensor_tensor(out=sl["sc"][:, :], in0=ps_sc[gi][:, :], in1=mkb.t.rearrange("p c t -> p (c t)"), op=ALU.mult),
                               reads=[ps_sc[gi], mkb], writes=[sl["sc"]])
                for c in range(NCH):
                    cs = slice(c * CH, (c + 1) * CH)
                    for gi, h in enumerate(hs):
                        sl = sls[gi]

                        def ymm(sl=sl, gi=gi, h=h):
                            ins = None
                            for vb in range(nvb):
                                vs = slice(vb * 128, vb * 128 + dvb)
                                ctx.mm(ps_y[gi][vb][0:dvb, cs], lhsT=sl["vtok"][0:CH, c, vs], rhs=sl["sc"][0:CH, cs], start=True, stop=False)
                                ins = ctx.mm(ps_y[gi][vb][0:dvb, cs], lhsT=Sb[:, h, vs], rhs=sl["qh"][:, cs], start=False, stop=True)
                            return ins
                        ctx.pe.op(ymm, reads=[sl["vtok"], sl["sc"], Sbh[h], sl["qh"]], writes=ps_y[gi])
                        pu = ps_u[gi % n_u]
                        ctx.pe.op(lambda sl=sl, pu=pu: ctx.mm(pu[:, 0:dv], lhsT=sl["kdtok"][0:CH, c, :], rhs=sl["vtok"][0:CH, c, :], start=True, stop=True),
                                  reads=[sl["kdtok"], sl["vtok"]], writes=[pu])
                        ctx.dve.op(lambda sl=sl, h=h, pu=pu: nc.vector.scalar_tensor_tensor(out=Sb[:, h, :], in0=S[:, h, :], scalar=sl["dcol"][:, c:c + 1], in1=pu[:, 0:dv],
                                                                                           op0=ALU.mult, op1=ALU.add), reads=[Sh[h], sl["dcol"], pu], writes=[Sbh[h]])
                        ctx.dve.op(lambda sl=sl, h=h, pu=pu: nc.vector.scalar_tensor_tensor(out=S[:, h, :], in0=S[:, h, :], scalar=sl["dcol"][:, c:c + 1], in1=pu[:, 0:dv],
                                                                                           op0=ALU.mult, op1=ALU.add), reads=[Sh[h], sl["dcol"], pu], writes=[Sh[h]])
                for gi, h in enumerate(hs):
                    sl = sls[gi]
                    for vb in range(nvb):
                        ctx.act.op(lambda sl=sl, gi=gi, vb=vb: nc.scalar.activation(out=sl["ysb"][vb][:, :], in_=ps_y[gi][vb][:, :], func=AF.Copy),
                                   reads=[ps_y[gi][vb]], writes=[sl["ysb"][vb]])
                        r0 = h * dv + vb * 128
                        ctx.sp.dma([(Y.t[r0:r0 + dvb, t0:t0 + SEG], sl["ysb"][vb][:, :])], sl["ysb"][vb], reads=[sl["ysb"][vb]], writes=[Y])
        ctx.barrier()


def lb_setup(ctx, es, logits_ap):
    nc = ctx.nc
    lg = ctx.sbuf(es, [128, 16, DEPTH], F32, "lb_lg")
    ctx.sp.dma([(lg[:, :, l], logits_ap[l].rearrange("(b p) -> p b", p=128)) for l in range(DEPTH)], lg, writes=[lg], allow_slow_non_contiguous=True)
    ctx.act.op(lambda: nc.scalar.activation(out=lg[:, :, :], in_=lg[:, :, :], func=AF.Exp), reads=[lg], writes=[lg])
    s = ctx.sbuf(es, [128, 16], F32, "lb_s")
    ctx.dve.op(lambda: nc.vector.tensor_reduce(out=s[:, :], in_=lg[:, :, :], axis=AX.X, op=ALU.add), reads=[lg], writes=[s])
    ctx.dve.op(lambda: nc.vector.reciprocal(out=s[:, :], in_=s[:, :]), reads=[s], writes=[s])
    res = {}
    for layer in (1, 3):
        lb = ctx.sbuf(es, [128, 16], F32, "lb")
        oml = ctx.sbuf(es, [128, 16], F32, "oml")
        noml = ctx.sbuf(es, [128, 16], F32, "noml")
        ctx.dve.op(lambda: nc.vector.tensor_copy(out=lb[:, :], in_=lg[:, :, 1]), reads=[lg], writes=[lb])
        for l in range(2, layer + 1):
            ctx.dve.op(lambda l=l: nc.vector.tensor_tensor(out=lb[:, :], in0=lb[:, :], in1=lg[:, :, l], op=ALU.add), reads=[lb, lg], writes=[lb])
        ctx.dve.op(lambda: nc.vector.tensor_tensor(out=lb[:, :], in0=lb[:, :], in1=s[:, :], op=ALU.mult), reads=[lb, s], writes=[lb])
        ctx.dve.op(lambda: nc.vector.tensor_scalar(out=oml[:, :], in0=lb[:, :], scalar1=-1.0, scalar2=1.0, op0=ALU.mult, op1=ALU.add), reads=[lb], writes=[oml])
        ctx.dve.op(lambda: nc.vector.tensor_scalar(out=noml[:, :], in0=oml[:, :], scalar1=-1.0, scalar2=None, op0=ALU.mult), reads=[oml], writes=[noml])
        res[layer] = (lb, oml, noml)
    return res


def conv_phase(ctx, T, XBC, XBCc, conv_w, conv_b):
    nc = ctx.nc
    NBK = 3072 // 128
    with ExitStack() as es:
        w = ctx.sbuf(es, [128, NBK, 4], F32, "cv_w")
        b = ctx.sbuf(es, [128, NBK], F32, "cv_b")
        ctx.sp.dma([(w[:, :, jj], conv_w[jj].rearrange("(b p) -> p b", p=128)) for jj in range(4)], w, writes=[w], allow_slow_non_contiguous=True)
        ctx.sp.dma([(b[:, :], conv_b.rearrange("(b p) -> p b", p=128))], b, writes=[b], allow_slow_non_contiguous=True)
        xps = [ctx.sbuf(es, [128, T + 3], BF16, "cv_x") for _ in range(2)]
        accs = [ctx.sbuf(es, [128, T], F32, "cv_a") for _ in range(2)]
        outs = [ctx.sbuf(es, [128, T], BF16, "cv_o") for _ in range(2)]
        for xp in xps:
            ctx.pool.op(lambda xp=xp: nc.gpsimd.memset(xp[:, 0:3], 0.0), writes=[xp])
        for blk in range(NBK):
            xp, acc, o = xps[blk % 2], accs[blk % 2], outs[blk % 2]
            ctx.sp.dma([(xp[:, 3:T + 3], XBC.t[blk * 128:(blk + 1) * 128, :])], xp, reads=[XBC], writes=[xp])
            ctx.dve.op(lambda: nc.vector.tensor_scalar(out=acc[:, :], in0=xp[:, 0:T], scalar1=w[:, blk, 0:1], scalar2=b[:, blk:blk + 1], op0=ALU.mult, op1=ALU.add),
                       reads=[xp, w, b], writes=[acc])
            for j in range(1, 4):
                ctx.dve.op(lambda j=j: nc.vector.scalar_tensor_tensor(out=acc[:, :], in0=xp[:, j:j + T], scalar=w[:, blk, j:j + 1], in1=acc[:, :], op0=ALU.mult, op1=ALU.add),
                           reads=[xp, w, acc], writes=[acc])
            ctx.act.op(lambda: nc.scalar.activation(out=o[:, :], in_=acc[:, :], func=AF.Silu), reads=[acc], writes=[o])
            ctx.sp.dma([(XBCc.t[blk * 128:(blk + 1) * 128, :], o[:, :])], o, reads=[o], writes=[XBCc])
        ctx.barrier()


def dt_phase(ctx, T, DT, SDT, dt_bias, a_log):
    nc = ctx.nc
    with ExitStack() as es:
        x = ctx.sbuf(es, [32, T], F32, "dt_x")
        y = ctx.sbuf(es, [32, T], F32, "dt_y")
        pb = ctx.sbuf(es, [32, 2], F32, "dt_pb")
        ctx.sp.dma([(pb[:, 0:1], dt_bias.rearrange("(h o) -> h o", o=1)), (pb[:, 1:2], a_log.rearrange("(h o) -> h o", o=1))], pb, writes=[pb],
                   allow_slow_non_contiguous=True)
        ctx.sp.dma([(x[:, :], DT.t[:, :])], x, reads=[DT], writes=[x])
        ctx.act.op(lambda: nc.scalar.activation(out=pb[:, 1:2], in_=pb[:, 1:2], func=AF.Exp), reads=[pb], writes=[pb])
        ctx.dve.op(lambda: nc.vector.tensor_scalar(out=pb[:, 1:2], in0=pb[:, 1:2], scalar1=-1.0, scalar2=None, op0=ALU.mult), reads=[pb], writes=[pb])
        ctx.act.op(lambda: nc.scalar.activation(out=x[:, :], in_=x[:, :], func=AF.Exp, bias=pb[:, 0:1]), reads=[x, pb], writes=[x])
        ctx.act.op(lambda: nc.scalar.activation(out=x[:, :], in_=x[:, :], func=AF.Ln, bias=1.0), reads=[x], writes=[x])
        ctx.dve.op(lambda: nc.vector.tensor_scalar(out=y[:, :], in0=x[:, :], scalar1=pb[:, 1:2], scalar2=None, op0=ALU.mult), reads=[x, pb], writes=[y])
        ctx.sp.dma([(SDT.t[0:32, :], x[:, :])], x, reads=[x], writes=[SDT])
        ctx.sp.dma([(SDT.t[32:64, :], y[:, :])], y, reads=[y], writes=[SDT])
        ctx.barrier()


def ret_scan(ctx, consts, T, P, Y):
    nc = ctx.nc
    with ExitStack() as es:
        ones_f = consts["ones_f"]
        ra = ctx.sbuf(es, [128, 128], F32, "rot_a")
        rb_ = ctx.sbuf(es, [128, 128], F32, "rot_b")
        rot = ctx.sbuf(es, [128, 128], BF16, "rot")
        ctx.pool.op(lambda: nc.gpsimd.affine_select(out=ra[:, :], in_=ones_f[:, :], pattern=[[-1, 128]], compare_op=ALU.is_equal, fill=0.0, base=64, channel_multiplier=1),
                    reads=[ones_f], writes=[ra])
        ctx.pool.op(lambda: nc.gpsimd.affine_select(out=rb_[:, :], in_=ones_f[:, :], pattern=[[-1, 128]], compare_op=ALU.is_equal, fill=0.0, base=-64, channel_multiplier=1),
                    reads=[ones_f], writes=[rb_])
        ctx.dve.op(lambda: nc.vector.tensor_tensor(out=rot[:, :], in0=ra[:, :], in1=rb_[:, :], op=ALU.subtract), reads=[ra, rb_], writes=[rot])
        pidx = ctx.sbuf(es, [128, 1], F32, "pidx")
        for half in range(2):
            ctx.pool.op(lambda half=half: nc.gpsimd.iota(pidx[half * 64:(half + 1) * 64, :], pattern=[[0, 1]], base=0, channel_multiplier=1,
                                                         allow_small_or_imprecise_dtypes=True), writes=[pidx])
        ifr = ctx.sbuf(es, [128, 1], F32, "ifr")
        ctx.act.op(lambda: nc.scalar.activation(out=ifr[:, :], in_=pidx[:, :], func=AF.Exp, scale=-math.log(10000.0) / 64.0), reads=[pidx], writes=[ifr])
        ctx.dve.op(lambda: nc.vector.tensor_scalar(out=ifr[:, :], in0=ifr[:, :], scalar1=1.0 / (2.0 * math.pi), scalar2=None, op0=ALU.mult), reads=[ifr], writes=[ifr])
        pos = ctx.sbuf(es, [128, SEG], F32, "pos")
        fr = ctx.sbuf(es, [128, SEG], F32, "fr")
        fi = ctx.sbuf(es, [128, SEG], I32, "fi")
        ff = ctx.sbuf(es, [128, SEG], F32, "ff")
        msk = ctx.sbuf(es, [128, SEG], F32, "msk")
        tabs = {k: ctx.sbuf(es, [128, SEG], F32, "tab_" + k) for k in ("cos", "sin", "cosk", "sink")}
        tb = Buf(ctx, None, "tabs")
        ks = 128.0 ** -0.5

        def wrap(src):
            ctx.dve.op(lambda: nc.vector.tensor_scalar(out=msk[:, :], in0=src[:, :], scalar1=0.5, scalar2=None, op0=ALU.is_gt), reads=[src], writes=[msk])
            ctx.dve.op(lambda: nc.vector.tensor_tensor(out=src[:, :], in0=src[:, :], in1=msk[:, :], op=ALU.subtract), reads=[src, msk], writes=[src])
            ctx.dve.op(lambda: nc.vector.tensor_scalar(out=msk[:, :], in0=src[:, :], scalar1=-0.5, scalar2=None, op0=ALU.is_lt), reads=[src], writes=[msk])
            ctx.dve.op(lambda: nc.vector.tensor_tensor(out=src[:, :], in0=src[:, :], in1=msk[:, :], op=ALU.add), reads=[src, msk], writes=[src])

        def seg_setup(seg, t0):
            ctx.pool.op(lambda: nc.gpsimd.iota(pos[:, :], pattern=[[1, SEG]], base=t0, channel_multiplier=0, allow_small_or_imprecise_dtypes=True),
                        reads=[], writes=[pos])
            ctx.dve.op(lambda: nc.vector.tensor_scalar(out=fr[:, :], in0=pos[:, :], scalar1=ifr[:, 0:1], scalar2=None, op0=ALU.mult), reads=[pos, ifr], writes=[fr])
            ctx.dve.op(lambda: nc.vector.tensor_copy(out=fi[:, :], in_=fr[:, :]), reads=[fr], writes=[fi])
            ctx.dve.op(lambda: nc.vector.tensor_copy(out=ff[:, :], in_=fi[:, :]), reads=[fi], writes=[ff])
            ctx.dve.op(lambda: nc.vector.tensor_tensor(out=fr[:, :], in0=fr[:, :], in1=ff[:, :], op=ALU.subtract), reads=[fr, ff], writes=[fr])
            wrap(fr)
            ctx.act.op(lambda: nc.scalar.activation(out=tabs["sin"][:, :], in_=fr[:, :], func=AF.Sin, scale=2.0 * math.pi), reads=[fr], writes=[tabs["sin"], tb])
            ctx.dve.op(lambda: nc.vector.tensor_scalar(out=ff[:, :], in0=fr[:, :], scalar1=0.25, scalar2=None, op0=ALU.add), reads=[fr], writes=[ff])
            wrap(ff)
            ctx.act.op(lambda: nc.scalar.activation(out=tabs["cos"][:, :], in_=ff[:, :], func=AF.Sin, scale=2.0 * math.pi), reads=[ff], writes=[tabs["cos"], tb])
            ctx.dve.op(lambda: nc.vector.tensor_scalar(out=tabs["sink"][:, :], in0=tabs["sin"][:, :], scalar1=ks, scalar2=None, op0=ALU.mult), reads=[tabs["sin"]], writes=[tabs["sink"], tb])
            ctx.dve.op(lambda: nc.vector.tensor_scalar(out=tabs["cosk"][:, :], in0=tabs["cos"][:, :], scalar1=ks, scalar2=None, op0=ALU.mult), reads=[tabs["cos"]], writes=[tabs["cosk"], tb])

        def prologue(h, seg, t0, sl, ps_misc):
            qa, ka = sl["auxb"]
            ts = slice(t0, t0 + SEG)
            ctx.sp.dma([(qa[:, :], P.t[h * 128:(h + 1) * 128, ts])], qa, reads=[P], writes=[qa])
            ctx.sp.dma([(ka[:, :], P.t[1024 + h * 128:1024 + (h + 1) * 128, ts])], ka, reads=[P], writes=[ka])
            for vb in range(2):
                r0 = 2048 + h * 256 + vb * 128
                ctx.sp.dma([(sl["vT"][vb][:, :], P.t[r0:r0 + 128, ts])], sl["vT"][vb], reads=[P], writes=[sl["vT"][vb]])
            lg = math.log1p(-2.0 ** (-5.0 - h))
            ctx.pool.op(lambda: nc.gpsimd.memset(sl["gf"][:, :], lg), writes=[sl["gf"]])
            for src, dst, c_, s_ in ((qa, sl["qf"], "cos", "sin"), (ka, sl["kf"], "cosk", "sink")):
                a0 = sl["aux"][0]
                ctx.pe.op(lambda src=src: ctx.mm(ps_misc[:, :], lhsT=rot[:, :], rhs=src[:, :], start=True, stop=True), reads=[rot, src], writes=[ps_misc])
                ctx.dve.op(lambda: nc.vector.tensor_tensor(out=a0[:, :], in0=ps_misc[:, :], in1=tabs[s_][:, :], op=ALU.mult), reads=[ps_misc, tb], writes=[a0])
                ctx.pool.op(lambda: nc.gpsimd.tensor_tensor(out=dst[:, :], in0=src[:, :], in1=tabs[c_][:, :], op=ALU.mult), reads=[src, tb], writes=[dst])
                ctx.dve.op(lambda: nc.vector.tensor_tensor(out=dst[:, :], in0=dst[:, :], in1=a0[:, :], op=ALU.add), reads=[dst, a0], writes=[dst])

        gla_scan(ctx, consts, T, 8, 256, Y, seg_setup, prologue)


def ssd_scan(ctx, consts, T, XBCc, SDT, Y):
    nc = ctx.nc

    def seg_setup(seg, t0):
        pass

    def prologue(h, seg, t0, sl, ps_misc):
        g = h // 8
        ts = slice(t0, t0 + SEG)
        ba, ca = sl["auxb"]
        dtb = sl["aux"][0]
        ctx.sp.dma([(ba[:, :], XBCc.t[2048 + g * 128:2048 + (g + 1) * 128, ts])], ba, reads=[XBCc], writes=[ba])
        ctx.sp.dma([(ca[:, :], XBCc.t[2560 + g * 128:2560 + (g + 1) * 128, ts])], ca, reads=[XBCc], writes=[ca])
        ctx.sp.dma([(sl["vT"][0][:, :], XBCc.t[h * 64:(h + 1) * 64, ts])], sl["vT"][0], reads=[XBCc], writes=[sl["vT"][0]])
        ctx.sp.dma([(dtb[:, :], SDT.t[h, ts].partition_broadcast(128))], dtb, reads=[SDT], writes=[dtb])
        ctx.sp.dma([(sl["gf"][:, :], SDT.t[32 + h, ts].partition_broadcast(128))], sl["gf"], reads=[SDT], writes=[sl["gf"]])
        ctx.pool.op(lambda: nc.gpsimd.tensor_tensor(out=sl["kf"][:, :], in0=ba[:, :], in1=dtb[:, :], op=ALU.mult), reads=[ba, dtb], writes=[sl["kf"]])
        ctx.act.op(lambda: nc.scalar.activation(out=sl["qf"][:, :], in_=ca[:, :], func=AF.Copy), reads=[ca], writes=[sl["qf"]])

    gla_scan(ctx, consts, T, 32, 64, Y, seg_setup, prologue, scalar_decay=True)


def hgrn_scan(ctx, consts, T, P, FR, lbs, Y):
    nc = ctx.nc
    lb, oml, noml = lbs

    def seg_setup(seg, t0):
        pass

    def prologue(h, seg, t0, sl, ps_misc):
        ts = slice(t0, t0 + SEG)
        qa = sl["auxb"][0]
        fr, sg = sl["aux"]
        ctx.sp.dma([(qa[:, :], P.t[h * 128:(h + 1) * 128, ts])], qa, reads=[P], writes=[qa])
        ctx.sp.dma([(sl["vT"][0][:, :], P.t[2048 + h * 128:2048 + (h + 1) * 128, ts])], sl["vT"][0], reads=[P], writes=[sl["vT"][0]])
        ctx.sp.dma([(fr[:, :], FR.t[h * 128:(h + 1) * 128, ts])], fr, reads=[FR], writes=[fr])
        ctx.act.op(lambda: nc.scalar.activation(out=sl["qf"][:, :], in_=qa[:, :], func=AF.Copy), reads=[qa], writes=[sl["qf"]])
        ctx.act.op(lambda: nc.scalar.activation(out=sg[:, :], in_=fr[:, :], func=AF.Sigmoid), reads=[fr], writes=[sg])
        ctx.dve.op(lambda: nc.vector.tensor_scalar(out=fr[:, :], in0=sg[:, :], scalar1=oml[:, h:h + 1], scalar2=lb[:, h:h + 1], op0=ALU.mult, op1=ALU.add),
                   reads=[sg, oml, lb], writes=[fr])
        ctx.act.op(lambda: nc.scalar.activation(out=sl["gf"][:, :], in_=fr[:, :], func=AF.Ln), reads=[fr], writes=[sl["gf"]])
        ctx.dve.op(lambda: nc.vector.tensor_scalar(out=sl["kf"][:, :], in0=sg[:, :], scalar1=noml[:, h:h + 1], scalar2=oml[:, h:h + 1], op0=ALU.mult, op1=ALU.add),
                   reads=[sg, noml, oml], writes=[sl["kf"]])

    gla_scan(ctx, consts, T, 16, 128, Y, seg_setup, prologue)


def post_phase(ctx, consts, T, kind, Y, CAT, cat_row0, wvec, gate_src, gate_row0, xs_src=None, dskip=None):
    nc = ctx.nc
    NBH = 8
    gsz = {"ret": 2, "ssd": 4, "hgrn": 1}[kind]
    with ExitStack() as es:
        wk = norm_work(ctx, es, NBH)
        wv = ctx.sbuf(es, [128, 16], F32, "po_w")
        ctx.sp.dma([(wv[:, :], wvec.rearrange("(b p) -> p b", p=128))], wv, writes=[wv], allow_slow_non_contiguous=True)
        if dskip is not None:
            dk = ctx.sbuf(es, [128, 16], F32, "po_dk")
            ctx.sp.dma([(dk[:, :], dskip.rearrange("(b p) -> p b", p=128))], dk, writes=[dk], allow_slow_non_contiguous=True)
        ys = [ctx.sbuf(es, [128, NBH, 512], F32, "po_y") for _ in range(2)]
        gs = [ctx.sbuf(es, [128, NBH, 512], BF16, "po_g") for _ in range(2)]
        xs_ = [ctx.sbuf(es, [128, NBH, 512], BF16, "po_x") for _ in range(2)] if xs_src is not None else None
        os_ = [ctx.sbuf(es, [128, NBH, 512], BF16, "po_o") for _ in range(2)]
        it = 0
        for nt in range(T // 512):
            tsl = slice(nt * 512, (nt + 1) * 512)
            for half in range(2):
                b0 = half * NBH
                y, g, o = ys[it % 2], gs[it % 2], os_[it % 2]
                rs = slice(b0 * 128, (b0 + NBH) * 128)
                ctx.sp.dma([(y[:, :, :], Y.t[rs, tsl].rearrange("(b p) t -> p b t", p=128))], y, reads=[Y], writes=[y])
                grs = slice(gate_row0 + b0 * 128, gate_row0 + (b0 + NBH) * 128)
                ctx.sp.dma([(g[:, :, :], gate_src.t[grs, tsl].rearrange("(b p) t -> p b t", p=128))], g, reads=[gate_src], writes=[g])
                if kind == "ssd":
                    x = xs_[it % 2]
                    ctx.sp.dma([(x[:, :, :], xs_src.t[rs, tsl].rearrange("(b p) t -> p b t", p=128))], x, reads=[xs_src], writes=[x])
                    for b in range(NBH):
                        ctx.dve.op(lambda b=b: nc.vector.scalar_tensor_tensor(out=y[:, b, :], in0=x[:, b, :], scalar=dk[:, b0 + b:b0 + b + 1], in1=y[:, b, :],
                                                                             op0=ALU.mult, op1=ALU.add), reads=[x, dk, y], writes=[y])
                    ctx.pool.op(lambda: nc.gpsimd.tensor_tensor(out=y[:, :, :], in0=y[:, :, :], in1=g[:, :, :], op=ALU.mult), reads=[y, g], writes=[y])
                group_norm_tile(ctx, consts, wk, y, y, NBH, gsz, kind == "ret", gsz * 128)
                for b in range(NBH):
                    if kind == "ssd":
                        ctx.act.op(lambda b=b: nc.scalar.activation(out=o[:, b, :], in_=y[:, b, :], func=AF.Copy, scale=wv[:, b0 + b:b0 + b + 1]), reads=[y, wv], writes=[o])
                    else:
                        ctx.dve.op(lambda b=b: nc.vector.scalar_tensor_tensor(out=o[:, b, :], in0=y[:, b, :], scalar=wv[:, b0 + b:b0 + b + 1], in1=g[:, b, :],
                                                                             op0=ALU.mult, op1=ALU.mult), reads=[y, wv, g], writes=[o])
                ors = slice(cat_row0 + b0 * 128, cat_row0 + (b0 + NBH) * 128)
                ctx.sp.dma([(CAT.t[ors, tsl].rearrange("(b p) t -> p b t", p=128), o[:, :, :])], o, reads=[o], writes=[CAT])
                it += 1
        ctx.barrier()


def router_phase(ctx, consts, T, XT, w_router, GB):
    nc = ctx.nc
    with ExitStack() as es:
        wr = ctx.sbuf(es, [128, 16, NEXP], F32, "rt_w")
        ctx.sp.dma([(wr[:, :, :], w_router.rearrange("(kc p) e -> p kc e", p=128))], wr, writes=[wr])
        xs = [ctx.sbuf(es, [128, 16, 512], F32, "rt_x") for _ in range(2)]
        gall = ctx.sbuf(es, [NEXP, T], F32, "rt_gall")
        ps_l = ctx.psum(es, [128, NEXP], F32, "rt_psl")
        ps_t = ctx.psum(es, [NEXP, 128], F32, "rt_pst")
        lg = ctx.sbuf(es, [128, NEXP], F32, "rt_lg")
        m8 = ctx.sbuf(es, [128, 8], F32, "rt_m8")
        nm = ctx.sbuf(es, [128, 1], F32, "rt_nm")
        mk = ctx.sbuf(es, [128, NEXP], F32, "rt_mk")
        ex = ctx.sbuf(es, [128, NEXP], F32, "rt_ex")
        sm = ctx.sbuf(es, [128, 1], F32, "rt_sm")
        ident_f = consts["ident_f"]
        for nt in range(T // 512):
            x = xs[nt % 2]
            ctx.sp.dma([(x[:, :, :], XT.t[:, nt * 512:(nt + 1) * 512].rearrange("(kc p) t -> p kc t", p=128))], x, reads=[XT], writes=[x])
            for j in range(4):
                tk = slice(j * 128, (j + 1) * 128)

                def mm():
                    ins = None
                    for kc in range(16):
                        ins = ctx.mm(ps_l[:, :], lhsT=x[:, kc, tk], rhs=wr[:, kc, :], start=(kc == 0), stop=(kc == 15))
                    return ins
                ctx.pe.op(mm, reads=[x, wr], writes=[ps_l])
                ctx.dve.op(lambda: nc.vector.tensor_copy(out=lg[:, :], in_=ps_l[:, :]), reads=[ps_l], writes=[lg])
                ctx.dve.op(lambda: nc.vector.max(out=m8[:, :], in_=lg[:, :]), reads=[lg], writes=[m8])
                ctx.dve.op(lambda: nc.vector.tensor_scalar(out=mk[:, :], in0=lg[:, :], scalar1=m8[:, 1:2], scalar2=None, op0=ALU.is_ge), reads=[lg, m8], writes=[mk])
                ctx.dve.op(lambda: nc.vector.tensor_scalar(out=nm[:, :], in0=m8[:, 0:1], scalar1=-1.0, scalar2=None, op0=ALU.mult), reads=[m8], writes=[nm])
                ctx.act.op(lambda: nc.scalar.activation(out=ex[:, :], in_=lg[:, :], func=AF.Exp, bias=nm[:, 0:1]), reads=[lg, nm], writes=[ex])
                ctx.dve.op(lambda: nc.vector.tensor_tensor(out=ex[:, :], in0=ex[:, :], in1=mk[:, :], op=ALU.mult), reads=[ex, mk], writes=[ex])
                ctx.dve.op(lambda: nc.vector.tensor_reduce(out=sm[:, :], in_=ex[:, :], axis=AX.X, op=ALU.add), reads=[ex], writes=[sm])
                ctx.dve.op(lambda: nc.vector.reciprocal(out=sm[:, :], in_=sm[:, :]), reads=[sm], writes=[sm])
                ctx.dve.op(lambda: nc.vector.tensor_scalar(out=ex[:, :], in0=ex[:, :], scalar1=sm[:, 0:1], scalar2=None, op0=ALU.mult), reads=[ex, sm], writes=[ex])
                ctx.pe.op(lambda: ctx.tr(out=ps_t[:, :], in_=ex[:, :], identity=ident_f[:, :]), reads=[ex, ident_f], writes=[ps_t])
                c0 = nt * 512 + j * 128
                ctx.act.op(lambda: nc.scalar.activation(out=gall[:, c0:c0 + 128], in_=ps_t[:, :], func=AF.Copy), reads=[ps_t], writes=[gall])
        ctx.sp.dma([(GB.t[:, :], gall[:, :])], gall, reads=[gall], writes=[GB])
        ctx.barrier()


def moe_down(ctx, H, T, W2s, M):
    nc = ctx.nc
    TS = min(T, 1024)
    NT = TS // 512
    KH = 22
    WCB = 256
    with ExitStack() as es:
        acc = ctx.sbuf(es, [128, 16, TS], F32, "md_acc")
        a_sb = [ctx.sbuf(es, [128, KH, TS], BF16, "md_a") for _ in range(2)]
        w_sb = [ctx.sbuf(es, [128, KH, WCB], BF16, "md_w") for _ in range(2)]
        pss = [ctx.psum(es, [128, 512], F32, "md_ps") for _ in range(3)]
        wi = 0
        pi = 0
        ai = 0
        for st in range(T // TS):
            t0 = st * TS
            first = True
            for e, W2 in enumerate(W2s):
                for half in range(2):
                    r0 = half * KH * 128
                    a = a_sb[ai % 2]
                    ai += 1
                    A_v = H[e].t[r0:r0 + KH * 128, t0:t0 + TS].rearrange("(kc p) t -> p kc t", p=128)
                    ctx.sp.dma([(a[:, k0:k0 + 11, :], A_v[:, k0:k0 + 11, :]) for k0 in range(0, KH, 11)], a, reads=[H[e]], writes=[a])
                    for c0 in range(0, D, WCB):
                        wb = w_sb[wi % 2]
                        wi += 1
                        W_v = W2[r0:r0 + KH * 128, c0:c0 + WCB].rearrange("(kc p) c -> p kc c", p=128)
                        ctx.pool.dma([(wb[:, k0:k0 + 11, :], W_v[:, k0:k0 + 11, :]) for k0 in range(0, KH, 11)], wb, writes=[wb])
                        for m0 in range(0, WCB, 128):
                            cb = (c0 + m0) // 128
                            for nt in range(NT):
                                ps = pss[pi % 3]
                                pi += 1
                                ns = slice(nt * 512, (nt + 1) * 512)

                                def mm(ps=ps, wb=wb, a=a, m0=m0, ns=ns):
                                    ins = None
                                    for kc in range(KH):
                                        ins = ctx.mm(ps[:, :], lhsT=wb[:, kc, m0:m0 + 128], rhs=a[:, kc, ns], start=(kc == 0), stop=(kc == KH - 1))
                                    return ins
                                ctx.pe.op(mm, reads=[wb, a], writes=[ps])
                                if first:
                                    ctx.act.op(lambda ps=ps, cb=cb, ns=ns: nc.scalar.activation(out=acc[:, cb, ns], in_=ps[:, :], func=AF.Copy), reads=[ps], writes=[acc])
                                else:
                                    ctx.dve.op(lambda ps=ps, cb=cb, ns=ns: nc.vector.tensor_tensor(out=acc[:, cb, ns], in0=acc[:, cb, ns], in1=ps[:, :], op=ALU.add),
                                               reads=[ps, acc], writes=[acc])
                    first = False
            ctx.sp.dma([(M.t[:, t0:t0 + TS].rearrange("(b p) t -> p b t", p=128), acc[:, :, :])], acc, reads=[acc], writes=[M])
        ctx.barrier()


def in_proj(ctx, XB, T, W, C, colmap):
    TS = min(T, 2048)
    with ExitStack() as es:
        stagers = {"b": Stager(ctx, es, TS, BF16, 3, "ip_b"), "f": Stager(ctx, es, TS, F32, 2, "ip_f")}
        et, eb = make_plain_epi(ctx, colmap, stagers)
        gemm(ctx, XB, D, T, [W], C, et, eb, TS, 512)


def out_proj(ctx, CAT, K, T, W, M):
    TS = min(T, 1024)
    with ExitStack() as es:
        stg = Stager(ctx, es, TS, F32, 3, "op_stg")
        et, eb = make_plain_epi(ctx, lambda cb0: (M, cb0, None, "m"), {"m": stg})
        gemm(ctx, CAT, K, T, [W], D, et, eb, TS, 256)


def even_layer(ctx, consts, T, S, w, j, last_out=None):
    P, DT, XBCc, SDT, Y, CAT, M, H, XT, XB = (S[k] for k in ("P", "DT", "XBCc", "SDT", "Y", "CAT", "M", "H", "XT", "XB"))

    def colmap(cb0):
        if cb0 < 4096:
            return (P, cb0, None, "b")
        if cb0 < 8192:
            return (P, cb0, AF.Silu, "b")
        if cb0 < 11264:
            return (P, cb0, None, "b")
        return (DT, 0, None, "f")
    in_proj(ctx, XB, T, w["w_in"], EV_IN, colmap)
    ret_scan(ctx, consts, T, P, Y)
    post_phase(ctx, consts, T, "ret", Y, CAT, 0, w["ret_norm_w"], P, 4096)
    XBC = Buf(ctx, P.t[8192:11264, :], "XBC")
    XBC.w, XBC.r = P.w, P.r
    conv_phase(ctx, T, XBC, XBCc, w["conv_w"], w["conv_b"])
    dt_phase(ctx, T, DT, SDT, w["dt_bias"], w["a_log"])
    ssd_scan(ctx, consts, T, XBCc, SDT, Y)
    post_phase(ctx, consts, T, "ssd", Y, CAT, 2048, w["ssd_norm_w"], P, 6144, xs_src=XBCc, dskip=w["d_skip_ch"])
    out_proj(ctx, CAT, 4096, T, w["w_out"], M)
    ln_phase(ctx, consts, XT, M, XB, w["ln1_g"], w["ln1_b"], T)
    swiglu_up(ctx, XB, T, [w["ffn_w1"]], [w["ffn_w3"]], H)
    ffn_down(ctx, H[0], T, w["ffn_w2"], M)
    ln_phase(ctx, consts, XT, M, XB, w["ln2_g"], w["ln2_b"], T, out_final=last_out)


def odd_layer(ctx, consts, T, S, w, j, lbs, last_out=None):
    P, FR, Y, CAT, M, H, XT, XB, GB = (S[k] for k in ("P", "FR", "Y", "CAT", "M", "H", "XT", "XB", "GB"))

    def colmap(cb0):
        if cb0 < 2048:
            return (P, cb0, AF.Silu, "b")
        if cb0 < 4096:
            return (FR, cb0 - 2048, None, "f")
        if cb0 < 6144:
            return (P, cb0 - 2048, None, "b")
        return (P, cb0 - 2048, AF.Silu, "b")
    in_proj(ctx, XB, T, w["w_in"], 8192, colmap)
    hgrn_scan(ctx, consts, T, P, FR, lbs, Y)
    post_phase(ctx, consts, T, "hgrn", Y, CAT, 0, w["hg_norm_w"], P, 4096)
    out_proj(ctx, CAT, 2048, T, w["w_out"], M)
    ln_phase(ctx, consts, XT, M, XB, w["ln1_g"], w["ln1_b"], T)
    router_phase(ctx, consts, T, XT, w["router"], GB)
    swiglu_up(ctx, XB, T, [w["moe_w1"][e] for e in range(NEXP)], [w["moe_w3"][e] for e in range(NEXP)], H, GB=GB)
    moe_down(ctx, H, T, [w["moe_w2"][e] for e in range(NEXP)], M)
    ln_phase(ctx, consts, XT, M, XB, w["ln2_g"], w["ln2_b"], T, out_final=last_out)


WSPEC = [
    ("ev_w_in", [2, D, EV_IN]), ("ev_ret_norm_w", [2, D]), ("ev_conv_w", [2, 4, 3072]), ("ev_conv_b", [2, 3072]),
    ("ev_dt_bias", [2, 32]), ("ev_a_log", [2, 32]), ("ev_d_skip_ch", [2, D]), ("ev_ssd_norm_w", [2, D]),
    ("ev_w_out", [2, 4096, D]), ("ev_ln1_g", [2, D]), ("ev_ln1_b", [2, D]),
    ("ffn_w1", [2, D, FFN]), ("ffn_w3", [2, D, FFN]), ("ffn_w2", [2, FFN, D]), ("ev_ln2_g", [2, D]), ("ev_ln2_b", [2, D]),
    ("od_w_in", [2, D, 8192]), ("hg_lb_logits", [DEPTH, D]), ("od_hg_norm_w", [2, D]), ("od_w_out", [2, D, D]),
    ("od_ln1_g", [2, D]), ("od_ln1_b", [2, D]), ("moe_router", [2, D, NEXP]),
    ("moe_w1", [2, NEXP, D, FFN]), ("moe_w3", [2, NEXP, D, FFN]), ("moe_w2", [2, NEXP, FFN, D]),
    ("od_ln2_g", [2, D]), ("od_ln2_b", [2, D]),
]


def build_program(T, layers=(0, 1, 2, 3), wfilter=None, nl=2):
    nc = bass.Bass("TRN2", target_bir_lowering=False)
    with ExitStack() as es:
        ctx = Ctx(nc, es)
        xin = ctx.dram("xin", [D, T], F32, kind="ExternalInput")
        out = ctx.dram("out", [D, T], F32, kind="ExternalOutput")
        W = {name: nc.dram_tensor(name, ([nl] + shape[1:]) if name != "hg_lb_logits" else shape, F32, kind="ExternalInput").ap()
             for name, shape in WSPEC if (wfilter is None or wfilter(name))}
        S = {
            "XT": ctx.dram("XT", [D, T], F32), "XB": ctx.dram("XB", [D, T], BF16),
            "P": ctx.dram("P", [11264, T], BF16), "DT": ctx.dram("DT", [32, T], F32), "FR": ctx.dram("FR", [D, T], F32),
            "XBCc": ctx.dram("XBCc", [3072, T], BF16), "SDT": ctx.dram("SDT", [64, T], F32),
            "Y": ctx.dram("Y", [D, T], F32), "CAT": ctx.dram("CAT", [4096, T], BF16), "M": ctx.dram("M", [D, T], F32),
            "H": [ctx.dram(f"H{e}", [FFN, T], BF16) for e in range(NEXP)], "GB": ctx.dram("GB", [NEXP, T], F32),
        }
        consts = make_consts(ctx, es)
        lbs = lb_setup(ctx, es, W["hg_lb_logits"])
        ctx.sp.dma([(S["XT"].t[:, :], xin.t[:, :])], S["XT"], reads=[xin], writes=[S["XT"]])
        cast_dram(ctx, S["XB"], xin)
        for li, layer in enumerate(layers):
            j = layer // 2
            last = out if li == len(layers) - 1 else None
            if layer % 2 == 0:
                w = {"w_in": W["ev_w_in"][j], "ret_norm_w": W["ev_ret_norm_w"][j], "conv_w": W["ev_conv_w"][j], "conv_b": W["ev_conv_b"][j],
                     "dt_bias": W["ev_dt_bias"][j], "a_log": W["ev_a_log"][j], "d_skip_ch": W["ev_d_skip_ch"][j], "ssd_norm_w": W["ev_ssd_norm_w"][j],
                     "w_out": W["ev_w_out"][j], "ln1_g": W["ev_ln1_g"][j], "ln1_b": W["ev_ln1_b"][j], "ffn_w1": W["ffn_w1"][j], "ffn_w3": W["ffn_w3"][j],
                     "ffn_w2": W["ffn_w2"][j], "ln2_g": W["ev_ln2_g"][j], "ln2_b": W["ev_ln2_b"][j]}
                even_layer(ctx, consts, T, S, w, j, last_out=last)
            else:
                w = {"w_in": W["od_w_in"][j], "hg_norm_w": W["od_hg_norm_w"][j], "w_out": W["od_w_out"][j], "ln1_g": W["od_ln1_g"][j], "ln1_b": W["od_ln1_b"][j],
                     "router": W["moe_router"][j], "moe_w1": W["moe_w1"][j], "moe_w3": W["moe_w3"][j], "moe_w2": W["moe_w2"][j],
                     "ln2_g": W["od_ln2_g"][j], "ln2_b": W["od_ln2_b"][j]}
                odd_layer(ctx, consts, T, S, w, j, lbs[layer], last_out=last)
        finish(ctx, [out])
    return nc


def host_inputs(inputs, xT):
    m = {"xin": np.ascontiguousarray(xT, dtype=np.float32)}
    for name, shape in WSPEC:
        if name == "ev_d_skip_ch":
            m[name] = np.ascontiguousarray(np.repeat(np.asarray(inputs["ev_d_skip"], dtype=np.float32), 64, axis=1))
        else:
            m[name] = np.ascontiguousarray(np.asarray(inputs[name], dtype=np.float32))
    return m


N_ACTIVE = 4


def kernel(**inputs):
    x = np.asarray(inputs["x"], dtype=np.float32)
    B, T, _ = x.shape
    nc = build_program(T)
    in_maps = []
    for c in range(N_ACTIVE):
        in_maps.append(host_inputs(inputs, x[c % B].T))
    res = run_bass_kernel_spmd(nc, in_maps, core_ids=list(range(N_ACTIVE)))
    outs = [np.ascontiguousarray(res.results[c]["out"].T) for c in range(B)]
    return np.stack(outs, axis=0).astype(np.float32)
```

```python
import math
from contextlib import ExitStack

import numpy as np
import concourse.bass as bass
import concourse.mybir as mybir
from concourse.alu_op_type import AluOpType as ALU
from concourse.bass_utils import run_bass_kernel_spmd

F32 = mybir.dt.float32
BF16 = mybir.dt.bfloat16
I32 = mybir.dt.int32
AF = mybir.ActivationFunctionType
AX = mybir.AxisListType

D = 2048
DEPTH = 4
LN_EPS = 1e-5
ALPHA = (2.0 * DEPTH) ** 0.25
FFN = 5632
NEXP = 8
EV_IN = 11296
SEM_LIMIT = 2000000


class Sem:
    def __init__(self, ctx, name):
        self.h = ctx.es.enter_context(ctx.nc.semaphore(name))
        self.v = 0
        ctx.sems.append(self)


class Buf:
    def __init__(self, ctx, t, name):
        self.ctx = ctx
        self.t = t
        self.name = name
        self.w = {}
        self.r = {}
        self.dsem = None

    def __getitem__(self, idx):
        return self.t[idx]

    def dma_sem(self):
        if self.dsem is None or self.dsem.v >= SEM_LIMIT:
            pool = self.ctx.sem_pool
            self.dsem = pool.pop() if pool else Sem(self.ctx, None)
            self.ctx.dma_bufs.append(self)
        return self.dsem


class Q:
    def __init__(self, ctx, name, eng, self_sync=True):
        self.ctx = ctx
        self.name = name
        self.eng = eng
        self.sem = None
        self.seen = {}
        self.self_sync = self_sync

    def _need(self, sem, val, pend):
        if not self.self_sync and sem is self.sem:
            return
        if self.seen.get(sem, 0) >= val or pend.get(sem, 0) >= val:
            return
        pend[sem] = val

    def _wait(self, sem, val):
        pend = {}
        self._need(sem, val, pend)
        self._emit(pend, None)

    def _emit(self, pend, keep_last):
        items = list(pend.items())
        ret = None
        if keep_last and items:
            ret = items.pop()
        for s, v in items:
            self.eng.wait_ge(s.h, v)
            self.seen[s] = v
        if ret is not None:
            self.seen[ret[0]] = ret[1]
        return ret

    def deps(self, reads, writes, keep_last=False):
        pend = {}
        for b in reads:
            for s, v in b.w.items():
                self._need(s, v, pend)
        for b in writes:
            for s, v in b.w.items():
                self._need(s, v, pend)
            for s, v in b.r.items():
                self._need(s, v, pend)
        return self._emit(pend, keep_last)

    def mark(self, tok, reads, writes):
        s, v = tok
        for b in reads:
            if b.r.get(s, 0) < v:
                b.r[s] = v
        for b in writes:
            if b.w.get(s, 0) < v:
                b.w[s] = v

    def op(self, ins_fn, reads=(), writes=(), single=None):
        if single is None:
            single = self.self_sync
        w = self.deps(reads, writes, keep_last=single)
        self.ctx._first = None
        ins = ins_fn()
        if w is not None:
            ins._wait_ge(w[0].h, w[1])
        if self.sem is None or self.sem.v >= SEM_LIMIT:
            self.sem = Sem(self.ctx, None)
        self.sem.v += 1
        ins.then_inc(self.sem.h, 1)
        self.mark((self.sem, self.sem.v), reads, writes)

    def dma(self, pairs, sb, reads=(), writes=(), **kw):
        w = self.deps(reads, writes, keep_last=True)
        s = sb.dma_sem()
        for out_ap, in_ap in pairs:
            s.v += 16
            ins = self.eng.dma_start(out=out_ap, in_=in_ap, **kw)
            if w is not None:
                ins._wait_ge(w[0].h, w[1])
                w = None
            ins.then_inc(s.h, 16)
        self.mark((s, s.v), reads, writes)


class Ctx:
    def __init__(self, nc, es):
        self.nc = nc
        self.es = es
        self.sems = []
        self.sem_pool = []
        self.dma_bufs = []
        self._first = None
        self.pe = Q(self, "pe", nc.tensor, self_sync=False)
        self.dve = Q(self, "dve", nc.vector)
        self.act = Q(self, "act", nc.scalar)
        self.pool = Q(self, "pool", nc.gpsimd)
        self.sp = Q(self, "sp", nc.sync)
        self.qs = [self.pe, self.dve, self.act, self.pool, self.sp]
        self.uid = 0

    def mm(self, *a, **k):
        ins = self.nc.tensor.matmul(*a, **k)
        if self._first is None:
            self._first = ins
        return ins

    def tr(self, *a, **k):
        ins = self.nc.tensor.transpose(*a, **k)
        if self._first is None:
            self._first = ins
        return ins

    def name(self, base):
        self.uid += 1
        return f"{base}_{self.uid}"

    def sbuf(self, es, shape, dt, name="sb"):
        t = es.enter_context(self.nc.sbuf_tensor(self.name(name), list(shape), dt))
        return Buf(self, t, name)

    def psum(self, es, shape, dt, name="ps"):
        t = es.enter_context(self.nc.psum_tensor(self.name(name), list(shape), dt))
        return Buf(self, t, name)

    def dram(self, name, shape, dt, kind="Internal"):
        t = self.nc.dram_tensor(name, list(shape), dt, kind=kind)
        return Buf(self, t.ap(), name)

    def barrier(self, qs=None):
        for q in (qs or self.qs):
            for s in self.sems:
                if s.v > 0:
                    q._wait(s, s.v)
        for b in self.dma_bufs:
            if b.dsem is not None and b.dsem.v < SEM_LIMIT:
                self.sem_pool.append(b.dsem)
            b.dsem = None
        self.dma_bufs = []


def gemm(ctx, A, K, T, Ws, C, epi_tile, epi_block, TS, WCB, n_par=1, acc=None, ps_bufs=None,
         a_row0=0, w_row0=0, keep_a=None):
    nc = ctx.nc
    KC = K // 128
    NT = TS // 512
    nW = len(Ws)
    with ExitStack() as es:
        a_sb = keep_a if keep_a is not None else ctx.sbuf(es, [128, KC, TS], BF16, "a_sb")
        w_sb = [[ctx.sbuf(es, [128, KC, WCB], BF16, "w_sb") for _ in range(2)] for _ in range(nW)]
        if ps_bufs is None:
            ps_bufs = [[ctx.psum(es, [128, 512], F32, "ps") for _ in range(2)] for _ in range(nW)]
        pi = 0
        wi = 0
        for st in range(T // TS):
            t0 = st * TS
            A_v = A.t[a_row0:a_row0 + K, t0:t0 + TS].rearrange("(kc p) t -> p kc t", p=128)
            g = 4 if KC % 4 == 0 else (2 if KC % 2 == 0 else 1)
            ctx.sp.dma([(a_sb[:, k0:k0 + g, :], A_v[:, k0:k0 + g, :]) for k0 in range(0, KC, g)],
                       a_sb, reads=[A], writes=[a_sb])
            for c0 in range(0, C, WCB):
                cw = min(WCB, C - c0)
                slot = wi % 2
                wi += 1
                for j, W in enumerate(Ws):
                    W_v = W[w_row0:w_row0 + K, c0:c0 + cw].rearrange("(kc p) c -> p kc c", p=128)
                    wb = w_sb[j][slot]
                    h = KC // 2 if KC % 2 == 0 else KC
                    ctx.pool.dma([(wb[:, k0:k0 + h, 0:cw], W_v[:, k0:k0 + h, :]) for k0 in range(0, KC, h)],
                                 wb, reads=[], writes=[wb])
                for m0 in range(0, cw, 128):
                    rows = min(128, cw - m0)
                    cb0 = c0 + m0
                    for nt in range(NT):
                        pss = []
                        for j in range(nW):
                            ps = ps_bufs[j][pi % 2]
                            wb = w_sb[j][slot]

                            def mm(ps=ps, wb=wb):
                                ins = None
                                for kc in range(KC):
                                    ins = ctx.mm(ps[0:rows, :], lhsT=wb[:, kc, m0:m0 + rows],
                                                           rhs=a_sb[:, kc, nt * 512:(nt + 1) * 512],
                                                           start=(kc == 0), stop=(kc == KC - 1))
                                return ins
                            ctx.pe.op(mm, reads=[wb, a_sb], writes=[ps])
                            pss.append(ps)
                        pi += 1
                        epi_tile(pss, cb0, rows, st, nt, t0)
                    if epi_block is not None:
                        epi_block(cb0, rows, st, t0)
        ctx.barrier()


class Stager:
    def __init__(self, ctx, es, TS, dt, n=3, name="stg"):
        self.ctx = ctx
        self.bufs = [ctx.sbuf(es, [128, TS], dt, name) for _ in range(n)]
        self.i = 0

    def cur(self):
        return self.bufs[self.i % len(self.bufs)]

    def next(self):
        self.i += 1


def make_plain_epi(ctx, colmap, stagers, flip=[0]):
    def epi_tile(pss, cb0, rows, st, nt, t0):
        dst, r0, func, key = colmap(cb0)
        sb = stagers[key].cur()
        ps = pss[0]
        sl = slice(nt * 512, (nt + 1) * 512)
        if func is None and flip[0] % 2 == 0:
            ctx.dve.op(lambda: ctx.nc.vector.tensor_copy(out=sb[0:rows, sl], in_=ps[0:rows, :]), reads=[ps], writes=[sb])
        else:
            f = AF.Copy if func is None else func
            ctx.act.op(lambda: ctx.nc.scalar.activation(out=sb[0:rows, sl], in_=ps[0:rows, :], func=f), reads=[ps], writes=[sb])
        flip[0] += 1

    def epi_block(cb0, rows, st, t0):
        dst, r0, func, key = colmap(cb0)
        stg = stagers[key]
        sb = stg.cur()
        TS = sb.t.shape[1]
        ctx.sp.dma([(dst.t[r0:r0 + rows, t0:t0 + TS], sb[0:rows, :])], sb, reads=[sb], writes=[dst])
        stg.next()

    return epi_tile, epi_block


def make_consts(ctx, es):
    nc = ctx.nc
    c = {}
    ones_f = ctx.sbuf(es, [128, 128], F32, "ones_f")
    ctx.pool.op(lambda: nc.gpsimd.memset(ones_f[:], 1.0), writes=[ones_f])
    ones_b = ctx.sbuf(es, [128, 128], BF16, "ones_b")
    ctx.pool.op(lambda: nc.gpsimd.memset(ones_b[:], 1.0), writes=[ones_b])
    ident_f = ctx.sbuf(es, [128, 128], F32, "ident_f")
    ctx.pool.op(lambda: nc.gpsimd.affine_select(out=ident_f[:], in_=ones_f[:], pattern=[[-1, 128]],
                                                compare_op=ALU.is_equal, fill=0.0, base=0, channel_multiplier=1),
                reads=[ones_f], writes=[ident_f])
    ident_b = ctx.sbuf(es, [128, 128], BF16, "ident_b")
    ctx.dve.op(lambda: nc.vector.tensor_copy(out=ident_b[:], in_=ident_f[:]), reads=[ident_f], writes=[ident_b])
    c["ones_f"], c["ones_b"], c["ident_f"], c["ident_b"] = ones_f, ones_b, ident_f, ident_b
    return c


def group_norm_tile(ctx, consts, wk, r, out, NB, gsz, center, nfeat, W=512):
    nc = ctx.nc
    rb, rsq, ps1, ps2, mean, rstd, tmp = wk["rb"], wk["rsq"], wk["ps1"], wk["ps2"], wk["mean"], wk["rstd"], wk["tmp"]
    ones_b = consts["ones_b"]
    inv = 1.0 / nfeat
    if center:
        ctx.act.op(lambda: nc.scalar.activation(out=rb[:, 0:NB, 0:W], in_=r[:, 0:NB, 0:W], func=AF.Copy), reads=[r], writes=[rb])
    ctx.pool.op(lambda: nc.gpsimd.tensor_tensor(out=rsq[:, 0:NB, 0:W], in0=r[:, 0:NB, 0:W], in1=r[:, 0:NB, 0:W], op=ALU.mult),
                reads=[r], writes=[rsq])
    for g0 in range(0, NB, gsz):
        if center:
            def mm1():
                ins = None
                for b in range(g0, g0 + gsz):
                    ins = ctx.mm(ps1[:, 0:W], lhsT=ones_b[:], rhs=rb[:, b, 0:W], start=(b == g0), stop=(b == g0 + gsz - 1))
                return ins
            ctx.pe.op(mm1, reads=[ones_b, rb], writes=[ps1])

        def mm2():
            ins = None
            for b in range(g0, g0 + gsz):
                ins = ctx.mm(ps2[:, 0:W], lhsT=ones_b[:], rhs=rsq[:, b, 0:W], start=(b == g0), stop=(b == g0 + gsz - 1))
            return ins
        ctx.pe.op(mm2, reads=[ones_b, rsq], writes=[ps2])
        if center:
            ctx.act.op(lambda: nc.scalar.activation(out=mean[:, 0:W], in_=ps1[:, 0:W], func=AF.Copy, scale=inv), reads=[ps1], writes=[mean])
            ctx.dve.op(lambda: nc.vector.tensor_tensor(out=tmp[:, 0:W], in0=mean[:, 0:W], in1=mean[:, 0:W], op=ALU.mult), reads=[mean], writes=[tmp])
            ctx.dve.op(lambda: nc.vector.scalar_tensor_tensor(out=tmp[:, 0:W], in0=ps2[:, 0:W], scalar=inv, in1=tmp[:, 0:W],
                                                              op0=ALU.mult, op1=ALU.subtract), reads=[ps2, tmp], writes=[tmp])
            ctx.dve.op(lambda: nc.vector.tensor_scalar(out=tmp[:, 0:W], in0=tmp[:, 0:W], scalar1=0.0, scalar2=LN_EPS, op0=ALU.max, op1=ALU.add),
                       reads=[tmp], writes=[tmp])
        else:
            ctx.dve.op(lambda: nc.vector.tensor_scalar(out=tmp[:, 0:W], in0=ps2[:, 0:W], scalar1=inv, scalar2=LN_EPS, op0=ALU.mult, op1=ALU.add),
                       reads=[ps2], writes=[tmp])
        ctx.act.op(lambda: nc.scalar.activation(out=tmp[:, 0:W], in_=tmp[:, 0:W], func=AF.Sqrt), reads=[tmp], writes=[tmp])
        ctx.dve.op(lambda: nc.vector.reciprocal(out=rstd[:, 0:W], in_=tmp[:, 0:W]), reads=[tmp], writes=[rstd])
        for b in range(g0, g0 + gsz):
            q = ctx.dve if b % 2 == 0 else ctx.pool
            e = nc.vector if b % 2 == 0 else nc.gpsimd
            if center:
                q.op(lambda e=e, b=b: e.tensor_tensor(out=out[:, b, 0:W], in0=r[:, b, 0:W], in1=mean[:, 0:W], op=ALU.subtract), reads=[r, mean], writes=[out])
                q.op(lambda e=e, b=b: e.tensor_tensor(out=out[:, b, 0:W], in0=out[:, b, 0:W], in1=rstd[:, 0:W], op=ALU.mult), reads=[out, rstd], writes=[out])
            else:
                q.op(lambda e=e, b=b: e.tensor_tensor(out=out[:, b, 0:W], in0=r[:, b, 0:W], in1=rstd[:, 0:W], op=ALU.mult), reads=[r, rstd], writes=[out])


def norm_work(ctx, es, NB, W=512):
    return {
        "rb": ctx.sbuf(es, [128, NB, W], BF16, "n_rb"),
        "rsq": ctx.sbuf(es, [128, NB, W], BF16, "n_rsq"),
        "ps1": ctx.psum(es, [128, 512], F32, "n_ps1"),
        "ps2": ctx.psum(es, [128, 512], F32, "n_ps2"),
        "mean": ctx.sbuf(es, [128, W], F32, "n_mean"),
        "rstd": ctx.sbuf(es, [128, W], F32, "n_rstd"),
        "tmp": ctx.sbuf(es, [128, W], F32, "n_tmp"),
    }


def ln_phase(ctx, consts, XT, M, XB, gvec, bvec, T, out_final=None):
    nc = ctx.nc
    NB = D // 128
    with ExitStack() as es:
        wk = norm_work(ctx, es, NB)
        gb = ctx.sbuf(es, [128, 2, NB], F32, "ln_gb")
        ctx.sp.dma([(gb[:, 0, :], gvec.rearrange("(b p) -> p b", p=128)), (gb[:, 1, :], bvec.rearrange("(b p) -> p b", p=128))],
                   gb, writes=[gb], allow_slow_non_contiguous=True)
        xs = [ctx.sbuf(es, [128, NB, 512], F32, "ln_x") for _ in range(2)]
        ms = [ctx.sbuf(es, [128, NB, 512], F32, "ln_m") for _ in range(2)]
        ob = [ctx.sbuf(es, [128, NB, 512], BF16, "ln_ob") for _ in range(2)]
        for nt in range(T // 512):
            x, m, o = xs[nt % 2], ms[nt % 2], ob[nt % 2]
            tsl = slice(nt * 512, (nt + 1) * 512)
            ctx.sp.dma([(x[:, :, :], XT.t[:, tsl].rearrange("(b p) t -> p b t", p=128))], x, reads=[XT], writes=[x])
            ctx.sp.dma([(m[:, :, :], M.t[:, tsl].rearrange("(b p) t -> p b t", p=128))], m, reads=[M], writes=[m])
            ctx.dve.op(lambda: nc.vector.scalar_tensor_tensor(out=m[:, :, :], in0=x[:, :, :], scalar=ALPHA, in1=m[:, :, :], op0=ALU.mult, op1=ALU.add),
                       reads=[x, m], writes=[m])
            group_norm_tile(ctx, consts, wk, m, x, NB, NB, True, D)
            for b in range(NB):
                q = ctx.dve if b % 2 == 0 else ctx.pool
                e = nc.vector if b % 2 == 0 else nc.gpsimd
                q.op(lambda e=e, b=b: e.tensor_scalar(out=x[:, b, :], in0=x[:, b, :], scalar1=gb[:, 0, b:b + 1], scalar2=gb[:, 1, b:b + 1],
                                                     op0=ALU.mult, op1=ALU.add), reads=[x, gb], writes=[x])
            if out_final is not None:
                ctx.sp.dma([(out_final.t[:, tsl].rearrange("(b p) t -> p b t", p=128), x[:, :, :])], x, reads=[x], writes=[out_final])
            else:
                ctx.act.op(lambda: nc.scalar.activation(out=o[:, :, :], in_=x[:, :, :], func=AF.Copy), reads=[x], writes=[o])
                ctx.sp.dma([(XT.t[:, tsl].rearrange("(b p) t -> p b t", p=128), x[:, :, :])], x, reads=[x], writes=[XT])
                ctx.sp.dma([(XB.t[:, tsl].rearrange("(b p) t -> p b t", p=128), o[:, :, :])], o, reads=[o], writes=[XB])
        ctx.barrier()


def swiglu_up(ctx, XB, T, W1s, W3s, H, GB=None):
    nc = ctx.nc
    TS = min(T, 2048)
    with ExitStack() as es:
        stg = Stager(ctx, es, TS, BF16, 3, "h_stg")
        tmps = [ctx.sbuf(es, [128, 512], F32, "sw_tmp") for _ in range(3)]
        gts = [ctx.sbuf(es, [128, TS], F32, "gate_t") for _ in range(2)] if GB is not None else None
        ti = [0]
        for e, (W1, W3) in enumerate(zip(W1s, W3s)):
            cur_g = [None]

            def epi_tile(pss, cb0, rows, st, nt, t0, e=e):
                tmp = tmps[ti[0] % 3]
                ti[0] += 1
                sb = stg.cur()
                sl = slice(nt * 512, (nt + 1) * 512)
                ctx.act.op(lambda: nc.scalar.activation(out=tmp[:, :], in_=pss[0][:, :], func=AF.Silu), reads=[pss[0]], writes=[tmp])
                if GB is not None:
                    if nt == 0 and cb0 == 0:
                        g = gts[(e + st) % 2]
                        ctx.sp.dma([(g[:, :], GB.t[e, t0:t0 + TS].partition_broadcast(128))], g, reads=[GB], writes=[g])
                        cur_g[0] = g
                    g = cur_g[0]
                    ctx.dve.op(lambda: nc.vector.tensor_tensor(out=tmp[:, :], in0=tmp[:, :], in1=pss[1][:, :], op=ALU.mult), reads=[tmp, pss[1]], writes=[tmp])
                    ctx.dve.op(lambda: nc.vector.tensor_tensor(out=sb[:, sl], in0=tmp[:, :], in1=g[:, sl], op=ALU.mult), reads=[tmp, g], writes=[sb])
                else:
                    ctx.dve.op(lambda: nc.vector.tensor_tensor(out=sb[:, sl], in0=tmp[:, :], in1=pss[1][:, :], op=ALU.mult), reads=[tmp, pss[1]], writes=[sb])

            def epi_block(cb0, rows, st, t0, e=e):
                sb = stg.cur()
                ctx.sp.dma([(H[e].t[cb0:cb0 + rows, t0:t0 + TS], sb[0:rows, :])], sb, reads=[sb], writes=[H[e]])
                stg.next()

            gemm(ctx, XB, D, T, [W1, W3], FFN, epi_tile, epi_block, TS, 256)


def cast_dram(ctx, dst, src):
    ctx.pool.dma([(dst.t[:, :], src.t[:, :])], dst, reads=[src], writes=[dst])


def ffn_down(ctx, H, T, W2, M, K=FFN):
    with ExitStack() as es:
        stg = Stager(ctx, es, 512, F32, 4, "m_stg")
        et, eb = make_plain_epi(ctx, lambda cb0: (M, cb0, None, "m"), {"m": stg})
        gemm(ctx, H, K, T, [W2], D, et, eb, 512, 256)


def finish(ctx, bufs):
    ctx.barrier()


def all_gather(ctx, dst, src, n_cores):
    q = ctx.pool
    q.deps([src], [dst])
    if getattr(ctx, "cc_sem", None) is None or ctx.cc_sem.v >= SEM_LIMIT:
        ctx.cc_sem = Sem(ctx, None)
    s = ctx.cc_sem
    s.v += 1
    ctx.nc.gpsimd.collective_compute("AllGather", ALU.bypass, replica_groups=[list(range(n_cores))],
                                     ins=[src.t.opt()], outs=[dst.t.opt()]).then_inc(s.h)
    q.mark((s, s.v), [src], [dst])


SEG = 512
CH = 32
NCH = SEG // CH


def gla_scan(ctx, consts, T, nheads, dv, Y, seg_setup, prologue, nslots=3, scalar_decay=False, CH=32, G=2):
    nc = ctx.nc
    NCH = SEG // CH
    mid = CH // 2 - 1
    nvb = (dv + 127) // 128
    dvb = min(dv, 128)
    with ExitStack() as es:
        m0 = ctx.sbuf(es, [128, NCH, CH], F32, "m0")
        ctx.pool.op(lambda: nc.gpsimd.memset(m0[:, :, :], 1.0), writes=[m0])
        ctx.pool.op(lambda: nc.gpsimd.memset(m0[:, :, 0:1], 0.0), writes=[m0])
        cm = ctx.sbuf(es, [CH, NCH, CH], F32, "cm")
        ctx.pool.op(lambda: nc.gpsimd.memset(cm[:, :, :], 1.0), writes=[cm])
        ctx.pool.op(lambda: nc.gpsimd.affine_select(out=cm[:, :, :], in_=cm[:, :, :], pattern=[[0, NCH], [1, CH]],
                                                    compare_op=ALU.is_ge, fill=0.0, base=0, channel_multiplier=-1),
                    reads=[cm], writes=[cm])
        if scalar_decay:
            idm = ctx.sbuf(es, [CH, NCH, CH], F32, "idm")
            ctx.pool.op(lambda: nc.gpsimd.memset(idm[:, :, :], 1.0), writes=[idm])
            ctx.pool.op(lambda: nc.gpsimd.affine_select(out=idm[:, :, :], in_=idm[:, :, :], pattern=[[0, NCH], [1, CH]],
                                                        compare_op=ALU.is_equal, fill=0.0, base=0, channel_multiplier=-1), reads=[idm], writes=[idm])
            ngm = ctx.sbuf(es, [CH, NCH, CH], F32, "ngm")
            ctx.pool.op(lambda: nc.gpsimd.memset(ngm[:, :, :], 0.0), writes=[ngm])
            ctx.pool.op(lambda: nc.gpsimd.affine_select(out=ngm[:, :, :], in_=ngm[:, :, :], pattern=[[0, NCH], [1, CH]],
                                                        compare_op=ALU.is_ge, fill=-1.0e30, base=0, channel_multiplier=-1), reads=[ngm], writes=[ngm])
        S = ctx.sbuf(es, [128, nheads, dv], F32, "S")
        Sb = ctx.sbuf(es, [128, nheads, dv], BF16, "Sb")
        Sh = [Buf(ctx, S.t, "S_h") for _ in range(nheads)]
        Sbh = [Buf(ctx, Sb.t, "Sb_h") for _ in range(nheads)]
        ctx.pool.op(lambda: nc.gpsimd.memset(S[:, :, :], 0.0), writes=Sh)
        ctx.pool.op(lambda: nc.gpsimd.memset(Sb[:, :, :], 0.0), writes=Sbh)
        slots = []
        for i in range(nslots):
            sl = {
                "qf": ctx.sbuf(es, [128, SEG], F32, "qf"), "kf": ctx.sbuf(es, [128, SEG], F32, "kf"),
                "gf": ctx.sbuf(es, [128, SEG], F32, "gf"), "cum": ctx.sbuf(es, [128, NCH, CH], F32, "cum"),
                "t1": ctx.sbuf(es, [128, NCH, CH], F32, "t1"), "e": ctx.sbuf(es, [128, NCH, CH], F32, "e"),
                "qt": ctx.sbuf(es, [128, SEG], BF16, "qt"), "kt": ctx.sbuf(es, [128, SEG], BF16, "kt"),
                "qh": ctx.sbuf(es, [128, SEG], BF16, "qh"), "kd": ctx.sbuf(es, [128, SEG], BF16, "kd"),
                "dcol": ctx.sbuf(es, [128, NCH], F32, "dcol"),
                "dm": ctx.sbuf(es, [CH, NCH, CH], F32, "dm"), "col": ctx.sbuf(es, [CH, NCH], F32, "col"),
                "vT": [ctx.sbuf(es, [dvb, SEG], BF16, "vT") for _ in range(nvb)],
                "vtok": ctx.sbuf(es, [CH, NCH, dv], BF16, "vtok"), "kdtok": ctx.sbuf(es, [CH, NCH, 128], BF16, "kdtok"),
                "sc": ctx.sbuf(es, [CH, SEG], BF16, "sc"),
                "ysb": [ctx.sbuf(es, [dvb, SEG], F32, "ysb") for _ in range(nvb)],
                "aux": [ctx.sbuf(es, [128, SEG], F32, "aux") for _ in range(2)],
                "auxb": [ctx.sbuf(es, [128, SEG], BF16, "auxb") for _ in range(2)],
            }
            slots.append(sl)
        rem = 8 - G * nvb - 1
        n_sc = 2 if rem >= 4 else 1
        n_u = max(1, min(G, rem - n_sc))
        ps_sc = [ctx.psum(es, [CH, SEG], F32, "ps_sc") for _ in range(n_sc)]
        ps_y = [[ctx.psum(es, [dvb, SEG], F32, "ps_y") for _ in range(nvb)] for _ in range(G)]
        ps_tr = ctx.psum(es, [CH, 1024], BF16, "ps_tr")
        ps_u = [ctx.psum(es, [128, 512], F32, "ps_u") for _ in range(n_u)]
        ps_misc = ps_u[0]
        ident_b = consts["ident_b"]
        si = 0
        for seg in range(T // SEG):
            t0 = seg * SEG
            seg_setup(seg, t0)
            for h0 in range(0, nheads, G):
                hs = list(range(h0, min(h0 + G, nheads)))
                sls = []
                for h in hs:
                    sl = slots[si % nslots]
                    si += 1
                    sls.append(sl)
                    prologue(h, seg, t0, sl, ps_misc)
                    qf, kf, gf, cum, t1, e = sl["qf"], sl["kf"], sl["gf"], sl["cum"], sl["t1"], sl["e"]
                    cumf = cum.t.rearrange("p c t -> p (c t)")
                    ctx.dve.op(lambda: nc.vector.tensor_tensor_scan(out=cumf, data0=m0.t.rearrange("p c t -> p (c t)"), data1=gf[:, :],
                                                                    initial=0.0, op0=ALU.mult, op1=ALU.add), reads=[m0, gf], writes=[cum])
                    if scalar_decay:
                        dm, col = sl["dm"], sl["col"]
                        ctx.dve.op(lambda: nc.vector.tensor_tensor(out=dm[:, :, :], in0=cum[0:CH, :, :], in1=idm[:, :, :], op=ALU.mult), reads=[cum, idm], writes=[dm])
                        ctx.dve.op(lambda: nc.vector.tensor_reduce(out=col[:, :], in_=dm[:, :, :], axis=AX.X, op=ALU.add), reads=[dm], writes=[col])
                        ctx.dve.op(lambda: nc.vector.tensor_tensor(out=dm[:, :, :], in0=cum[0:CH, :, :], in1=col[:, :].unsqueeze(2).to_broadcast([CH, NCH, CH]),
                                                                   op=ALU.subtract), reads=[cum, col], writes=[dm])
                        ctx.dve.op(lambda: nc.vector.tensor_tensor(out=dm[:, :, :], in0=dm[:, :, :], in1=ngm[:, :, :], op=ALU.min), reads=[dm, ngm], writes=[dm])
                        ctx.act.op(lambda: nc.scalar.activation(out=dm[:, :, :], in_=dm[:, :, :], func=AF.Exp), reads=[dm], writes=[dm])
                        ctx.act.op(lambda: nc.scalar.activation(out=sl["qt"][:, :], in_=qf[:, :], func=AF.Copy), reads=[qf], writes=[sl["qt"]])
                        ctx.pool.op(lambda: nc.gpsimd.tensor_copy(out=sl["kt"][:, :], in_=kf[:, :]), reads=[kf], writes=[sl["kt"]])
                    else:
                        ctx.dve.op(lambda: nc.vector.tensor_tensor(out=t1[:, :, :], in0=cum[:, :, :], in1=cum[:, :, mid:mid + 1].to_broadcast([128, NCH, CH]),
                                                                   op=ALU.subtract), reads=[cum], writes=[t1])
                        ctx.act.op(lambda: nc.scalar.activation(out=e[:, :, :], in_=t1[:, :, :], func=AF.Exp), reads=[t1], writes=[e])
                        ctx.dve.op(lambda: nc.vector.tensor_tensor(out=sl["qt"][:, :], in0=qf[:, :], in1=e.t.rearrange("p c t -> p (c t)"), op=ALU.mult),
                                   reads=[qf, e], writes=[sl["qt"]])
                        ctx.act.op(lambda: nc.scalar.activation(out=e[:, :, :], in_=t1[:, :, :], func=AF.Exp, scale=-1.0), reads=[t1], writes=[e])
                        ctx.pool.op(lambda: nc.gpsimd.tensor_tensor(out=sl["kt"][:, :], in0=kf[:, :], in1=e.t.rearrange("p c t -> p (c t)"), op=ALU.mult),
                                    reads=[kf, e], writes=[sl["kt"]])
                    ctx.act.op(lambda: nc.scalar.activation(out=e[:, :, :], in_=cum[:, :, :], func=AF.Exp), reads=[cum], writes=[e])
                    ctx.dve.op(lambda: nc.vector.tensor_tensor(out=sl["qh"][:, :], in0=qf[:, :], in1=e.t.rearrange("p c t -> p (c t)"), op=ALU.mult),
                               reads=[qf, e], writes=[sl["qh"]])
                    ctx.act.op(lambda: nc.scalar.activation(out=sl["dcol"][:, :], in_=cum[:, :, CH - 1], func=AF.Exp), reads=[cum], writes=[sl["dcol"]])
                    ctx.pool.op(lambda: nc.gpsimd.tensor_tensor(out=t1[:, :, :], in0=cum[:, :, :], in1=cum[:, :, CH - 1:CH].to_broadcast([128, NCH, CH]),
                                                                op=ALU.subtract), reads=[cum], writes=[t1])
                    ctx.act.op(lambda: nc.scalar.activation(out=e[:, :, :], in_=t1[:, :, :], func=AF.Exp, scale=-1.0), reads=[t1], writes=[e])
                    ctx.pool.op(lambda: nc.gpsimd.tensor_tensor(out=sl["kd"][:, :], in0=kf[:, :], in1=e.t.rearrange("p c t -> p (c t)"), op=ALU.mult),
                                reads=[kf, e], writes=[sl["kd"]])
                    per = max(1, min(NCH, 1024 // dv))
                    kg = min(NCH, 8)
                    for c0 in range(0, NCH, per):
                        def trv(c0=c0):
                            ins = None
                            for c in range(c0, c0 + per):
                                for vb in range(nvb):
                                    o0 = (c - c0) * dv + vb * 128
                                    ins = ctx.tr(out=ps_tr[0:CH, o0:o0 + dvb], in_=sl["vT"][vb][0:dvb, c * CH:(c + 1) * CH],
                                                              identity=ident_b[0:dvb, 0:dvb])
                            return ins
                        ctx.pe.op(trv, reads=sl["vT"] + [ident_b], writes=[ps_tr])
                        ctx.act.op(lambda c0=c0: nc.scalar.activation(out=sl["vtok"][:, c0:c0 + per, :], in_=ps_tr.t[0:CH, 0:per * dv].rearrange("p (c d) -> p c d", d=dv),
                                                                      func=AF.Copy), reads=[ps_tr], writes=[sl["vtok"]])
                    for c0 in range(0, NCH, kg):
                        def trk(c0=c0):
                            ins = None
                            for c in range(c0, c0 + kg):
                                ins = ctx.tr(out=ps_tr[0:CH, (c - c0) * 128:(c - c0 + 1) * 128], in_=sl["kd"][:, c * CH:(c + 1) * CH],
                                                          identity=ident_b[:, :])
                            return ins
                        ctx.pe.op(trk, reads=[sl["kd"], ident_b], writes=[ps_tr])
                        ctx.dve.op(lambda c0=c0: nc.vector.tensor_copy(out=sl["kdtok"][:, c0:c0 + kg, :], in_=ps_tr.t[0:CH, 0:kg * 128].rearrange("p (c d) -> p c d", d=128)),
                                   reads=[ps_tr], writes=[sl["kdtok"]])
                for gi, h in enumerate(hs):
                    sl = sls[gi]

                    def scm(sl=sl, gi=gi):
                        ins = None
                        for c in range(NCH):
                            cs = slice(c * CH, (c + 1) * CH)
                            ins = ctx.mm(ps_sc[gi % n_sc][0:CH, cs], lhsT=sl["kt"][:, cs], rhs=sl["qt"][:, cs], start=True, stop=True)
                        return ins
                    ctx.pe.op(scm, reads=[sl["kt"], sl["qt"]], writes=[ps_sc[gi % n_sc]])
                    mkb = sl["dm"] if scalar_decay else cm
                    ctx.dve.op(lambda sl=sl, gi=gi, mkb=mkb: nc.vector.tensor_tensor(out=sl["sc"][:, :], in0=ps_sc[gi % n_sc][:, :], in1=mkb.t.rearrange("p c t -> p (c t)"), op=ALU.mult),
                               reads=[ps_sc[gi % n_sc], mkb], writes=[sl["sc"]])
                for c in range(NCH):
                    cs = slice(c * CH, (c + 1) * CH)
                    for gi, h in enumerate(hs):
                        sl = sls[gi]

                        def ymm(sl=sl, gi=gi, h=h):
                            ins = None
                            for vb in range(nvb):
                                vs = slice(vb * 128, vb * 128 + dvb)
                                ctx.mm(ps_y[gi][vb][0:dvb, cs], lhsT=sl["vtok"][0:CH, c, vs], rhs=sl["sc"][0:CH, cs], start=True, stop=False)
                                ins = ctx.mm(ps_y[gi][vb][0:dvb, cs], lhsT=Sb[:, h, vs], rhs=sl["qh"][:, cs], start=False, stop=True)
                            return ins
                        ctx.pe.op(ymm, reads=[sl["vtok"], sl["sc"], Sbh[h], sl["qh"]], writes=ps_y[gi])
                        pu = ps_u[gi % n_u]
                        ctx.pe.op(lambda sl=sl, pu=pu: ctx.mm(pu[:, 0:dv], lhsT=sl["kdtok"][0:CH, c, :], rhs=sl["vtok"][0:CH, c, :], start=True, stop=True),
                                  reads=[sl["kdtok"], sl["vtok"]], writes=[pu])
                        ctx.dve.op(lambda sl=sl, h=h, pu=pu: nc.vector.scalar_tensor_tensor(out=Sb[:, h, :], in0=S[:, h, :], scalar=sl["dcol"][:, c:c + 1], in1=pu[:, 0:dv],
                                                                                           op0=ALU.mult, op1=ALU.add), reads=[Sh[h], sl["dcol"], pu], writes=[Sbh[h]])
                        ctx.dve.op(lambda sl=sl, h=h, pu=pu: nc.vector.scalar_tensor_tensor(out=S[:, h, :], in0=S[:, h, :], scalar=sl["dcol"][:, c:c + 1], in1=pu[:, 0:dv],
                                                                                           op0=ALU.mult, op1=ALU.add), reads=[Sh[h], sl["dcol"], pu], writes=[Sh[h]])
                for gi, h in enumerate(hs):
                    sl = sls[gi]
                    for vb in range(nvb):
                        ctx.act.op(lambda sl=sl, gi=gi, vb=vb: nc.scalar.activation(out=sl["ysb"][vb][:, :], in_=ps_y[gi][vb][:, :], func=AF.Copy),
                                   reads=[ps_y[gi][vb]], writes=[sl["ysb"][vb]])
                        r0 = h * dv + vb * 128
                        ctx.sp.dma([(Y.t[r0:r0 + dvb, t0:t0 + SEG], sl["ysb"][vb][:, :])], sl["ysb"][vb], reads=[sl["ysb"][vb]], writes=[Y])
        ctx.barrier()


def lb_setup(ctx, es, logits_ap):
    nc = ctx.nc
    lg = ctx.sbuf(es, [128, 16, DEPTH], F32, "lb_lg")
    ctx.sp.dma([(lg[:, :, l], logits_ap[l].rearrange("(b p) -> p b", p=128)) for l in range(DEPTH)], lg, writes=[lg], allow_slow_non_contiguous=True)
    ctx.act.op(lambda: nc.scalar.activation(out=lg[:, :, :], in_=lg[:, :, :], func=AF.Exp), reads=[lg], writes=[lg])
    s = ctx.sbuf(es, [128, 16], F32, "lb_s")
    ctx.dve.op(lambda: nc.vector.tensor_reduce(out=s[:, :], in_=lg[:, :, :], axis=AX.X, op=ALU.add), reads=[lg], writes=[s])
    ctx.dve.op(lambda: nc.vector.reciprocal(out=s[:, :], in_=s[:, :]), reads=[s], writes=[s])
    res = {}
    for layer in (1, 3):
        lb = ctx.sbuf(es, [128, 16], F32, "lb")
        oml = ctx.sbuf(es, [128, 16], F32, "oml")
        noml = ctx.sbuf(es, [128, 16], F32, "noml")
        ctx.dve.op(lambda: nc.vector.tensor_copy(out=lb[:, :], in_=lg[:, :, 1]), reads=[lg], writes=[lb])
        for l in range(2, layer + 1):
            ctx.dve.op(lambda l=l: nc.vector.tensor_tensor(out=lb[:, :], in0=lb[:, :], in1=lg[:, :, l], op=ALU.add), reads=[lb, lg], writes=[lb])
        ctx.dve.op(lambda: nc.vector.tensor_tensor(out=lb[:, :], in0=lb[:, :], in1=s[:, :], op=ALU.mult), reads=[lb, s], writes=[lb])
        ctx.dve.op(lambda: nc.vector.tensor_scalar(out=oml[:, :], in0=lb[:, :], scalar1=-1.0, scalar2=1.0, op0=ALU.mult, op1=ALU.add), reads=[lb], writes=[oml])
        ctx.dve.op(lambda: nc.vector.tensor_scalar(out=noml[:, :], in0=oml[:, :], scalar1=-1.0, scalar2=None, op0=ALU.mult), reads=[oml], writes=[noml])
        res[layer] = (lb, oml, noml)
    return res


def conv_phase(ctx, T, XBC, XBCc, conv_w, conv_b):
    nc = ctx.nc
    NBK = 3072 // 128
    with ExitStack() as es:
        w = ctx.sbuf(es, [128, NBK, 4], F32, "cv_w")
        b = ctx.sbuf(es, [128, NBK], F32, "cv_b")
        ctx.sp.dma([(w[:, :, jj], conv_w[jj].rearrange("(b p) -> p b", p=128)) for jj in range(4)], w, writes=[w], allow_slow_non_contiguous=True)
        ctx.sp.dma([(b[:, :], conv_b.rearrange("(b p) -> p b", p=128))], b, writes=[b], allow_slow_non_contiguous=True)
        xps = [ctx.sbuf(es, [128, T + 3], BF16, "cv_x") for _ in range(2)]
        accs = [ctx.sbuf(es, [128, T], F32, "cv_a") for _ in range(2)]
        outs = [ctx.sbuf(es, [128, T], BF16, "cv_o") for _ in range(2)]
        for xp in xps:
            ctx.pool.op(lambda xp=xp: nc.gpsimd.memset(xp[:, 0:3], 0.0), writes=[xp])
        for blk in range(NBK):
            xp, acc, o = xps[blk % 2], accs[blk % 2], outs[blk % 2]
            ctx.sp.dma([(xp[:, 3:T + 3], XBC.t[blk * 128:(blk + 1) * 128, :])], xp, reads=[XBC], writes=[xp])
            ctx.dve.op(lambda: nc.vector.tensor_scalar(out=acc[:, :], in0=xp[:, 0:T], scalar1=w[:, blk, 0:1], scalar2=b[:, blk:blk + 1], op0=ALU.mult, op1=ALU.add),
                       reads=[xp, w, b], writes=[acc])
            for j in range(1, 4):
                ctx.dve.op(lambda j=j: nc.vector.scalar_tensor_tensor(out=acc[:, :], in0=xp[:, j:j + T], scalar=w[:, blk, j:j + 1], in1=acc[:, :], op0=ALU.mult, op1=ALU.add),
                           reads=[xp, w, acc], writes=[acc])
            ctx.act.op(lambda: nc.scalar.activation(out=o[:, :], in_=acc[:, :], func=AF.Silu), reads=[acc], writes=[o])
            ctx.sp.dma([(XBCc.t[blk * 128:(blk + 1) * 128, :], o[:, :])], o, reads=[o], writes=[XBCc])
        ctx.barrier()


def dt_phase(ctx, T, DT, SDT, dt_bias, a_log):
    nc = ctx.nc
    with ExitStack() as es:
        x = ctx.sbuf(es, [32, T], F32, "dt_x")
        y = ctx.sbuf(es, [32, T], F32, "dt_y")
        pb = ctx.sbuf(es, [32, 2], F32, "dt_pb")
        ctx.sp.dma([(pb[:, 0:1], dt_bias.rearrange("(h o) -> h o", o=1)), (pb[:, 1:2], a_log.rearrange("(h o) -> h o", o=1))], pb, writes=[pb],
                   allow_slow_non_contiguous=True)
        ctx.sp.dma([(x[:, :], DT.t[:, :])], x, reads=[DT], writes=[x])
        ctx.act.op(lambda: nc.scalar.activation(out=pb[:, 1:2], in_=pb[:, 1:2], func=AF.Exp), reads=[pb], writes=[pb])
        ctx.dve.op(lambda: nc.vector.tensor_scalar(out=pb[:, 1:2], in0=pb[:, 1:2], scalar1=-1.0, scalar2=None, op0=ALU.mult), reads=[pb], writes=[pb])
        ctx.act.op(lambda: nc.scalar.activation(out=x[:, :], in_=x[:, :], func=AF.Exp, bias=pb[:, 0:1]), reads=[x, pb], writes=[x])
        ctx.act.op(lambda: nc.scalar.activation(out=x[:, :], in_=x[:, :], func=AF.Ln, bias=1.0), reads=[x], writes=[x])
        ctx.dve.op(lambda: nc.vector.tensor_scalar(out=y[:, :], in0=x[:, :], scalar1=pb[:, 1:2], scalar2=None, op0=ALU.mult), reads=[x, pb], writes=[y])
        ctx.sp.dma([(SDT.t[0:32, :], x[:, :])], x, reads=[x], writes=[SDT])
        ctx.sp.dma([(SDT.t[32:64, :], y[:, :])], y, reads=[y], writes=[SDT])
        ctx.barrier()


def ret_scan(ctx, consts, T, P, Y):
    nc = ctx.nc
    with ExitStack() as es:
        ones_f = consts["ones_f"]
        ra = ctx.sbuf(es, [128, 128], F32, "rot_a")
        rb_ = ctx.sbuf(es, [128, 128], F32, "rot_b")
        rot = ctx.sbuf(es, [128, 128], BF16, "rot")
        ctx.pool.op(lambda: nc.gpsimd.affine_select(out=ra[:, :], in_=ones_f[:, :], pattern=[[-1, 128]], compare_op=ALU.is_equal, fill=0.0, base=64, channel_multiplier=1),
                    reads=[ones_f], writes=[ra])
        ctx.pool.op(lambda: nc.gpsimd.affine_select(out=rb_[:, :], in_=ones_f[:, :], pattern=[[-1, 128]], compare_op=ALU.is_equal, fill=0.0, base=-64, channel_multiplier=1),
                    reads=[ones_f], writes=[rb_])
        ctx.dve.op(lambda: nc.vector.tensor_tensor(out=rot[:, :], in0=ra[:, :], in1=rb_[:, :], op=ALU.subtract), reads=[ra, rb_], writes=[rot])
        pidx = ctx.sbuf(es, [128, 1], F32, "pidx")
        for half in range(2):
            ctx.pool.op(lambda half=half: nc.gpsimd.iota(pidx[half * 64:(half + 1) * 64, :], pattern=[[0, 1]], base=0, channel_multiplier=1,
                                                         allow_small_or_imprecise_dtypes=True), writes=[pidx])
        ifr = ctx.sbuf(es, [128, 1], F32, "ifr")
        ctx.act.op(lambda: nc.scalar.activation(out=ifr[:, :], in_=pidx[:, :], func=AF.Exp, scale=-math.log(10000.0) / 64.0), reads=[pidx], writes=[ifr])
        ctx.dve.op(lambda: nc.vector.tensor_scalar(out=ifr[:, :], in0=ifr[:, :], scalar1=1.0 / (2.0 * math.pi), scalar2=None, op0=ALU.mult), reads=[ifr], writes=[ifr])
        pos = ctx.sbuf(es, [128, SEG], F32, "pos")
        fr = ctx.sbuf(es, [128, SEG], F32, "fr")
        fi = ctx.sbuf(es, [128, SEG], I32, "fi")
        ff = ctx.sbuf(es, [128, SEG], F32, "ff")
        msk = ctx.sbuf(es, [128, SEG], F32, "msk")
        tabs = {k: ctx.sbuf(es, [128, SEG], F32, "tab_" + k) for k in ("cos", "sin", "cosk", "sink")}
        tb = Buf(ctx, None, "tabs")
        ks = 128.0 ** -0.5

        def wrap(src):
            ctx.dve.op(lambda: nc.vector.tensor_scalar(out=msk[:, :], in0=src[:, :], scalar1=0.5, scalar2=None, op0=ALU.is_gt), reads=[src], writes=[msk])
            ctx.dve.op(lambda: nc.vector.tensor_tensor(out=src[:, :], in0=src[:, :], in1=msk[:, :], op=ALU.subtract), reads=[src, msk], writes=[src])
            ctx.dve.op(lambda: nc.vector.tensor_scalar(out=msk[:, :], in0=src[:, :], scalar1=-0.5, scalar2=None, op0=ALU.is_lt), reads=[src], writes=[msk])
            ctx.dve.op(lambda: nc.vector.tensor_tensor(out=src[:, :], in0=src[:, :], in1=msk[:, :], op=ALU.add), reads=[src, msk], writes=[src])

        def seg_setup(seg, t0):
            ctx.pool.op(lambda: nc.gpsimd.iota(pos[:, :], pattern=[[1, SEG]], base=t0, channel_multiplier=0, allow_small_or_imprecise_dtypes=True),
                        reads=[], writes=[pos])
            ctx.dve.op(lambda: nc.vector.tensor_scalar(out=fr[:, :], in0=pos[:, :], scalar1=ifr[:, 0:1], scalar2=None, op0=ALU.mult), reads=[pos, ifr], writes=[fr])
            ctx.dve.op(lambda: nc.vector.tensor_copy(out=fi[:, :], in_=fr[:, :]), reads=[fr], writes=[fi])
            ctx.dve.op(lambda: nc.vector.tensor_copy(out=ff[:, :], in_=fi[:, :]), reads=[fi], writes=[ff])
            ctx.dve.op(lambda: nc.vector.tensor_tensor(out=fr[:, :], in0=fr[:, :], in1=ff[:, :], op=ALU.subtract), reads=[fr, ff], writes=[fr])
            wrap(fr)
            ctx.act.op(lambda: nc.scalar.activation(out=tabs["sin"][:, :], in_=fr[:, :], func=AF.Sin, scale=2.0 * math.pi), reads=[fr], writes=[tabs["sin"], tb])
            ctx.dve.op(lambda: nc.vector.tensor_scalar(out=ff[:, :], in0=fr[:, :], scalar1=0.25, scalar2=None, op0=ALU.add), reads=[fr], writes=[ff])
            wrap(ff)
            ctx.act.op(lambda: nc.scalar.activation(out=tabs["cos"][:, :], in_=ff[:, :], func=AF.Sin, scale=2.0 * math.pi), reads=[ff], writes=[tabs["cos"], tb])
            ctx.dve.op(lambda: nc.vector.tensor_scalar(out=tabs["sink"][:, :], in0=tabs["sin"][:, :], scalar1=ks, scalar2=None, op0=ALU.mult), reads=[tabs["sin"]], writes=[tabs["sink"], tb])
            ctx.dve.op(lambda: nc.vector.tensor_scalar(out=tabs["cosk"][:, :], in0=tabs["cos"][:, :], scalar1=ks, scalar2=None, op0=ALU.mult), reads=[tabs["cos"]], writes=[tabs["cosk"], tb])

        def prologue(h, seg, t0, sl, ps_misc):
            qa, ka = sl["auxb"]
            ts = slice(t0, t0 + SEG)
            ctx.sp.dma([(qa[:, :], P.t[h * 128:(h + 1) * 128, ts])], qa, reads=[P], writes=[qa])
            ctx.sp.dma([(ka[:, :], P.t[1024 + h * 128:1024 + (h + 1) * 128, ts])], ka, reads=[P], writes=[ka])
            for vb in range(2):
                r0 = 2048 + h * 256 + vb * 128
                ctx.sp.dma([(sl["vT"][vb][:, :], P.t[r0:r0 + 128, ts])], sl["vT"][vb], reads=[P], writes=[sl["vT"][vb]])
            lg = math.log1p(-2.0 ** (-5.0 - h))
            ctx.pool.op(lambda: nc.gpsimd.memset(sl["gf"][:, :], lg), writes=[sl["gf"]])
            for src, dst, c_, s_ in ((qa, sl["qf"], "cos", "sin"), (ka, sl["kf"], "cosk", "sink")):
                a0 = sl["aux"][0]
                ctx.pe.op(lambda src=src: ctx.mm(ps_misc[:, :], lhsT=rot[:, :], rhs=src[:, :], start=True, stop=True), reads=[rot, src], writes=[ps_misc])
                ctx.dve.op(lambda: nc.vector.tensor_tensor(out=a0[:, :], in0=ps_misc[:, :], in1=tabs[s_][:, :], op=ALU.mult), reads=[ps_misc, tb], writes=[a0])
                ctx.pool.op(lambda: nc.gpsimd.tensor_tensor(out=dst[:, :], in0=src[:, :], in1=tabs[c_][:, :], op=ALU.mult), reads=[src, tb], writes=[dst])
                ctx.dve.op(lambda: nc.vector.tensor_tensor(out=dst[:, :], in0=dst[:, :], in1=a0[:, :], op=ALU.add), reads=[dst, a0], writes=[dst])

        gla_scan(ctx, consts, T, 8, 256, Y, seg_setup, prologue, CH=128, G=2)


def ssd_scan(ctx, consts, T, XBCc, SDT, Y):
    nc = ctx.nc

    def seg_setup(seg, t0):
        pass

    def prologue(h, seg, t0, sl, ps_misc):
        g = h // 8
        ts = slice(t0, t0 + SEG)
        ba, ca = sl["auxb"]
        dtb = sl["aux"][0]
        ctx.sp.dma([(ba[:, :], XBCc.t[2048 + g * 128:2048 + (g + 1) * 128, ts])], ba, reads=[XBCc], writes=[ba])
        ctx.sp.dma([(ca[:, :], XBCc.t[2560 + g * 128:2560 + (g + 1) * 128, ts])], ca, reads=[XBCc], writes=[ca])
        ctx.sp.dma([(sl["vT"][0][:, :], XBCc.t[h * 64:(h + 1) * 64, ts])], sl["vT"][0], reads=[XBCc], writes=[sl["vT"][0]])
        ctx.sp.dma([(dtb[:, :], SDT.t[h, ts].partition_broadcast(128))], dtb, reads=[SDT], writes=[dtb])
        ctx.sp.dma([(sl["gf"][:, :], SDT.t[32 + h, ts].partition_broadcast(128))], sl["gf"], reads=[SDT], writes=[sl["gf"]])
        ctx.pool.op(lambda: nc.gpsimd.tensor_tensor(out=sl["kf"][:, :], in0=ba[:, :], in1=dtb[:, :], op=ALU.mult), reads=[ba, dtb], writes=[sl["kf"]])
        ctx.act.op(lambda: nc.scalar.activation(out=sl["qf"][:, :], in_=ca[:, :], func=AF.Copy), reads=[ca], writes=[sl["qf"]])

    gla_scan(ctx, consts, T, 32, 64, Y, seg_setup, prologue, scalar_decay=True, CH=128, G=2)


def hgrn_scan(ctx, consts, T, P, FR, lbs, Y):
    nc = ctx.nc
    lb, oml, noml = lbs

    def seg_setup(seg, t0):
        pass

    def prologue(h, seg, t0, sl, ps_misc):
        ts = slice(t0, t0 + SEG)
        qa = sl["auxb"][0]
        fr, sg = sl["aux"]
        ctx.sp.dma([(qa[:, :], P.t[h * 128:(h + 1) * 128, ts])], qa, reads=[P], writes=[qa])
        ctx.sp.dma([(sl["vT"][0][:, :], P.t[2048 + h * 128:2048 + (h + 1) * 128, ts])], sl["vT"][0], reads=[P], writes=[sl["vT"][0]])
        ctx.sp.dma([(fr[:, :], FR.t[h * 128:(h + 1) * 128, ts])], fr, reads=[FR], writes=[fr])
        ctx.act.op(lambda: nc.scalar.activation(out=sl["qf"][:, :], in_=qa[:, :], func=AF.Copy), reads=[qa], writes=[sl["qf"]])
        ctx.act.op(lambda: nc.scalar.activation(out=sg[:, :], in_=fr[:, :], func=AF.Sigmoid), reads=[fr], writes=[sg])
        ctx.dve.op(lambda: nc.vector.tensor_scalar(out=fr[:, :], in0=sg[:, :], scalar1=oml[:, h:h + 1], scalar2=lb[:, h:h + 1], op0=ALU.mult, op1=ALU.add),
                   reads=[sg, oml, lb], writes=[fr])
        ctx.act.op(lambda: nc.scalar.activation(out=sl["gf"][:, :], in_=fr[:, :], func=AF.Ln), reads=[fr], writes=[sl["gf"]])
        ctx.dve.op(lambda: nc.vector.tensor_scalar(out=sl["kf"][:, :], in0=sg[:, :], scalar1=noml[:, h:h + 1], scalar2=oml[:, h:h + 1], op0=ALU.mult, op1=ALU.add),
                   reads=[sg, noml, oml], writes=[sl["kf"]])

    gla_scan(ctx, consts, T, 16, 128, Y, seg_setup, prologue, CH=32, G=4, nslots=5)


def post_phase(ctx, consts, T, kind, Y, CAT, cat_row0, wvec, gate_src, gate_row0, xs_src=None, dskip=None):
    nc = ctx.nc
    NBH = 8
    gsz = {"ret": 2, "ssd": 4, "hgrn": 1}[kind]
    with ExitStack() as es:
        wk = norm_work(ctx, es, NBH)
        wv = ctx.sbuf(es, [128, 16], F32, "po_w")
        ctx.sp.dma([(wv[:, :], wvec.rearrange("(b p) -> p b", p=128))], wv, writes=[wv], allow_slow_non_contiguous=True)
        if dskip is not None:
            dk = ctx.sbuf(es, [128, 16], F32, "po_dk")
            ctx.sp.dma([(dk[:, :], dskip.rearrange("(b p) -> p b", p=128))], dk, writes=[dk], allow_slow_non_contiguous=True)
        ys = [ctx.sbuf(es, [128, NBH, 512], F32, "po_y") for _ in range(2)]
        gs = [ctx.sbuf(es, [128, NBH, 512], BF16, "po_g") for _ in range(2)]
        xs_ = [ctx.sbuf(es, [128, NBH, 512], BF16, "po_x") for _ in range(2)] if xs_src is not None else None
        os_ = [ctx.sbuf(es, [128, NBH, 512], BF16, "po_o") for _ in range(2)]
        it = 0
        for nt in range(T // 512):
            tsl = slice(nt * 512, (nt + 1) * 512)
            for half in range(2):
                b0 = half * NBH
                y, g, o = ys[it % 2], gs[it % 2], os_[it % 2]
                rs = slice(b0 * 128, (b0 + NBH) * 128)
                ctx.sp.dma([(y[:, :, :], Y.t[rs, tsl].rearrange("(b p) t -> p b t", p=128))], y, reads=[Y], writes=[y])
                grs = slice(gate_row0 + b0 * 128, gate_row0 + (b0 + NBH) * 128)
                ctx.sp.dma([(g[:, :, :], gate_src.t[grs, tsl].rearrange("(b p) t -> p b t", p=128))], g, reads=[gate_src], writes=[g])
                if kind == "ssd":
                    x = xs_[it % 2]
                    ctx.sp.dma([(x[:, :, :], xs_src.t[rs, tsl].rearrange("(b p) t -> p b t", p=128))], x, reads=[xs_src], writes=[x])
                    for b in range(NBH):
                        ctx.dve.op(lambda b=b: nc.vector.scalar_tensor_tensor(out=y[:, b, :], in0=x[:, b, :], scalar=dk[:, b0 + b:b0 + b + 1], in1=y[:, b, :],
                                                                             op0=ALU.mult, op1=ALU.add), reads=[x, dk, y], writes=[y])
                    ctx.pool.op(lambda: nc.gpsimd.tensor_tensor(out=y[:, :, :], in0=y[:, :, :], in1=g[:, :, :], op=ALU.mult), reads=[y, g], writes=[y])
                group_norm_tile(ctx, consts, wk, y, y, NBH, gsz, kind == "ret", gsz * 128)
                for b in range(NBH):
                    if kind == "ssd":
                        ctx.act.op(lambda b=b: nc.scalar.activation(out=o[:, b, :], in_=y[:, b, :], func=AF.Copy, scale=wv[:, b0 + b:b0 + b + 1]), reads=[y, wv], writes=[o])
                    else:
                        ctx.dve.op(lambda b=b: nc.vector.scalar_tensor_tensor(out=o[:, b, :], in0=y[:, b, :], scalar=wv[:, b0 + b:b0 + b + 1], in1=g[:, b, :],
                                                                             op0=ALU.mult, op1=ALU.mult), reads=[y, wv, g], writes=[o])
                ors = slice(cat_row0 + b0 * 128, cat_row0 + (b0 + NBH) * 128)
                ctx.sp.dma([(CAT.t[ors, tsl].rearrange("(b p) t -> p b t", p=128), o[:, :, :])], o, reads=[o], writes=[CAT])
                it += 1
        ctx.barrier()


def router_phase(ctx, consts, T, XT, w_router, GB):
    nc = ctx.nc
    with ExitStack() as es:
        wr = ctx.sbuf(es, [128, 16, NEXP], F32, "rt_w")
        ctx.sp.dma([(wr[:, :, :], w_router.rearrange("(kc p) e -> p kc e", p=128))], wr, writes=[wr])
        xs = [ctx.sbuf(es, [128, 16, 512], F32, "rt_x") for _ in range(2)]
        gall = ctx.sbuf(es, [NEXP, T], F32, "rt_gall")
        ps_l = ctx.psum(es, [128, NEXP], F32, "rt_psl")
        ps_t = ctx.psum(es, [NEXP, 128], F32, "rt_pst")
        lg = ctx.sbuf(es, [128, NEXP], F32, "rt_lg")
        m8 = ctx.sbuf(es, [128, 8], F32, "rt_m8")
        nm = ctx.sbuf(es, [128, 1], F32, "rt_nm")
        mk = ctx.sbuf(es, [128, NEXP], F32, "rt_mk")
        ex = ctx.sbuf(es, [128, NEXP], F32, "rt_ex")
        sm = ctx.sbuf(es, [128, 1], F32, "rt_sm")
        ident_f = consts["ident_f"]
        for nt in range(T // 512):
            x = xs[nt % 2]
            ctx.sp.dma([(x[:, :, :], XT.t[:, nt * 512:(nt + 1) * 512].rearrange("(kc p) t -> p kc t", p=128))], x, reads=[XT], writes=[x])
            for j in range(4):
                tk = slice(j * 128, (j + 1) * 128)

                def mm():
                    ins = None
                    for kc in range(16):
                        ins = ctx.mm(ps_l[:, :], lhsT=x[:, kc, tk], rhs=wr[:, kc, :], start=(kc == 0), stop=(kc == 15))
                    return ins
                ctx.pe.op(mm, reads=[x, wr], writes=[ps_l])
                ctx.dve.op(lambda: nc.vector.tensor_copy(out=lg[:, :], in_=ps_l[:, :]), reads=[ps_l], writes=[lg])
                ctx.dve.op(lambda: nc.vector.max(out=m8[:, :], in_=lg[:, :]), reads=[lg], writes=[m8])
                ctx.dve.op(lambda: nc.vector.tensor_scalar(out=mk[:, :], in0=lg[:, :], scalar1=m8[:, 1:2], scalar2=None, op0=ALU.is_ge), reads=[lg, m8], writes=[mk])
                ctx.dve.op(lambda: nc.vector.tensor_scalar(out=nm[:, :], in0=m8[:, 0:1], scalar1=-1.0, scalar2=None, op0=ALU.mult), reads=[m8], writes=[nm])
                ctx.act.op(lambda: nc.scalar.activation(out=ex[:, :], in_=lg[:, :], func=AF.Exp, bias=nm[:, 0:1]), reads=[lg, nm], writes=[ex])
                ctx.dve.op(lambda: nc.vector.tensor_tensor(out=ex[:, :], in0=ex[:, :], in1=mk[:, :], op=ALU.mult), reads=[ex, mk], writes=[ex])
                ctx.dve.op(lambda: nc.vector.tensor_reduce(out=sm[:, :], in_=ex[:, :], axis=AX.X, op=ALU.add), reads=[ex], writes=[sm])
                ctx.dve.op(lambda: nc.vector.reciprocal(out=sm[:, :], in_=sm[:, :]), reads=[sm], writes=[sm])
                ctx.dve.op(lambda: nc.vector.tensor_scalar(out=ex[:, :], in0=ex[:, :], scalar1=sm[:, 0:1], scalar2=None, op0=ALU.mult), reads=[ex, sm], writes=[ex])
                ctx.pe.op(lambda: ctx.tr(out=ps_t[:, :], in_=ex[:, :], identity=ident_f[:, :]), reads=[ex, ident_f], writes=[ps_t])
                c0 = nt * 512 + j * 128
                ctx.act.op(lambda: nc.scalar.activation(out=gall[:, c0:c0 + 128], in_=ps_t[:, :], func=AF.Copy), reads=[ps_t], writes=[gall])
        ctx.sp.dma([(GB.t[:, :], gall[:, :])], gall, reads=[gall], writes=[GB])
        ctx.barrier()


def moe_down(ctx, H, T, W2s, M):
    nc = ctx.nc
    TS = min(T, 1024)
    NT = TS // 512
    KH = 22
    WCB = 256
    with ExitStack() as es:
        acc = ctx.sbuf(es, [128, 16, TS], F32, "md_acc")
        a_sb = [ctx.sbuf(es, [128, KH, TS], BF16, "md_a") for _ in range(2)]
        w_sb = [ctx.sbuf(es, [128, KH, WCB], BF16, "md_w") for _ in range(2)]
        pss = [ctx.psum(es, [128, 512], F32, "md_ps") for _ in range(3)]
        wi = 0
        pi = 0
        ai = 0
        for st in range(T // TS):
            t0 = st * TS
            first = True
            for e, W2 in enumerate(W2s):
                for half in range(2):
                    r0 = half * KH * 128
                    a = a_sb[ai % 2]
                    ai += 1
                    A_v = H[e].t[r0:r0 + KH * 128, t0:t0 + TS].rearrange("(kc p) t -> p kc t", p=128)
                    ctx.sp.dma([(a[:, k0:k0 + 11, :], A_v[:, k0:k0 + 11, :]) for k0 in range(0, KH, 11)], a, reads=[H[e]], writes=[a])
                    for c0 in range(0, D, WCB):
                        wb = w_sb[wi % 2]
                        wi += 1
                        W_v = W2[r0:r0 + KH * 128, c0:c0 + WCB].rearrange("(kc p) c -> p kc c", p=128)
                        ctx.pool.dma([(wb[:, k0:k0 + 11, :], W_v[:, k0:k0 + 11, :]) for k0 in range(0, KH, 11)], wb, writes=[wb])
                        for m0 in range(0, WCB, 128):
                            cb = (c0 + m0) // 128
                            for nt in range(NT):
                                ps = pss[pi % 3]
                                pi += 1
                                ns = slice(nt * 512, (nt + 1) * 512)

                                def mm(ps=ps, wb=wb, a=a, m0=m0, ns=ns):
                                    ins = None
                                    for kc in range(KH):
                                        ins = ctx.mm(ps[:, :], lhsT=wb[:, kc, m0:m0 + 128], rhs=a[:, kc, ns], start=(kc == 0), stop=(kc == KH - 1))
                                    return ins
                                ctx.pe.op(mm, reads=[wb, a], writes=[ps])
                                if first:
                                    ctx.act.op(lambda ps=ps, cb=cb, ns=ns: nc.scalar.activation(out=acc[:, cb, ns], in_=ps[:, :], func=AF.Copy), reads=[ps], writes=[acc])
                                else:
                                    ctx.dve.op(lambda ps=ps, cb=cb, ns=ns: nc.vector.tensor_tensor(out=acc[:, cb, ns], in0=acc[:, cb, ns], in1=ps[:, :], op=ALU.add),
                                               reads=[ps, acc], writes=[acc])
                    first = False
            ctx.sp.dma([(M.t[:, t0:t0 + TS].rearrange("(b p) t -> p b t", p=128), acc[:, :, :])], acc, reads=[acc], writes=[M])
        ctx.barrier()


def in_proj(ctx, XB, T, W, C, colmap):
    TS = min(T, 2048)
    with ExitStack() as es:
        stagers = {"b": Stager(ctx, es, TS, BF16, 3, "ip_b"), "f": Stager(ctx, es, TS, F32, 2, "ip_f")}
        et, eb = make_plain_epi(ctx, colmap, stagers)
        gemm(ctx, XB, D, T, [W], C, et, eb, TS, 512)


def out_proj(ctx, CAT, K, T, W, M):
    TS = min(T, 1024)
    with ExitStack() as es:
        stg = Stager(ctx, es, TS, F32, 3, "op_stg")
        et, eb = make_plain_epi(ctx, lambda cb0: (M, cb0, None, "m"), {"m": stg})
        gemm(ctx, CAT, K, T, [W], D, et, eb, TS, 256)


def even_layer(ctx, consts, T, S, w, j, last_out=None):
    P, DT, XBCc, SDT, Y, CAT, M, H, XT, XB = (S[k] for k in ("P", "DT", "XBCc", "SDT", "Y", "CAT", "M", "H", "XT", "XB"))

    def colmap(cb0):
        if cb0 < 4096:
            return (P, cb0, None, "b")
        if cb0 < 8192:
            return (P, cb0, AF.Silu, "b")
        if cb0 < 11264:
            return (P, cb0, None, "b")
        return (DT, 0, None, "f")
    in_proj(ctx, XB, T, w["w_in"], EV_IN, colmap)
    ret_scan(ctx, consts, T, P, Y)
    post_phase(ctx, consts, T, "ret", Y, CAT, 0, w["ret_norm_w"], P, 4096)
    XBC = Buf(ctx, P.t[8192:11264, :], "XBC")
    XBC.w, XBC.r = P.w, P.r
    conv_phase(ctx, T, XBC, XBCc, w["conv_w"], w["conv_b"])
    dt_phase(ctx, T, DT, SDT, w["dt_bias"], w["a_log"])
    ssd_scan(ctx, consts, T, XBCc, SDT, Y)
    post_phase(ctx, consts, T, "ssd", Y, CAT, 2048, w["ssd_norm_w"], P, 6144, xs_src=XBCc, dskip=w["d_skip_ch"])
    out_proj(ctx, CAT, 4096, T, w["w_out"], M)
    ln_phase(ctx, consts, XT, M, XB, w["ln1_g"], w["ln1_b"], T)
    swiglu_up(ctx, XB, T, [w["ffn_w1"]], [w["ffn_w3"]], H)
    ffn_down(ctx, H[0], T, w["ffn_w2"], M)
    ln_phase(ctx, consts, XT, M, XB, w["ln2_g"], w["ln2_b"], T, out_final=last_out)


def odd_layer(ctx, consts, T, S, w, j, lbs, last_out=None):
    P, FR, Y, CAT, M, H, XT, XB, GB = (S[k] for k in ("P", "FR", "Y", "CAT", "M", "H", "XT", "XB", "GB"))

    def colmap(cb0):
        if cb0 < 2048:
            return (P, cb0, AF.Silu, "b")
        if cb0 < 4096:
            return (FR, cb0 - 2048, None, "f")
        if cb0 < 6144:
            return (P, cb0 - 2048, None, "b")
        return (P, cb0 - 2048, AF.Silu, "b")
    in_proj(ctx, XB, T, w["w_in"], 8192, colmap)
    hgrn_scan(ctx, consts, T, P, FR, lbs, Y)
    post_phase(ctx, consts, T, "hgrn", Y, CAT, 0, w["hg_norm_w"], P, 4096)
    out_proj(ctx, CAT, 2048, T, w["w_out"], M)
    ln_phase(ctx, consts, XT, M, XB, w["ln1_g"], w["ln1_b"], T)
    router_phase(ctx, consts, T, XT, w["router"], GB)
    swiglu_up(ctx, XB, T, [w["moe_w1"][e] for e in range(NEXP)], [w["moe_w3"][e] for e in range(NEXP)], H, GB=GB)
    moe_down(ctx, H, T, [w["moe_w2"][e] for e in range(NEXP)], M)
    ln_phase(ctx, consts, XT, M, XB, w["ln2_g"], w["ln2_b"], T, out_final=last_out)


WSPEC = [
    ("ev_w_in", [2, D, EV_IN]), ("ev_ret_norm_w", [2, D]), ("ev_conv_w", [2, 4, 3072]), ("ev_conv_b", [2, 3072]),
    ("ev_dt_bias", [2, 32]), ("ev_a_log", [2, 32]), ("ev_d_skip_ch", [2, D]), ("ev_ssd_norm_w", [2, D]),
    ("ev_w_out", [2, 4096, D]), ("ev_ln1_g", [2, D]), ("ev_ln1_b", [2, D]),
    ("ffn_w1", [2, D, FFN]), ("ffn_w3", [2, D, FFN]), ("ffn_w2", [2, FFN, D]), ("ev_ln2_g", [2, D]), ("ev_ln2_b", [2, D]),
    ("od_w_in", [2, D, 8192]), ("hg_lb_logits", [DEPTH, D]), ("od_hg_norm_w", [2, D]), ("od_w_out", [2, D, D]),
    ("od_ln1_g", [2, D]), ("od_ln1_b", [2, D]), ("moe_router", [2, D, NEXP]),
    ("moe_w1", [2, NEXP, D, FFN]), ("moe_w3", [2, NEXP, D, FFN]), ("moe_w2", [2, NEXP, FFN, D]),
    ("od_ln2_g", [2, D]), ("od_ln2_b", [2, D]),
]


def build_program(T, layers=(0, 1, 2, 3), wfilter=None, nl=2):
    nc = bass.Bass("TRN2", target_bir_lowering=False)
    with ExitStack() as es:
        ctx = Ctx(nc, es)
        xin = ctx.dram("xin", [D, T], F32, kind="ExternalInput")
        out = ctx.dram("out", [D, T], F32, kind="ExternalOutput")
        W = {name: nc.dram_tensor(name, ([nl] + shape[1:]) if name != "hg_lb_logits" else shape, F32, kind="ExternalInput").ap()
             for name, shape in WSPEC if (wfilter is None or wfilter(name))}
        S = {
            "XT": ctx.dram("XT", [D, T], F32), "XB": ctx.dram("XB", [D, T], BF16),
            "P": ctx.dram("P", [11264, T], BF16), "DT": ctx.dram("DT", [32, T], F32), "FR": ctx.dram("FR", [D, T], F32),
            "XBCc": ctx.dram("XBCc", [3072, T], BF16), "SDT": ctx.dram("SDT", [64, T], F32),
            "Y": ctx.dram("Y", [D, T], F32), "CAT": ctx.dram("CAT", [4096, T], BF16), "M": ctx.dram("M", [D, T], F32),
            "H": [ctx.dram(f"H{e}", [FFN, T], BF16) for e in range(NEXP)], "GB": ctx.dram("GB", [NEXP, T], F32),
        }
        consts = make_consts(ctx, es)
        lbs = lb_setup(ctx, es, W["hg_lb_logits"])
        ctx.sp.dma([(S["XT"].t[:, :], xin.t[:, :])], S["XT"], reads=[xin], writes=[S["XT"]])
        cast_dram(ctx, S["XB"], xin)
        for li, layer in enumerate(layers):
            j = layer // 2
            last = out if li == len(layers) - 1 else None
            if layer % 2 == 0:
                w = {"w_in": W["ev_w_in"][j], "ret_norm_w": W["ev_ret_norm_w"][j], "conv_w": W["ev_conv_w"][j], "conv_b": W["ev_conv_b"][j],
                     "dt_bias": W["ev_dt_bias"][j], "a_log": W["ev_a_log"][j], "d_skip_ch": W["ev_d_skip_ch"][j], "ssd_norm_w": W["ev_ssd_norm_w"][j],
                     "w_out": W["ev_w_out"][j], "ln1_g": W["ev_ln1_g"][j], "ln1_b": W["ev_ln1_b"][j], "ffn_w1": W["ffn_w1"][j], "ffn_w3": W["ffn_w3"][j],
                     "ffn_w2": W["ffn_w2"][j], "ln2_g": W["ev_ln2_g"][j], "ln2_b": W["ev_ln2_b"][j]}
                even_layer(ctx, consts, T, S, w, j, last_out=last)
            else:
                w = {"w_in": W["od_w_in"][j], "hg_norm_w": W["od_hg_norm_w"][j], "w_out": W["od_w_out"][j], "ln1_g": W["od_ln1_g"][j], "ln1_b": W["od_ln1_b"][j],
                     "router": W["moe_router"][j], "moe_w1": W["moe_w1"][j], "moe_w3": W["moe_w3"][j], "moe_w2": W["moe_w2"][j],
                     "ln2_g": W["od_ln2_g"][j], "ln2_b": W["od_ln2_b"][j]}
                odd_layer(ctx, consts, T, S, w, j, lbs[layer], last_out=last)
        finish(ctx, [out])
    return nc


def host_inputs(inputs, xT):
    m = {"xin": np.ascontiguousarray(xT, dtype=np.float32)}
    for name, shape in WSPEC:
        if name == "ev_d_skip_ch":
            m[name] = np.ascontiguousarray(np.repeat(np.asarray(inputs["ev_d_skip"], dtype=np.float32), 64, axis=1))
        else:
            m[name] = np.ascontiguousarray(np.asarray(inputs[name], dtype=np.float32))
    return m


N_ACTIVE = 4


def kernel(**inputs):
    x = np.asarray(inputs["x"], dtype=np.float32)
    B, T, _ = x.shape
    nc = build_program(T)
    in_maps = []
    for c in range(N_ACTIVE):
        in_maps.append(host_inputs(inputs, x[c % B].T))
    res = run_bass_kernel_spmd(nc, in_maps, core_ids=list(range(N_ACTIVE)))
    outs = [np.ascontiguousarray(res.results[c]["out"].T) for c in range(B)]
    return np.stack(outs, axis=0).astype(np.float32)
```

```python
import math
from contextlib import ExitStack

import numpy as np
import concourse.bass as bass
import concourse.mybir as mybir
from concourse.alu_op_type import AluOpType as ALU
from concourse.bass_utils import run_bass_kernel_spmd

F32 = mybir.dt.float32
BF16 = mybir.dt.bfloat16
I32 = mybir.dt.int32
AF = mybir.ActivationFunctionType
AX = mybir.AxisListType

D = 2048
DEPTH = 4
LN_EPS = 1e-5
ALPHA = (2.0 * DEPTH) ** 0.25
FFN = 5632
NEXP = 8
EV_IN = 11296
SEM_LIMIT = 2000000


class Sem:
    def __init__(self, ctx, name):
        self.h = ctx.es.enter_context(ctx.nc.semaphore(name))
        self.v = 0
        ctx.sems.append(self)


class Buf:
    def __init__(self, ctx, t, name):
        self.ctx = ctx
        self.t = t
        self.name = name
        self.w = {}
        self.r = {}
        self.dsem = None

    def __getitem__(self, idx):
        return self.t[idx]

    def dma_sem(self):
        if self.dsem is None or self.dsem.v >= SEM_LIMIT:
            pool = self.ctx.sem_pool
            self.dsem = pool.pop() if pool else Sem(self.ctx, None)
            self.ctx.dma_bufs.append(self)
        return self.dsem


class Q:
    def __init__(self, ctx, name, eng, self_sync=True):
        self.ctx = ctx
        self.name = name
        self.eng = eng
        self.sem = None
        self.seen = {}
        self.self_sync = self_sync

    def _need(self, sem, val, pend):
        if not self.self_sync and sem is self.sem:
            return
        if self.seen.get(sem, 0) >= val or pend.get(sem, 0) >= val:
            return
        pend[sem] = val

    def _wait(self, sem, val):
        pend = {}
        self._need(sem, val, pend)
        self._emit(pend, None)

    def _emit(self, pend, keep_last):
        items = list(pend.items())
        ret = None
        if keep_last and items:
            ret = items.pop()
        for s, v in items:
            self.eng.wait_ge(s.h, v)
            self.seen[s] = v
        if ret is not None:
            self.seen[ret[0]] = ret[1]
        return ret

    def deps(self, reads, writes, keep_last=False):
        pend = {}
        for b in reads:
            for s, v in b.w.items():
                self._need(s, v, pend)
        for b in writes:
            for s, v in b.w.items():
                self._need(s, v, pend)
            for s, v in b.r.items():
                self._need(s, v, pend)
        return self._emit(pend, keep_last)

    def mark(self, tok, reads, writes):
        s, v = tok
        for b in reads:
            if b.r.get(s, 0) < v:
                b.r[s] = v
        for b in writes:
            if b.w.get(s, 0) < v:
                b.w[s] = v

    def op(self, ins_fn, reads=(), writes=(), single=None):
        if single is None:
            single = self.self_sync
        w = self.deps(reads, writes, keep_last=single)
        self.ctx._first = None
        ins = ins_fn()
        if w is not None:
            ins._wait_ge(w[0].h, w[1])
        if self.sem is None or self.sem.v >= SEM_LIMIT:
            self.sem = Sem(self.ctx, None)
        self.sem.v += 1
        ins.then_inc(self.sem.h, 1)
        self.mark((self.sem, self.sem.v), reads, writes)

    def dma(self, pairs, sb, reads=(), writes=(), **kw):
        w = self.deps(reads, writes, keep_last=True)
        s = sb.dma_sem()
        for out_ap, in_ap in pairs:
            s.v += 16
            ins = self.eng.dma_start(out=out_ap, in_=in_ap, **kw)
            if w is not None:
                ins._wait_ge(w[0].h, w[1])
                w = None
            ins.then_inc(s.h, 16)
        self.mark((s, s.v), reads, writes)


class Ctx:
    def __init__(self, nc, es):
        self.nc = nc
        self.es = es
        self.sems = []
        self.sem_pool = []
        self.dma_bufs = []
        self._first = None
        self.pe = Q(self, "pe", nc.tensor, self_sync=False)
        self.dve = Q(self, "dve", nc.vector)
        self.act = Q(self, "act", nc.scalar)
        self.pool = Q(self, "pool", nc.gpsimd)
        self.sp = Q(self, "sp", nc.sync)
        self.qs = [self.pe, self.dve, self.act, self.pool, self.sp]
        self.uid = 0

    def mm(self, *a, **k):
        ins = self.nc.tensor.matmul(*a, **k)
        if self._first is None:
            self._first = ins
        return ins

    def tr(self, *a, **k):
        ins = self.nc.tensor.transpose(*a, **k)
        if self._first is None:
            self._first = ins
        return ins

    def name(self, base):
        self.uid += 1
        return f"{base}_{self.uid}"

    def sbuf(self, es, shape, dt, name="sb"):
        t = es.enter_context(self.nc.sbuf_tensor(self.name(name), list(shape), dt))
        return Buf(self, t, name)

    def psum(self, es, shape, dt, name="ps"):
        t = es.enter_context(self.nc.psum_tensor(self.name(name), list(shape), dt))
        return Buf(self, t, name)

    def dram(self, name, shape, dt, kind="Internal"):
        t = self.nc.dram_tensor(name, list(shape), dt, kind=kind)
        return Buf(self, t.ap(), name)

    def barrier(self, qs=None):
        for q in (qs or self.qs):
            for s in self.sems:
                if s.v > 0:
                    q._wait(s, s.v)
        for b in self.dma_bufs:
            if b.dsem is not None and b.dsem.v < SEM_LIMIT:
                self.sem_pool.append(b.dsem)
            b.dsem = None
        self.dma_bufs = []


def gemm(ctx, A, K, T, Ws, C, epi_tile, epi_block, TS, WCB, n_par=1, acc=None, ps_bufs=None,
         a_row0=0, w_row0=0, keep_a=None):
    nc = ctx.nc
    KC = K // 128
    NT = TS // 512
    nW = len(Ws)
    with ExitStack() as es:
        a_sb = keep_a if keep_a is not None else ctx.sbuf(es, [128, KC, TS], BF16, "a_sb")
        w_sb = [[ctx.sbuf(es, [128, KC, WCB], BF16, "w_sb") for _ in range(2)] for _ in range(nW)]
        if ps_bufs is None:
            ps_bufs = [[ctx.psum(es, [128, 512], F32, "ps") for _ in range(2)] for _ in range(nW)]
        pi = 0
        wi = 0
        for st in range(T // TS):
            t0 = st * TS
            A_v = A.t[a_row0:a_row0 + K, t0:t0 + TS].rearrange("(kc p) t -> p kc t", p=128)
            g = 4 if KC % 4 == 0 else (2 if KC % 2 == 0 else 1)
            ctx.sp.dma([(a_sb[:, k0:k0 + g, :], A_v[:, k0:k0 + g, :]) for k0 in range(0, KC, g)],
                       a_sb, reads=[A], writes=[a_sb])
            for c0 in range(0, C, WCB):
                cw = min(WCB, C - c0)
                slot = wi % 2
                wi += 1
                for j, W in enumerate(Ws):
                    W_v = W[w_row0:w_row0 + K, c0:c0 + cw].rearrange("(kc p) c -> p kc c", p=128)
                    wb = w_sb[j][slot]
                    h = KC // 2 if KC % 2 == 0 else KC
                    ctx.pool.dma([(wb[:, k0:k0 + h, 0:cw], W_v[:, k0:k0 + h, :]) for k0 in range(0, KC, h)],
                                 wb, reads=[], writes=[wb])
                for m0 in range(0, cw, 128):
                    rows = min(128, cw - m0)
                    cb0 = c0 + m0
                    for nt in range(NT):
                        pss = []
                        for j in range(nW):
                            ps = ps_bufs[j][pi % 2]
                            wb = w_sb[j][slot]

                            def mm(ps=ps, wb=wb):
                                ins = None
                                for kc in range(KC):
                                    ins = ctx.mm(ps[0:rows, :], lhsT=wb[:, kc, m0:m0 + rows],
                                                           rhs=a_sb[:, kc, nt * 512:(nt + 1) * 512],
                                                           start=(kc == 0), stop=(kc == KC - 1))
                                return ins
                            ctx.pe.op(mm, reads=[wb, a_sb], writes=[ps])
                            pss.append(ps)
                        pi += 1
                        epi_tile(pss, cb0, rows, st, nt, t0)
                    if epi_block is not None:
                        epi_block(cb0, rows, st, t0)
        ctx.barrier()


class Stager:
    def __init__(self, ctx, es, TS, dt, n=3, name="stg"):
        self.ctx = ctx
        self.bufs = [ctx.sbuf(es, [128, TS], dt, name) for _ in range(n)]
        self.i = 0

    def cur(self):
        return self.bufs[self.i % len(self.bufs)]

    def next(self):
        self.i += 1


def make_plain_epi(ctx, colmap, stagers, flip=[0]):
    def epi_tile(pss, cb0, rows, st, nt, t0):
        dst, r0, func, key = colmap(cb0)
        sb = stagers[key].cur()
        ps = pss[0]
        sl = slice(nt * 512, (nt + 1) * 512)
        if func is None and flip[0] % 2 == 0:
            ctx.dve.op(lambda: ctx.nc.vector.tensor_copy(out=sb[0:rows, sl], in_=ps[0:rows, :]), reads=[ps], writes=[sb])
        else:
            f = AF.Copy if func is None else func
            ctx.act.op(lambda: ctx.nc.scalar.activation(out=sb[0:rows, sl], in_=ps[0:rows, :], func=f), reads=[ps], writes=[sb])
        flip[0] += 1

    def epi_block(cb0, rows, st, t0):
        dst, r0, func, key = colmap(cb0)
        stg = stagers[key]
        sb = stg.cur()
        TS = sb.t.shape[1]
        ctx.sp.dma([(dst.t[r0:r0 + rows, t0:t0 + TS], sb[0:rows, :])], sb, reads=[sb], writes=[dst])
        stg.next()

    return epi_tile, epi_block


def make_consts(ctx, es):
    nc = ctx.nc
    c = {}
    ones_f = ctx.sbuf(es, [128, 128], F32, "ones_f")
    ctx.pool.op(lambda: nc.gpsimd.memset(ones_f[:], 1.0), writes=[ones_f])
    ones_b = ctx.sbuf(es, [128, 128], BF16, "ones_b")
    ctx.pool.op(lambda: nc.gpsimd.memset(ones_b[:], 1.0), writes=[ones_b])
    ident_f = ctx.sbuf(es, [128, 128], F32, "ident_f")
    ctx.pool.op(lambda: nc.gpsimd.affine_select(out=ident_f[:], in_=ones_f[:], pattern=[[-1, 128]],
                                                compare_op=ALU.is_equal, fill=0.0, base=0, channel_multiplier=1),
                reads=[ones_f], writes=[ident_f])
    ident_b = ctx.sbuf(es, [128, 128], BF16, "ident_b")
    ctx.dve.op(lambda: nc.vector.tensor_copy(out=ident_b[:], in_=ident_f[:]), reads=[ident_f], writes=[ident_b])
    c["ones_f"], c["ones_b"], c["ident_f"], c["ident_b"] = ones_f, ones_b, ident_f, ident_b
    return c


def group_norm_tile(ctx, consts, wk, r, out, NB, gsz, center, nfeat, W=512):
    nc = ctx.nc
    rb, rsq, ps1, ps2, mean, rstd, tmp = wk["rb"], wk["rsq"], wk["ps1"], wk["ps2"], wk["mean"], wk["rstd"], wk["tmp"]
    ones_b = consts["ones_b"]
    inv = 1.0 / nfeat
    if center:
        ctx.act.op(lambda: nc.scalar.activation(out=rb[:, 0:NB, 0:W], in_=r[:, 0:NB, 0:W], func=AF.Copy), reads=[r], writes=[rb])
    ctx.pool.op(lambda: nc.gpsimd.tensor_tensor(out=rsq[:, 0:NB, 0:W], in0=r[:, 0:NB, 0:W], in1=r[:, 0:NB, 0:W], op=ALU.mult),
                reads=[r], writes=[rsq])
    for g0 in range(0, NB, gsz):
        if center:
            def mm1():
                ins = None
                for b in range(g0, g0 + gsz):
                    ins = ctx.mm(ps1[:, 0:W], lhsT=ones_b[:], rhs=rb[:, b, 0:W], start=(b == g0), stop=(b == g0 + gsz - 1))
                return ins
            ctx.pe.op(mm1, reads=[ones_b, rb], writes=[ps1])

        def mm2():
            ins = None
            for b in range(g0, g0 + gsz):
                ins = ctx.mm(ps2[:, 0:W], lhsT=ones_b[:], rhs=rsq[:, b, 0:W], start=(b == g0), stop=(b == g0 + gsz - 1))
            return ins
        ctx.pe.op(mm2, reads=[ones_b, rsq], writes=[ps2])
        if center:
            ctx.act.op(lambda: nc.scalar.activation(out=mean[:, 0:W], in_=ps1[:, 0:W], func=AF.Copy, scale=inv), reads=[ps1], writes=[mean])
            ctx.dve.op(lambda: nc.vector.tensor_tensor(out=tmp[:, 0:W], in0=mean[:, 0:W], in1=mean[:, 0:W], op=ALU.mult), reads=[mean], writes=[tmp])
            ctx.dve.op(lambda: nc.vector.scalar_tensor_tensor(out=tmp[:, 0:W], in0=ps2[:, 0:W], scalar=inv, in1=tmp[:, 0:W],
                                                              op0=ALU.mult, op1=ALU.subtract), reads=[ps2, tmp], writes=[tmp])
            ctx.dve.op(lambda: nc.vector.tensor_scalar(out=tmp[:, 0:W], in0=tmp[:, 0:W], scalar1=0.0, scalar2=LN_EPS, op0=ALU.max, op1=ALU.add),
                       reads=[tmp], writes=[tmp])
        else:
            ctx.dve.op(lambda: nc.vector.tensor_scalar(out=tmp[:, 0:W], in0=ps2[:, 0:W], scalar1=inv, scalar2=LN_EPS, op0=ALU.mult, op1=ALU.add),
                       reads=[ps2], writes=[tmp])
        ctx.act.op(lambda: nc.scalar.activation(out=tmp[:, 0:W], in_=tmp[:, 0:W], func=AF.Sqrt), reads=[tmp], writes=[tmp])
        ctx.dve.op(lambda: nc.vector.reciprocal(out=rstd[:, 0:W], in_=tmp[:, 0:W]), reads=[tmp], writes=[rstd])
        for b in range(g0, g0 + gsz):
            q = ctx.dve if b % 2 == 0 else ctx.pool
            e = nc.vector if b % 2 == 0 else nc.gpsimd
            if center:
                q.op(lambda e=e, b=b: e.tensor_tensor(out=out[:, b, 0:W], in0=r[:, b, 0:W], in1=mean[:, 0:W], op=ALU.subtract), reads=[r, mean], writes=[out])
                q.op(lambda e=e, b=b: e.tensor_tensor(out=out[:, b, 0:W], in0=out[:, b, 0:W], in1=rstd[:, 0:W], op=ALU.mult), reads=[out, rstd], writes=[out])
            else:
                q.op(lambda e=e, b=b: e.tensor_tensor(out=out[:, b, 0:W], in0=r[:, b, 0:W], in1=rstd[:, 0:W], op=ALU.mult), reads=[r, rstd], writes=[out])


def norm_work(ctx, es, NB, W=512):
    return {
        "rb": ctx.sbuf(es, [128, NB, W], BF16, "n_rb"),
        "rsq": ctx.sbuf(es, [128, NB, W], BF16, "n_rsq"),
        "ps1": ctx.psum(es, [128, 512], F32, "n_ps1"),
        "ps2": ctx.psum(es, [128, 512], F32, "n_ps2"),
        "mean": ctx.sbuf(es, [128, W], F32, "n_mean"),
        "rstd": ctx.sbuf(es, [128, W], F32, "n_rstd"),
        "tmp": ctx.sbuf(es, [128, W], F32, "n_tmp"),
    }


def ln_phase(ctx, consts, XT, M, XB, gvec, bvec, T, out_final=None):
    nc = ctx.nc
    NB = D // 128
    with ExitStack() as es:
        wk = norm_work(ctx, es, NB)
        gb = ctx.sbuf(es, [128, 2, NB], F32, "ln_gb")
        ctx.sp.dma([(gb[:, 0, :], gvec.rearrange("(b p) -> p b", p=128)), (gb[:, 1, :], bvec.rearrange("(b p) -> p b", p=128))],
                   gb, writes=[gb], allow_slow_non_contiguous=True)
        xs = [ctx.sbuf(es, [128, NB, 512], F32, "ln_x") for _ in range(2)]
        ms = [ctx.sbuf(es, [128, NB, 512], F32, "ln_m") for _ in range(2)]
        ob = [ctx.sbuf(es, [128, NB, 512], BF16, "ln_ob") for _ in range(2)]
        for nt in range(T // 512):
            x, m, o = xs[nt % 2], ms[nt % 2], ob[nt % 2]
            tsl = slice(nt * 512, (nt + 1) * 512)
            ctx.sp.dma([(x[:, :, :], XT.t[:, tsl].rearrange("(b p) t -> p b t", p=128))], x, reads=[XT], writes=[x])
            ctx.sp.dma([(m[:, :, :], M.t[:, tsl].rearrange("(b p) t -> p b t", p=128))], m, reads=[M], writes=[m])
            ctx.dve.op(lambda: nc.vector.scalar_tensor_tensor(out=m[:, :, :], in0=x[:, :, :], scalar=ALPHA, in1=m[:, :, :], op0=ALU.mult, op1=ALU.add),
                       reads=[x, m], writes=[m])
            group_norm_tile(ctx, consts, wk, m, x, NB, NB, True, D)
            for b in range(NB):
                q = ctx.dve if b % 2 == 0 else ctx.pool
                e = nc.vector if b % 2 == 0 else nc.gpsimd
                q.op(lambda e=e, b=b: e.tensor_scalar(out=x[:, b, :], in0=x[:, b, :], scalar1=gb[:, 0, b:b + 1], scalar2=gb[:, 1, b:b + 1],
                                                     op0=ALU.mult, op1=ALU.add), reads=[x, gb], writes=[x])
            if out_final is not None:
                ctx.sp.dma([(out_final.t[:, tsl].rearrange("(b p) t -> p b t", p=128), x[:, :, :])], x, reads=[x], writes=[out_final])
            else:
                ctx.act.op(lambda: nc.scalar.activation(out=o[:, :, :], in_=x[:, :, :], func=AF.Copy), reads=[x], writes=[o])
                ctx.sp.dma([(XT.t[:, tsl].rearrange("(b p) t -> p b t", p=128), x[:, :, :])], x, reads=[x], writes=[XT])
                ctx.sp.dma([(XB.t[:, tsl].rearrange("(b p) t -> p b t", p=128), o[:, :, :])], o, reads=[o], writes=[XB])
        ctx.barrier()


def swiglu_up(ctx, XB, T, W1s, W3s, H, GB=None):
    nc = ctx.nc
    TS = min(T, 2048)
    with ExitStack() as es:
        stg = Stager(ctx, es, TS, BF16, 3, "h_stg")
        tmps = [ctx.sbuf(es, [128, 512], F32, "sw_tmp") for _ in range(3)]
        gts = [ctx.sbuf(es, [128, TS], F32, "gate_t") for _ in range(2)] if GB is not None else None
        ti = [0]
        for e, (W1, W3) in enumerate(zip(W1s, W3s)):
            cur_g = [None]

            def epi_tile(pss, cb0, rows, st, nt, t0, e=e):
                tmp = tmps[ti[0] % 3]
                ti[0] += 1
                sb = stg.cur()
                sl = slice(nt * 512, (nt + 1) * 512)
                ctx.act.op(lambda: nc.scalar.activation(out=tmp[:, :], in_=pss[0][:, :], func=AF.Silu), reads=[pss[0]], writes=[tmp])
                if GB is not None:
                    if nt == 0 and cb0 == 0:
                        g = gts[(e + st) % 2]
                        ctx.sp.dma([(g[:, :], GB.t[e, t0:t0 + TS].partition_broadcast(128))], g, reads=[GB], writes=[g])
                        cur_g[0] = g
                    g = cur_g[0]
                    ctx.dve.op(lambda: nc.vector.tensor_tensor(out=tmp[:, :], in0=tmp[:, :], in1=pss[1][:, :], op=ALU.mult), reads=[tmp, pss[1]], writes=[tmp])
                    ctx.dve.op(lambda: nc.vector.tensor_tensor(out=sb[:, sl], in0=tmp[:, :], in1=g[:, sl], op=ALU.mult), reads=[tmp, g], writes=[sb])
                else:
                    ctx.dve.op(lambda: nc.vector.tensor_tensor(out=sb[:, sl], in0=tmp[:, :], in1=pss[1][:, :], op=ALU.mult), reads=[tmp, pss[1]], writes=[sb])

            def epi_block(cb0, rows, st, t0, e=e):
                sb = stg.cur()
                ctx.sp.dma([(H[e].t[cb0:cb0 + rows, t0:t0 + TS], sb[0:rows, :])], sb, reads=[sb], writes=[H[e]])
                stg.next()

            gemm(ctx, XB, D, T, [W1, W3], FFN, epi_tile, epi_block, TS, 256)


def cast_dram(ctx, dst, src):
    ctx.pool.dma([(dst.t[:, :], src.t[:, :])], dst, reads=[src], writes=[dst])


def ffn_down(ctx, H, T, W2, M, K=FFN):
    with ExitStack() as es:
        stg = Stager(ctx, es, 512, F32, 4, "m_stg")
        et, eb = make_plain_epi(ctx, lambda cb0: (M, cb0, None, "m"), {"m": stg})
        gemm(ctx, H, K, T, [W2], D, et, eb, 512, 256)


def finish(ctx, bufs):
    ctx.barrier()


def all_gather(ctx, dst, src, n_cores):
    q = ctx.pool
    q.deps([src], [dst])
    if getattr(ctx, "cc_sem", None) is None or ctx.cc_sem.v >= SEM_LIMIT:
        ctx.cc_sem = Sem(ctx, None)
    s = ctx.cc_sem
    s.v += 1
    ctx.nc.gpsimd.collective_compute("AllGather", ALU.bypass, replica_groups=[list(range(n_cores))],
                                     ins=[src.t.opt()], outs=[dst.t.opt()]).then_inc(s.h)
    q.mark((s, s.v), [src], [dst])


SEG = 512
CH = 32
NCH = SEG // CH


def gla_scan(ctx, consts, T, nheads, dv, Y, seg_setup, prologue, nslots=3, scalar_decay=False, CH=32, G=2):
    nc = ctx.nc
    NCH = SEG // CH
    mid = CH // 2 - 1
    nvb = (dv + 127) // 128
    dvb = min(dv, 128)
    with ExitStack() as es:
        m0 = ctx.sbuf(es, [128, NCH, CH], F32, "m0")
        ctx.pool.op(lambda: nc.gpsimd.memset(m0[:, :, :], 1.0), writes=[m0])
        ctx.pool.op(lambda: nc.gpsimd.memset(m0[:, :, 0:1], 0.0), writes=[m0])
        cm = ctx.sbuf(es, [CH, NCH, CH], F32, "cm")
        ctx.pool.op(lambda: nc.gpsimd.memset(cm[:, :, :], 1.0), writes=[cm])
        ctx.pool.op(lambda: nc.gpsimd.affine_select(out=cm[:, :, :], in_=cm[:, :, :], pattern=[[0, NCH], [1, CH]],
                                                    compare_op=ALU.is_ge, fill=0.0, base=0, channel_multiplier=-1),
                    reads=[cm], writes=[cm])
        if scalar_decay:
            idm = ctx.sbuf(es, [CH, NCH, CH], F32, "idm")
            ctx.pool.op(lambda: nc.gpsimd.memset(idm[:, :, :], 1.0), writes=[idm])
            ctx.pool.op(lambda: nc.gpsimd.affine_select(out=idm[:, :, :], in_=idm[:, :, :], pattern=[[0, NCH], [1, CH]],
                                                        compare_op=ALU.is_equal, fill=0.0, base=0, channel_multiplier=-1), reads=[idm], writes=[idm])
            ngm = ctx.sbuf(es, [CH, NCH, CH], F32, "ngm")
            ctx.pool.op(lambda: nc.gpsimd.memset(ngm[:, :, :], 0.0), writes=[ngm])
            ctx.pool.op(lambda: nc.gpsimd.affine_select(out=ngm[:, :, :], in_=ngm[:, :, :], pattern=[[0, NCH], [1, CH]],
                                                        compare_op=ALU.is_ge, fill=-1.0e30, base=0, channel_multiplier=-1), reads=[ngm], writes=[ngm])
        S = ctx.sbuf(es, [128, nheads, dv], F32, "S")
        Sb = ctx.sbuf(es, [128, nheads, dv], BF16, "Sb")
        Sh = [Buf(ctx, S.t, "S_h") for _ in range(nheads)]
        Sbh = [Buf(ctx, Sb.t, "Sb_h") for _ in range(nheads)]
        ctx.pool.op(lambda: nc.gpsimd.memset(S[:, :, :], 0.0), writes=Sh)
        ctx.pool.op(lambda: nc.gpsimd.memset(Sb[:, :, :], 0.0), writes=Sbh)
        slots = []
        for i in range(nslots):
            sl = {
                "qf": ctx.sbuf(es, [128, SEG], F32, "qf"), "kf": ctx.sbuf(es, [128, SEG], F32, "kf"),
                "gf": ctx.sbuf(es, [128, SEG], F32, "gf"), "cum": ctx.sbuf(es, [128, NCH, CH], F32, "cum"),
                "t1": ctx.sbuf(es, [128, NCH, CH], F32, "t1"), "e": ctx.sbuf(es, [128, NCH, CH], F32, "e"),
                "qt": ctx.sbuf(es, [128, SEG], BF16, "qt"), "kt": ctx.sbuf(es, [128, SEG], BF16, "kt"),
                "qh": ctx.sbuf(es, [128, SEG], BF16, "qh"), "kd": ctx.sbuf(es, [128, SEG], BF16, "kd"),
                "dcol": ctx.sbuf(es, [128, NCH], F32, "dcol"),
                "dm": ctx.sbuf(es, [CH, NCH, CH], F32, "dm"), "col": ctx.sbuf(es, [CH, NCH], F32, "col"),
                "vT": [ctx.sbuf(es, [dvb, SEG], BF16, "vT") for _ in range(nvb)],
                "vtok": ctx.sbuf(es, [CH, NCH, dv], BF16, "vtok"), "kdtok": ctx.sbuf(es, [CH, NCH, 128], BF16, "kdtok"),
                "sc": ctx.sbuf(es, [CH, SEG], BF16, "sc"),
                "ysb": [ctx.sbuf(es, [dvb, SEG], F32, "ysb") for _ in range(nvb)],
                "aux": [ctx.sbuf(es, [128, SEG], F32, "aux") for _ in range(2)],
                "auxb": [ctx.sbuf(es, [128, SEG], BF16, "auxb") for _ in range(2)],
            }
            slots.append(sl)
        rem = 8 - G * nvb - 1
        n_sc = 2 if rem >= 4 else 1
        n_u = max(1, min(G, rem - n_sc))
        ps_sc = [ctx.psum(es, [CH, SEG], F32, "ps_sc") for _ in range(n_sc)]
        ps_y = [[ctx.psum(es, [dvb, SEG], F32, "ps_y") for _ in range(nvb)] for _ in range(G)]
        ps_tr = ctx.psum(es, [CH, 1024], BF16, "ps_tr")
        ps_u = [ctx.psum(es, [128, 512], F32, "ps_u") for _ in range(n_u)]
        ps_misc = ps_u[0]
        ident_b = consts["ident_b"]
        si = 0
        for seg in range(T // SEG):
            t0 = seg * SEG
            seg_setup(seg, t0)
            for h0 in range(0, nheads, G):
                hs = list(range(h0, min(h0 + G, nheads)))
                def head_gen(h, sl):
                    yield from prologue(h, seg, t0, sl, ps_misc)
                    qf, kf, gf, cum, t1, e = sl["qf"], sl["kf"], sl["gf"], sl["cum"], sl["t1"], sl["e"]
                    cumf = cum.t.rearrange("p c t -> p (c t)")
                    ctx.dve.op(lambda: nc.vector.tensor_tensor_scan(out=cumf, data0=m0.t.rearrange("p c t -> p (c t)"), data1=gf[:, :],
                                                                    initial=0.0, op0=ALU.mult, op1=ALU.add), reads=[m0, gf], writes=[cum])
                    yield
                    if scalar_decay:
                        dm, col = sl["dm"], sl["col"]
                        ctx.dve.op(lambda: nc.vector.tensor_tensor(out=dm[:, :, :], in0=cum[0:CH, :, :], in1=idm[:, :, :], op=ALU.mult), reads=[cum, idm], writes=[dm])
                        yield
                        ctx.dve.op(lambda: nc.vector.tensor_reduce(out=col[:, :], in_=dm[:, :, :], axis=AX.X, op=ALU.add), reads=[dm], writes=[col])
                        yield
                        ctx.dve.op(lambda: nc.vector.tensor_tensor(out=dm[:, :, :], in0=cum[0:CH, :, :], in1=col[:, :].unsqueeze(2).to_broadcast([CH, NCH, CH]),
                                                                   op=ALU.subtract), reads=[cum, col], writes=[dm])
                        yield
                        ctx.dve.op(lambda: nc.vector.tensor_tensor(out=dm[:, :, :], in0=dm[:, :, :], in1=ngm[:, :, :], op=ALU.min), reads=[dm, ngm], writes=[dm])
                        yield
                        ctx.act.op(lambda: nc.scalar.activation(out=dm[:, :, :], in_=dm[:, :, :], func=AF.Exp), reads=[dm], writes=[dm])
                        yield
                        ctx.act.op(lambda: nc.scalar.activation(out=sl["qt"][:, :], in_=qf[:, :], func=AF.Copy), reads=[qf], writes=[sl["qt"]])
                        yield
                        ctx.pool.op(lambda: nc.gpsimd.tensor_copy(out=sl["kt"][:, :], in_=kf[:, :]), reads=[kf], writes=[sl["kt"]])
                        yield
                    else:
                        ctx.dve.op(lambda: nc.vector.tensor_tensor(out=t1[:, :, :], in0=cum[:, :, :], in1=cum[:, :, mid:mid + 1].to_broadcast([128, NCH, CH]),
                                                                   op=ALU.subtract), reads=[cum], writes=[t1])
                        yield
                        ctx.act.op(lambda: nc.scalar.activation(out=e[:, :, :], in_=t1[:, :, :], func=AF.Exp), reads=[t1], writes=[e])
                        yield
                        ctx.dve.op(lambda: nc.vector.tensor_tensor(out=sl["qt"][:, :], in0=qf[:, :], in1=e.t.rearrange("p c t -> p (c t)"), op=ALU.mult),
                                   reads=[qf, e], writes=[sl["qt"]])
                        yield
                        ctx.act.op(lambda: nc.scalar.activation(out=e[:, :, :], in_=t1[:, :, :], func=AF.Exp, scale=-1.0), reads=[t1], writes=[e])
                        yield
                        ctx.pool.op(lambda: nc.gpsimd.tensor_tensor(out=sl["kt"][:, :], in0=kf[:, :], in1=e.t.rearrange("p c t -> p (c t)"), op=ALU.mult),
                                    reads=[kf, e], writes=[sl["kt"]])
                        yield
                    ctx.act.op(lambda: nc.scalar.activation(out=e[:, :, :], in_=cum[:, :, :], func=AF.Exp), reads=[cum], writes=[e])
                    yield
                    ctx.dve.op(lambda: nc.vector.tensor_tensor(out=sl["qh"][:, :], in0=qf[:, :], in1=e.t.rearrange("p c t -> p (c t)"), op=ALU.mult),
                               reads=[qf, e], writes=[sl["qh"]])
                    yield
                    ctx.act.op(lambda: nc.scalar.activation(out=sl["dcol"][:, :], in_=cum[:, :, CH - 1], func=AF.Exp), reads=[cum], writes=[sl["dcol"]])
                    yield
                    ctx.pool.op(lambda: nc.gpsimd.tensor_tensor(out=t1[:, :, :], in0=cum[:, :, :], in1=cum[:, :, CH - 1:CH].to_broadcast([128, NCH, CH]),
                                                                op=ALU.subtract), reads=[cum], writes=[t1])
                    yield
                    ctx.act.op(lambda: nc.scalar.activation(out=e[:, :, :], in_=t1[:, :, :], func=AF.Exp, scale=-1.0), reads=[t1], writes=[e])
                    yield
                    ctx.pool.op(lambda: nc.gpsimd.tensor_tensor(out=sl["kd"][:, :], in0=kf[:, :], in1=e.t.rearrange("p c t -> p (c t)"), op=ALU.mult),
                                reads=[kf, e], writes=[sl["kd"]])
                    yield
                    per = max(1, min(NCH, 1024 // dv))
                    kg = min(NCH, 8)
                    for c0 in range(0, NCH, per):
                        def trv(c0=c0):
                            ins = None
                            for c in range(c0, c0 + per):
                                for vb in range(nvb):
                                    o0 = (c - c0) * dv + vb * 128
                                    ins = ctx.tr(out=ps_tr[0:CH, o0:o0 + dvb], in_=sl["vT"][vb][0:dvb, c * CH:(c + 1) * CH],
                                                              identity=ident_b[0:dvb, 0:dvb])
                            return ins
                        ctx.pe.op(trv, reads=sl["vT"] + [ident_b], writes=[ps_tr])
                        ctx.act.op(lambda c0=c0: nc.scalar.activation(out=sl["vtok"][:, c0:c0 + per, :], in_=ps_tr.t[0:CH, 0:per * dv].rearrange("p (c d) -> p c d", d=dv),
                                                                      func=AF.Copy), reads=[ps_tr], writes=[sl["vtok"]])
                        yield
                    for c0 in range(0, NCH, kg):
                        def trk(c0=c0):
                            ins = None
                            for c in range(c0, c0 + kg):
                                ins = ctx.tr(out=ps_tr[0:CH, (c - c0) * 128:(c - c0 + 1) * 128], in_=sl["kd"][:, c * CH:(c + 1) * CH],
                                                          identity=ident_b[:, :])
                            return ins
                        ctx.pe.op(trk, reads=[sl["kd"], ident_b], writes=[ps_tr])
                        ctx.dve.op(lambda c0=c0: nc.vector.tensor_copy(out=sl["kdtok"][:, c0:c0 + kg, :], in_=ps_tr.t[0:CH, 0:kg * 128].rearrange("p (c d) -> p c d", d=128)),
                                   reads=[ps_tr], writes=[sl["kdtok"]])
                        yield
                sls = []
                gens = []
                for h in hs:
                    sl = slots[si % nslots]
                    si += 1
                    sls.append(sl)
                    gens.append(head_gen(h, sl))
                while gens:
                    for gg in list(gens):
                        try:
                            next(gg)
                        except StopIteration:
                            gens.remove(gg)
                for gi, h in enumerate(hs):
                    sl = sls[gi]

                    def scm(sl=sl, gi=gi):
                        ins = None
                        for c in range(NCH):
                            cs = slice(c * CH, (c + 1) * CH)
                            ins = ctx.mm(ps_sc[gi % n_sc][0:CH, cs], lhsT=sl["kt"][:, cs], rhs=sl["qt"][:, cs], start=True, stop=True)
                        return ins
                    ctx.pe.op(scm, reads=[sl["kt"], sl["qt"]], writes=[ps_sc[gi % n_sc]])
                    mkb = sl["dm"] if scalar_decay else cm
                    ctx.dve.op(lambda sl=sl, gi=gi, mkb=mkb: nc.vector.tensor_tensor(out=sl["sc"][:, :], in0=ps_sc[gi % n_sc][:, :], in1=mkb.t.rearrange("p c t -> p (c t)"), op=ALU.mult),
                               reads=[ps_sc[gi % n_sc], mkb], writes=[sl["sc"]])
                for c in range(NCH):
                    cs = slice(c * CH, (c + 1) * CH)
                    for gi, h in enumerate(hs):
                        sl = sls[gi]

                        def ymm(sl=sl, gi=gi, h=h):
                            ins = None
                            for vb in range(nvb):
                                vs = slice(vb * 128, vb * 128 + dvb)
                                ctx.mm(ps_y[gi][vb][0:dvb, cs], lhsT=sl["vtok"][0:CH, c, vs], rhs=sl["sc"][0:CH, cs], start=True, stop=False)
                                ins = ctx.mm(ps_y[gi][vb][0:dvb, cs], lhsT=Sb[:, h, vs], rhs=sl["qh"][:, cs], start=False, stop=True)
                            return ins
                        ctx.pe.op(ymm, reads=[sl["vtok"], sl["sc"], Sbh[h], sl["qh"]], writes=ps_y[gi])
                        pu = ps_u[gi % n_u]
                        ctx.pe.op(lambda sl=sl, pu=pu: ctx.mm(pu[:, 0:dv], lhsT=sl["kdtok"][0:CH, c, :], rhs=sl["vtok"][0:CH, c, :], start=True, stop=True),
                                  reads=[sl["kdtok"], sl["vtok"]], writes=[pu])
                        ctx.dve.op(lambda sl=sl, h=h, pu=pu: nc.vector.scalar_tensor_tensor(out=Sb[:, h, :], in0=S[:, h, :], scalar=sl["dcol"][:, c:c + 1], in1=pu[:, 0:dv],
                                                                                           op0=ALU.mult, op1=ALU.add), reads=[Sh[h], sl["dcol"], pu], writes=[Sbh[h]])
                        ctx.dve.op(lambda sl=sl, h=h, pu=pu: nc.vector.scalar_tensor_tensor(out=S[:, h, :], in0=S[:, h, :], scalar=sl["dcol"][:, c:c + 1], in1=pu[:, 0:dv],
                                                                                           op0=ALU.mult, op1=ALU.add), reads=[Sh[h], sl["dcol"], pu], writes=[Sh[h]])
                for gi, h in enumerate(hs):
                    sl = sls[gi]
                    for vb in range(nvb):
                        ctx.act.op(lambda sl=sl, gi=gi, vb=vb: nc.scalar.activation(out=sl["ysb"][vb][:, :], in_=ps_y[gi][vb][:, :], func=AF.Copy),
                                   reads=[ps_y[gi][vb]], writes=[sl["ysb"][vb]])
                        r0 = h * dv + vb * 128
                        ctx.sp.dma([(Y.t[r0:r0 + dvb, t0:t0 + SEG], sl["ysb"][vb][:, :])], sl["ysb"][vb], reads=[sl["ysb"][vb]], writes=[Y])
        ctx.barrier()


def lb_setup(ctx, es, logits_ap):
    nc = ctx.nc
    lg = ctx.sbuf(es, [128, 16, DEPTH], F32, "lb_lg")
    ctx.sp.dma([(lg[:, :, l], logits_ap[l].rearrange("(b p) -> p b", p=128)) for l in range(DEPTH)], lg, writes=[lg], allow_slow_non_contiguous=True)
    ctx.act.op(lambda: nc.scalar.activation(out=lg[:, :, :], in_=lg[:, :, :], func=AF.Exp), reads=[lg], writes=[lg])
    s = ctx.sbuf(es, [128, 16], F32, "lb_s")
    ctx.dve.op(lambda: nc.vector.tensor_reduce(out=s[:, :], in_=lg[:, :, :], axis=AX.X, op=ALU.add), reads=[lg], writes=[s])
    ctx.dve.op(lambda: nc.vector.reciprocal(out=s[:, :], in_=s[:, :]), reads=[s], writes=[s])
    res = {}
    for layer in (1, 3):
        lb = ctx.sbuf(es, [128, 16], F32, "lb")
        oml = ctx.sbuf(es, [128, 16], F32, "oml")
        noml = ctx.sbuf(es, [128, 16], F32, "noml")
        ctx.dve.op(lambda: nc.vector.tensor_copy(out=lb[:, :], in_=lg[:, :, 1]), reads=[lg], writes=[lb])
        for l in range(2, layer + 1):
            ctx.dve.op(lambda l=l: nc.vector.tensor_tensor(out=lb[:, :], in0=lb[:, :], in1=lg[:, :, l], op=ALU.add), reads=[lb, lg], writes=[lb])
        ctx.dve.op(lambda: nc.vector.tensor_tensor(out=lb[:, :], in0=lb[:, :], in1=s[:, :], op=ALU.mult), reads=[lb, s], writes=[lb])
        ctx.dve.op(lambda: nc.vector.tensor_scalar(out=oml[:, :], in0=lb[:, :], scalar1=-1.0, scalar2=1.0, op0=ALU.mult, op1=ALU.add), reads=[lb], writes=[oml])
        ctx.dve.op(lambda: nc.vector.tensor_scalar(out=noml[:, :], in0=oml[:, :], scalar1=-1.0, scalar2=None, op0=ALU.mult), reads=[oml], writes=[noml])
        res[layer] = (lb, oml, noml)
    return res


def conv_phase(ctx, T, XBC, XBCc, conv_w, conv_b):
    nc = ctx.nc
    NBK = 3072 // 128
    with ExitStack() as es:
        w = ctx.sbuf(es, [128, NBK, 4], F32, "cv_w")
        b = ctx.sbuf(es, [128, NBK], F32, "cv_b")
        ctx.sp.dma([(w[:, :, jj], conv_w[jj].rearrange("(b p) -> p b", p=128)) for jj in range(4)], w, writes=[w], allow_slow_non_contiguous=True)
        ctx.sp.dma([(b[:, :], conv_b.rearrange("(b p) -> p b", p=128))], b, writes=[b], allow_slow_non_contiguous=True)
        xps = [ctx.sbuf(es, [128, T + 3], BF16, "cv_x") for _ in range(2)]
        accs = [ctx.sbuf(es, [128, T], F32, "cv_a") for _ in range(2)]
        outs = [ctx.sbuf(es, [128, T], BF16, "cv_o") for _ in range(2)]
        for xp in xps:
            ctx.pool.op(lambda xp=xp: nc.gpsimd.memset(xp[:, 0:3], 0.0), writes=[xp])
        for blk in range(NBK):
            xp, acc, o = xps[blk % 2], accs[blk % 2], outs[blk % 2]
            ctx.sp.dma([(xp[:, 3:T + 3], XBC.t[blk * 128:(blk + 1) * 128, :])], xp, reads=[XBC], writes=[xp])
            ctx.dve.op(lambda: nc.vector.tensor_scalar(out=acc[:, :], in0=xp[:, 0:T], scalar1=w[:, blk, 0:1], scalar2=b[:, blk:blk + 1], op0=ALU.mult, op1=ALU.add),
                       reads=[xp, w, b], writes=[acc])
            for j in range(1, 4):
                ctx.dve.op(lambda j=j: nc.vector.scalar_tensor_tensor(out=acc[:, :], in0=xp[:, j:j + T], scalar=w[:, blk, j:j + 1], in1=acc[:, :], op0=ALU.mult, op1=ALU.add),
                           reads=[xp, w, acc], writes=[acc])
            ctx.act.op(lambda: nc.scalar.activation(out=o[:, :], in_=acc[:, :], func=AF.Silu), reads=[acc], writes=[o])
            ctx.sp.dma([(XBCc.t[blk * 128:(blk + 1) * 128, :], o[:, :])], o, reads=[o], writes=[XBCc])
        ctx.barrier()


def dt_phase(ctx, T, DT, SDT, dt_bias, a_log):
    nc = ctx.nc
    with ExitStack() as es:
        x = ctx.sbuf(es, [32, T], F32, "dt_x")
        y = ctx.sbuf(es, [32, T], F32, "dt_y")
        pb = ctx.sbuf(es, [32, 2], F32, "dt_pb")
        ctx.sp.dma([(pb[:, 0:1], dt_bias.rearrange("(h o) -> h o", o=1)), (pb[:, 1:2], a_log.rearrange("(h o) -> h o", o=1))], pb, writes=[pb],
                   allow_slow_non_contiguous=True)
        ctx.sp.dma([(x[:, :], DT.t[:, :])], x, reads=[DT], writes=[x])
        ctx.act.op(lambda: nc.scalar.activation(out=pb[:, 1:2], in_=pb[:, 1:2], func=AF.Exp), reads=[pb], writes=[pb])
        ctx.dve.op(lambda: nc.vector.tensor_scalar(out=pb[:, 1:2], in0=pb[:, 1:2], scalar1=-1.0, scalar2=None, op0=ALU.mult), reads=[pb], writes=[pb])
        ctx.act.op(lambda: nc.scalar.activation(out=x[:, :], in_=x[:, :], func=AF.Exp, bias=pb[:, 0:1]), reads=[x, pb], writes=[x])
        ctx.act.op(lambda: nc.scalar.activation(out=x[:, :], in_=x[:, :], func=AF.Ln, bias=1.0), reads=[x], writes=[x])
        ctx.dve.op(lambda: nc.vector.tensor_scalar(out=y[:, :], in0=x[:, :], scalar1=pb[:, 1:2], scalar2=None, op0=ALU.mult), reads=[x, pb], writes=[y])
        ctx.sp.dma([(SDT.t[0:32, :], x[:, :])], x, reads=[x], writes=[SDT])
        ctx.sp.dma([(SDT.t[32:64, :], y[:, :])], y, reads=[y], writes=[SDT])
        ctx.barrier()


def ret_scan(ctx, consts, T, P, Y):
    nc = ctx.nc
    with ExitStack() as es:
        ones_f = consts["ones_f"]
        ra = ctx.sbuf(es, [128, 128], F32, "rot_a")
        rb_ = ctx.sbuf(es, [128, 128], F32, "rot_b")
        rot = ctx.sbuf(es, [128, 128], BF16, "rot")
        ctx.pool.op(lambda: nc.gpsimd.affine_select(out=ra[:, :], in_=ones_f[:, :], pattern=[[-1, 128]], compare_op=ALU.is_equal, fill=0.0, base=64, channel_multiplier=1),
                    reads=[ones_f], writes=[ra])
        ctx.pool.op(lambda: nc.gpsimd.affine_select(out=rb_[:, :], in_=ones_f[:, :], pattern=[[-1, 128]], compare_op=ALU.is_equal, fill=0.0, base=-64, channel_multiplier=1),
                    reads=[ones_f], writes=[rb_])
        ctx.dve.op(lambda: nc.vector.tensor_tensor(out=rot[:, :], in0=ra[:, :], in1=rb_[:, :], op=ALU.subtract), reads=[ra, rb_], writes=[rot])
        pidx = ctx.sbuf(es, [128, 1], F32, "pidx")
        for half in range(2):
            ctx.pool.op(lambda half=half: nc.gpsimd.iota(pidx[half * 64:(half + 1) * 64, :], pattern=[[0, 1]], base=0, channel_multiplier=1,
                                                         allow_small_or_imprecise_dtypes=True), writes=[pidx])
        ifr = ctx.sbuf(es, [128, 1], F32, "ifr")
        ctx.act.op(lambda: nc.scalar.activation(out=ifr[:, :], in_=pidx[:, :], func=AF.Exp, scale=-math.log(10000.0) / 64.0), reads=[pidx], writes=[ifr])
        ctx.dve.op(lambda: nc.vector.tensor_scalar(out=ifr[:, :], in0=ifr[:, :], scalar1=1.0 / (2.0 * math.pi), scalar2=None, op0=ALU.mult), reads=[ifr], writes=[ifr])
        pos = ctx.sbuf(es, [128, SEG], F32, "pos")
        fr = ctx.sbuf(es, [128, SEG], F32, "fr")
        fi = ctx.sbuf(es, [128, SEG], I32, "fi")
        ff = ctx.sbuf(es, [128, SEG], F32, "ff")
        msk = ctx.sbuf(es, [128, SEG], F32, "msk")
        tabs = {k: ctx.sbuf(es, [128, SEG], F32, "tab_" + k) for k in ("cos", "sin", "cosk", "sink")}
        tb = Buf(ctx, None, "tabs")
        ks = 128.0 ** -0.5

        def wrap(src):
            ctx.dve.op(lambda: nc.vector.tensor_scalar(out=msk[:, :], in0=src[:, :], scalar1=0.5, scalar2=None, op0=ALU.is_gt), reads=[src], writes=[msk])
            ctx.dve.op(lambda: nc.vector.tensor_tensor(out=src[:, :], in0=src[:, :], in1=msk[:, :], op=ALU.subtract), reads=[src, msk], writes=[src])
            ctx.dve.op(lambda: nc.vector.tensor_scalar(out=msk[:, :], in0=src[:, :], scalar1=-0.5, scalar2=None, op0=ALU.is_lt), reads=[src], writes=[msk])
            ctx.dve.op(lambda: nc.vector.tensor_tensor(out=src[:, :], in0=src[:, :], in1=msk[:, :], op=ALU.add), reads=[src, msk], writes=[src])

        def seg_setup(seg, t0):
            ctx.pool.op(lambda: nc.gpsimd.iota(pos[:, :], pattern=[[1, SEG]], base=t0, channel_multiplier=0, allow_small_or_imprecise_dtypes=True),
                        reads=[], writes=[pos])
            ctx.dve.op(lambda: nc.vector.tensor_scalar(out=fr[:, :], in0=pos[:, :], scalar1=ifr[:, 0:1], scalar2=None, op0=ALU.mult), reads=[pos, ifr], writes=[fr])
            ctx.dve.op(lambda: nc.vector.tensor_copy(out=fi[:, :], in_=fr[:, :]), reads=[fr], writes=[fi])
            ctx.dve.op(lambda: nc.vector.tensor_copy(out=ff[:, :], in_=fi[:, :]), reads=[fi], writes=[ff])
            ctx.dve.op(lambda: nc.vector.tensor_tensor(out=fr[:, :], in0=fr[:, :], in1=ff[:, :], op=ALU.subtract), reads=[fr, ff], writes=[fr])
            wrap(fr)
            ctx.act.op(lambda: nc.scalar.activation(out=tabs["sin"][:, :], in_=fr[:, :], func=AF.Sin, scale=2.0 * math.pi), reads=[fr], writes=[tabs["sin"], tb])
            ctx.dve.op(lambda: nc.vector.tensor_scalar(out=ff[:, :], in0=fr[:, :], scalar1=0.25, scalar2=None, op0=ALU.add), reads=[fr], writes=[ff])
            wrap(ff)
            ctx.act.op(lambda: nc.scalar.activation(out=tabs["cos"][:, :], in_=ff[:, :], func=AF.Sin, scale=2.0 * math.pi), reads=[ff], writes=[tabs["cos"], tb])
            ctx.dve.op(lambda: nc.vector.tensor_scalar(out=tabs["sink"][:, :], in0=tabs["sin"][:, :], scalar1=ks, scalar2=None, op0=ALU.mult), reads=[tabs["sin"]], writes=[tabs["sink"], tb])
            ctx.dve.op(lambda: nc.vector.tensor_scalar(out=tabs["cosk"][:, :], in0=tabs["cos"][:, :], scalar1=ks, scalar2=None, op0=ALU.mult), reads=[tabs["cos"]], writes=[tabs["cosk"], tb])

        def prologue(h, seg, t0, sl, ps_misc):
            qa, ka = sl["auxb"]
            ts = slice(t0, t0 + SEG)
            ctx.sp.dma([(qa[:, :], P.t[h * 128:(h + 1) * 128, ts])], qa, reads=[P], writes=[qa])
            yield
            ctx.sp.dma([(ka[:, :], P.t[1024 + h * 128:1024 + (h + 1) * 128, ts])], ka, reads=[P], writes=[ka])
            yield
            for vb in range(2):
                r0 = 2048 + h * 256 + vb * 128
                ctx.sp.dma([(sl["vT"][vb][:, :], P.t[r0:r0 + 128, ts])], sl["vT"][vb], reads=[P], writes=[sl["vT"][vb]])
                yield
            lg = math.log1p(-2.0 ** (-5.0 - h))
            ctx.pool.op(lambda: nc.gpsimd.memset(sl["gf"][:, :], lg), writes=[sl["gf"]])
            yield
            for src, dst, c_, s_ in ((qa, sl["qf"], "cos", "sin"), (ka, sl["kf"], "cosk", "sink")):
                a0 = sl["aux"][0]
                ctx.pe.op(lambda src=src: ctx.mm(ps_misc[:, :], lhsT=rot[:, :], rhs=src[:, :], start=True, stop=True), reads=[rot, src], writes=[ps_misc])
                ctx.dve.op(lambda: nc.vector.tensor_tensor(out=a0[:, :], in0=ps_misc[:, :], in1=tabs[s_][:, :], op=ALU.mult), reads=[ps_misc, tb], writes=[a0])
                yield
                ctx.pool.op(lambda: nc.gpsimd.tensor_tensor(out=dst[:, :], in0=src[:, :], in1=tabs[c_][:, :], op=ALU.mult), reads=[src, tb], writes=[dst])
                yield
                ctx.dve.op(lambda: nc.vector.tensor_tensor(out=dst[:, :], in0=dst[:, :], in1=a0[:, :], op=ALU.add), reads=[dst, a0], writes=[dst])
                yield

        gla_scan(ctx, consts, T, 8, 256, Y, seg_setup, prologue, CH=128, G=2, nslots=4)


def ssd_scan(ctx, consts, T, XBCc, SDT, Y):
    nc = ctx.nc

    def seg_setup(seg, t0):
        pass

    def prologue(h, seg, t0, sl, ps_misc):
        g = h // 8
        ts = slice(t0, t0 + SEG)
        ba, ca = sl["auxb"]
        dtb = sl["aux"][0]
        ctx.sp.dma([(ba[:, :], XBCc.t[2048 + g * 128:2048 + (g + 1) * 128, ts])], ba, reads=[XBCc], writes=[ba])
        yield
        ctx.sp.dma([(ca[:, :], XBCc.t[2560 + g * 128:2560 + (g + 1) * 128, ts])], ca, reads=[XBCc], writes=[ca])
        yield
        ctx.sp.dma([(sl["vT"][0][:, :], XBCc.t[h * 64:(h + 1) * 64, ts])], sl["vT"][0], reads=[XBCc], writes=[sl["vT"][0]])
        yield
        ctx.sp.dma([(dtb[:, :], SDT.t[h, ts].partition_broadcast(128))], dtb, reads=[SDT], writes=[dtb])
        yield
        ctx.sp.dma([(sl["gf"][:, :], SDT.t[32 + h, ts].partition_broadcast(128))], sl["gf"], reads=[SDT], writes=[sl["gf"]])
        yield
        ctx.pool.op(lambda: nc.gpsimd.tensor_tensor(out=sl["kf"][:, :], in0=ba[:, :], in1=dtb[:, :], op=ALU.mult), reads=[ba, dtb], writes=[sl["kf"]])
        yield
        ctx.act.op(lambda: nc.scalar.activation(out=sl["qf"][:, :], in_=ca[:, :], func=AF.Copy), reads=[ca], writes=[sl["qf"]])
        yield

    gla_scan(ctx, consts, T, 32, 64, Y, seg_setup, prologue, scalar_decay=True, CH=128, G=4, nslots=5)


def hgrn_scan(ctx, consts, T, P, FR, lbs, Y):
    nc = ctx.nc
    lb, oml, noml = lbs

    def seg_setup(seg, t0):
        pass

    def prologue(h, seg, t0, sl, ps_misc):
        ts = slice(t0, t0 + SEG)
        qa = sl["auxb"][0]
        fr, sg = sl["aux"]
        ctx.sp.dma([(qa[:, :], P.t[h * 128:(h + 1) * 128, ts])], qa, reads=[P], writes=[qa])
        yield
        ctx.sp.dma([(sl["vT"][0][:, :], P.t[2048 + h * 128:2048 + (h + 1) * 128, ts])], sl["vT"][0], reads=[P], writes=[sl["vT"][0]])
        yield
        ctx.sp.dma([(fr[:, :], FR.t[h * 128:(h + 1) * 128, ts])], fr, reads=[FR], writes=[fr])
        yield
        ctx.act.op(lambda: nc.scalar.activation(out=sl["qf"][:, :], in_=qa[:, :], func=AF.Copy), reads=[qa], writes=[sl["qf"]])
        yield
        ctx.act.op(lambda: nc.scalar.activation(out=sg[:, :], in_=fr[:, :], func=AF.Sigmoid), reads=[fr], writes=[sg])
        yield
        ctx.dve.op(lambda: nc.vector.tensor_scalar(out=fr[:, :], in0=sg[:, :], scalar1=oml[:, h:h + 1], scalar2=lb[:, h:h + 1], op0=ALU.mult, op1=ALU.add),
                   reads=[sg, oml, lb], writes=[fr])
        yield
        ctx.act.op(lambda: nc.scalar.activation(out=sl["gf"][:, :], in_=fr[:, :], func=AF.Ln), reads=[fr], writes=[sl["gf"]])
        yield
        ctx.dve.op(lambda: nc.vector.tensor_scalar(out=sl["kf"][:, :], in0=sg[:, :], scalar1=noml[:, h:h + 1], scalar2=oml[:, h:h + 1], op0=ALU.mult, op1=ALU.add),
                   reads=[sg, noml, oml], writes=[sl["kf"]])
        yield

    gla_scan(ctx, consts, T, 16, 128, Y, seg_setup, prologue, CH=32, G=4, nslots=5)


def post_phase(ctx, consts, T, kind, Y, CAT, cat_row0, wvec, gate_src, gate_row0, xs_src=None, dskip=None):
    nc = ctx.nc
    NBH = 8
    gsz = {"ret": 2, "ssd": 4, "hgrn": 1}[kind]
    with ExitStack() as es:
        wk = norm_work(ctx, es, NBH)
        wv = ctx.sbuf(es, [128, 16], F32, "po_w")
        ctx.sp.dma([(wv[:, :], wvec.rearrange("(b p) -> p b", p=128))], wv, writes=[wv], allow_slow_non_contiguous=True)
        if dskip is not None:
            dk = ctx.sbuf(es, [128, 16], F32, "po_dk")
            ctx.sp.dma([(dk[:, :], dskip.rearrange("(b p) -> p b", p=128))], dk, writes=[dk], allow_slow_non_contiguous=True)
        ys = [ctx.sbuf(es, [128, NBH, 512], F32, "po_y") for _ in range(2)]
        gs = [ctx.sbuf(es, [128, NBH, 512], BF16, "po_g") for _ in range(2)]
        xs_ = [ctx.sbuf(es, [128, NBH, 512], BF16, "po_x") for _ in range(2)] if xs_src is not None else None
        os_ = [ctx.sbuf(es, [128, NBH, 512], BF16, "po_o") for _ in range(2)]
        it = 0
        for nt in range(T // 512):
            tsl = slice(nt * 512, (nt + 1) * 512)
            for half in range(2):
                b0 = half * NBH
                y, g, o = ys[it % 2], gs[it % 2], os_[it % 2]
                rs = slice(b0 * 128, (b0 + NBH) * 128)
                ctx.sp.dma([(y[:, :, :], Y.t[rs, tsl].rearrange("(b p) t -> p b t", p=128))], y, reads=[Y], writes=[y])
                grs = slice(gate_row0 + b0 * 128, gate_row0 + (b0 + NBH) * 128)
                ctx.sp.dma([(g[:, :, :], gate_src.t[grs, tsl].rearrange("(b p) t -> p b t", p=128))], g, reads=[gate_src], writes=[g])
                if kind == "ssd":
                    x = xs_[it % 2]
                    ctx.sp.dma([(x[:, :, :], xs_src.t[rs, tsl].rearrange("(b p) t -> p b t", p=128))], x, reads=[xs_src], writes=[x])
                    for b in range(NBH):
                        ctx.dve.op(lambda b=b: nc.vector.scalar_tensor_tensor(out=y[:, b, :], in0=x[:, b, :], scalar=dk[:, b0 + b:b0 + b + 1], in1=y[:, b, :],
                                                                             op0=ALU.mult, op1=ALU.add), reads=[x, dk, y], writes=[y])
                    ctx.pool.op(lambda: nc.gpsimd.tensor_tensor(out=y[:, :, :], in0=y[:, :, :], in1=g[:, :, :], op=ALU.mult), reads=[y, g], writes=[y])
                group_norm_tile(ctx, consts, wk, y, y, NBH, gsz, kind == "ret", gsz * 128)
                for b in range(NBH):
                    if kind == "ssd":
                        ctx.act.op(lambda b=b: nc.scalar.activation(out=o[:, b, :], in_=y[:, b, :], func=AF.Copy, scale=wv[:, b0 + b:b0 + b + 1]), reads=[y, wv], writes=[o])
                    else:
                        ctx.dve.op(lambda b=b: nc.vector.scalar_tensor_tensor(out=o[:, b, :], in0=y[:, b, :], scalar=wv[:, b0 + b:b0 + b + 1], in1=g[:, b, :],
                                                                             op0=ALU.mult, op1=ALU.mult), reads=[y, wv, g], writes=[o])
                ors = slice(cat_row0 + b0 * 128, cat_row0 + (b0 + NBH) * 128)
                ctx.sp.dma([(CAT.t[ors, tsl].rearrange("(b p) t -> p b t", p=128), o[:, :, :])], o, reads=[o], writes=[CAT])
                it += 1
        ctx.barrier()


def router_phase(ctx, consts, T, XT, w_router, GB):
    nc = ctx.nc
    with ExitStack() as es:
        wr = ctx.sbuf(es, [128, 16, NEXP], F32, "rt_w")
        ctx.sp.dma([(wr[:, :, :], w_router.rearrange("(kc p) e -> p kc e", p=128))], wr, writes=[wr])
        xs = [ctx.sbuf(es, [128, 16, 512], F32, "rt_x") for _ in range(2)]
        gall = ctx.sbuf(es, [NEXP, T], F32, "rt_gall")
        ps_l = ctx.psum(es, [128, NEXP], F32, "rt_psl")
        ps_t = ctx.psum(es, [NEXP, 128], F32, "rt_pst")
        lg = ctx.sbuf(es, [128, NEXP], F32, "rt_lg")
        m8 = ctx.sbuf(es, [128, 8], F32, "rt_m8")
        nm = ctx.sbuf(es, [128, 1], F32, "rt_nm")
        mk = ctx.sbuf(es, [128, NEXP], F32, "rt_mk")
        ex = ctx.sbuf(es, [128, NEXP], F32, "rt_ex")
        sm = ctx.sbuf(es, [128, 1], F32, "rt_sm")
        ident_f = consts["ident_f"]
        for nt in range(T // 512):
            x = xs[nt % 2]
            ctx.sp.dma([(x[:, :, :], XT.t[:, nt * 512:(nt + 1) * 512].rearrange("(kc p) t -> p kc t", p=128))], x, reads=[XT], writes=[x])
            for j in range(4):
                tk = slice(j * 128, (j + 1) * 128)

                def mm():
                    ins = None
                    for kc in range(16):
                        ins = ctx.mm(ps_l[:, :], lhsT=x[:, kc, tk], rhs=wr[:, kc, :], start=(kc == 0), stop=(kc == 15))
                    return ins
                ctx.pe.op(mm, reads=[x, wr], writes=[ps_l])
                ctx.dve.op(lambda: nc.vector.tensor_copy(out=lg[:, :], in_=ps_l[:, :]), reads=[ps_l], writes=[lg])
                ctx.dve.op(lambda: nc.vector.max(out=m8[:, :], in_=lg[:, :]), reads=[lg], writes=[m8])
                ctx.dve.op(lambda: nc.vector.tensor_scalar(out=mk[:, :], in0=lg[:, :], scalar1=m8[:, 1:2], scalar2=None, op0=ALU.is_ge), reads=[lg, m8], writes=[mk])
                ctx.dve.op(lambda: nc.vector.tensor_scalar(out=nm[:, :], in0=m8[:, 0:1], scalar1=-1.0, scalar2=None, op0=ALU.mult), reads=[m8], writes=[nm])
                ctx.act.op(lambda: nc.scalar.activation(out=ex[:, :], in_=lg[:, :], func=AF.Exp, bias=nm[:, 0:1]), reads=[lg, nm], writes=[ex])
                ctx.dve.op(lambda: nc.vector.tensor_tensor(out=ex[:, :], in0=ex[:, :], in1=mk[:, :], op=ALU.mult), reads=[ex, mk], writes=[ex])
                ctx.dve.op(lambda: nc.vector.tensor_reduce(out=sm[:, :], in_=ex[:, :], axis=AX.X, op=ALU.add), reads=[ex], writes=[sm])
                ctx.dve.op(lambda: nc.vector.reciprocal(out=sm[:, :], in_=sm[:, :]), reads=[sm], writes=[sm])
                ctx.dve.op(lambda: nc.vector.tensor_scalar(out=ex[:, :], in0=ex[:, :], scalar1=sm[:, 0:1], scalar2=None, op0=ALU.mult), reads=[ex, sm], writes=[ex])
                ctx.pe.op(lambda: ctx.tr(out=ps_t[:, :], in_=ex[:, :], identity=ident_f[:, :]), reads=[ex, ident_f], writes=[ps_t])
                c0 = nt * 512 + j * 128
                ctx.act.op(lambda: nc.scalar.activation(out=gall[:, c0:c0 + 128], in_=ps_t[:, :], func=AF.Copy), reads=[ps_t], writes=[gall])
        ctx.sp.dma([(GB.t[:, :], gall[:, :])], gall, reads=[gall], writes=[GB])
        ctx.barrier()


def moe_down(ctx, H, T, W2s, M):
    nc = ctx.nc
    TS = min(T, 1024)
    NT = TS // 512
    KH = 22
    WCB = 256
    with ExitStack() as es:
        acc = ctx.sbuf(es, [128, 16, TS], F32, "md_acc")
        a_sb = [ctx.sbuf(es, [128, KH, TS], BF16, "md_a") for _ in range(2)]
        w_sb = [ctx.sbuf(es, [128, KH, WCB], BF16, "md_w") for _ in range(2)]
        pss = [ctx.psum(es, [128, 512], F32, "md_ps") for _ in range(3)]
        wi = 0
        pi = 0
        ai = 0
        for st in range(T // TS):
            t0 = st * TS
            first = True
            for e, W2 in enumerate(W2s):
                for half in range(2):
                    r0 = half * KH * 128
                    a = a_sb[ai % 2]
                    ai += 1
                    A_v = H[e].t[r0:r0 + KH * 128, t0:t0 + TS].rearrange("(kc p) t -> p kc t", p=128)
                    ctx.sp.dma([(a[:, k0:k0 + 11, :], A_v[:, k0:k0 + 11, :]) for k0 in range(0, KH, 11)], a, reads=[H[e]], writes=[a])
                    for c0 in range(0, D, WCB):
                        wb = w_sb[wi % 2]
                        wi += 1
                        W_v = W2[r0:r0 + KH * 128, c0:c0 + WCB].rearrange("(kc p) c -> p kc c", p=128)
                        ctx.pool.dma([(wb[:, k0:k0 + 11, :], W_v[:, k0:k0 + 11, :]) for k0 in range(0, KH, 11)], wb, writes=[wb])
                        for m0 in range(0, WCB, 128):
                            cb = (c0 + m0) // 128
                            for nt in range(NT):
                                ps = pss[pi % 3]
                                pi += 1
                                ns = slice(nt * 512, (nt + 1) * 512)

                                def mm(ps=ps, wb=wb, a=a, m0=m0, ns=ns):
                                    ins = None
                                    for kc in range(KH):
                                        ins = ctx.mm(ps[:, :], lhsT=wb[:, kc, m0:m0 + 128], rhs=a[:, kc, ns], start=(kc == 0), stop=(kc == KH - 1))
                                    return ins
                                ctx.pe.op(mm, reads=[wb, a], writes=[ps])
                                if first:
                                    ctx.act.op(lambda ps=ps, cb=cb, ns=ns: nc.scalar.activation(out=acc[:, cb, ns], in_=ps[:, :], func=AF.Copy), reads=[ps], writes=[acc])
                                else:
                                    ctx.dve.op(lambda ps=ps, cb=cb, ns=ns: nc.vector.tensor_tensor(out=acc[:, cb, ns], in0=acc[:, cb, ns], in1=ps[:, :], op=ALU.add),
                                               reads=[ps, acc], writes=[acc])
                    first = False
            ctx.sp.dma([(M.t[:, t0:t0 + TS].rearrange("(b p) t -> p b t", p=128), acc[:, :, :])], acc, reads=[acc], writes=[M])
        ctx.barrier()


def in_proj(ctx, XB, T, W, C, colmap):
    TS = min(T, 2048)
    with ExitStack() as es:
        stagers = {"b": Stager(ctx, es, TS, BF16, 3, "ip_b"), "f": Stager(ctx, es, TS, F32, 2, "ip_f")}
        et, eb = make_plain_epi(ctx, colmap, stagers)
        gemm(ctx, XB, D, T, [W], C, et, eb, TS, 512)


def out_proj(ctx, CAT, K, T, W, M):
    TS = min(T, 1024)
    with ExitStack() as es:
        stg = Stager(ctx, es, TS, F32, 3, "op_stg")
        et, eb = make_plain_epi(ctx, lambda cb0: (M, cb0, None, "m"), {"m": stg})
        gemm(ctx, CAT, K, T, [W], D, et, eb, TS, 256)


def even_layer(ctx, consts, T, S, w, j, last_out=None):
    P, DT, XBCc, SDT, Y, CAT, M, H, XT, XB = (S[k] for k in ("P", "DT", "XBCc", "SDT", "Y", "CAT", "M", "H", "XT", "XB"))

    def colmap(cb0):
        if cb0 < 4096:
            return (P, cb0, None, "b")
        if cb0 < 8192:
            return (P, cb0, AF.Silu, "b")
        if cb0 < 11264:
            return (P, cb0, None, "b")
        return (DT, 0, None, "f")
    in_proj(ctx, XB, T, w["w_in"], EV_IN, colmap)
    ret_scan(ctx, consts, T, P, Y)
    post_phase(ctx, consts, T, "ret", Y, CAT, 0, w["ret_norm_w"], P, 4096)
    XBC = Buf(ctx, P.t[8192:11264, :], "XBC")
    XBC.w, XBC.r = P.w, P.r
    conv_phase(ctx, T, XBC, XBCc, w["conv_w"], w["conv_b"])
    dt_phase(ctx, T, DT, SDT, w["dt_bias"], w["a_log"])
    ssd_scan(ctx, consts, T, XBCc, SDT, Y)
    post_phase(ctx, consts, T, "ssd", Y, CAT, 2048, w["ssd_norm_w"], P, 6144, xs_src=XBCc, dskip=w["d_skip_ch"])
    out_proj(ctx, CAT, 4096, T, w["w_out"], M)
    ln_phase(ctx, consts, XT, M, XB, w["ln1_g"], w["ln1_b"], T)
    swiglu_up(ctx, XB, T, [w["ffn_w1"]], [w["ffn_w3"]], H)
    ffn_down(ctx, H[0], T, w["ffn_w2"], M)
    ln_phase(ctx, consts, XT, M, XB, w["ln2_g"], w["ln2_b"], T, out_final=last_out)


def odd_layer(ctx, consts, T, S, w, j, lbs, last_out=None):
    P, FR, Y, CAT, M, H, XT, XB, GB = (S[k] for k in ("P", "FR", "Y", "CAT", "M", "H", "XT", "XB", "GB"))

    def colmap(cb0):
        if cb0 < 2048:
            return (P, cb0, AF.Silu, "b")
        if cb0 < 4096:
            return (FR, cb0 - 2048, None, "f")
        if cb0 < 6144:
            return (P, cb0 - 2048, None, "b")
        return (P, cb0 - 2048, AF.Silu, "b")
    in_proj(ctx, XB, T, w["w_in"], 8192, colmap)
    hgrn_scan(ctx, consts, T, P, FR, lbs, Y)
    post_phase(ctx, consts, T, "hgrn", Y, CAT, 0, w["hg_norm_w"], P, 4096)
    out_proj(ctx, CAT, 2048, T, w["w_out"], M)
    ln_phase(ctx, consts, XT, M, XB, w["ln1_g"], w["ln1_b"], T)
    router_phase(ctx, consts, T, XT, w["router"], GB)
    swiglu_up(ctx, XB, T, [w["moe_w1"][e] for e in range(NEXP)], [w["moe_w3"][e] for e in range(NEXP)], H, GB=GB)
    moe_down(ctx, H, T, [w["moe_w2"][e] for e in range(NEXP)], M)
    ln_phase(ctx, consts, XT, M, XB, w["ln2_g"], w["ln2_b"], T, out_final=last_out)


WSPEC = [
    ("ev_w_in", [2, D, EV_IN]), ("ev_ret_norm_w", [2, D]), ("ev_conv_w", [2, 4, 3072]), ("ev_conv_b", [2, 3072]),
    ("ev_dt_bias", [2, 32]), ("ev_a_log", [2, 32]), ("ev_d_skip_ch", [2, D]), ("ev_ssd_norm_w", [2, D]),
    ("ev_w_out", [2, 4096, D]), ("ev_ln1_g", [2, D]), ("ev_ln1_b", [2, D]),
    ("ffn_w1", [2, D, FFN]), ("ffn_w3", [2, D, FFN]), ("ffn_w2", [2, FFN, D]), ("ev_ln2_g", [2, D]), ("ev_ln2_b", [2, D]),
    ("od_w_in", [2, D, 8192]), ("hg_lb_logits", [DEPTH, D]), ("od_hg_norm_w", [2, D]), ("od_w_out", [2, D, D]),
    ("od_ln1_g", [2, D]), ("od_ln1_b", [2, D]), ("moe_router", [2, D, NEXP]),
    ("moe_w1", [2, NEXP, D, FFN]), ("moe_w3", [2, NEXP, D, FFN]), ("moe_w2", [2, NEXP, FFN, D]),
    ("od_ln2_g", [2, D]), ("od_ln2_b", [2, D]),
]


def build_program(T, layers=(0, 1, 2, 3), wfilter=None, nl=2):
    nc = bass.Bass("TRN2", target_bir_lowering=False)
    with ExitStack() as es:
        ctx = Ctx(nc, es)
        xin = ctx.dram("xin", [D, T], F32, kind="ExternalInput")
        out = ctx.dram("out", [D, T], F32, kind="ExternalOutput")
        W = {name: nc.dram_tensor(name, ([nl] + shape[1:]) if name != "hg_lb_logits" else shape, F32, kind="ExternalInput").ap()
             for name, shape in WSPEC if (wfilter is None or wfilter(name))}
        S = {
            "XT": ctx.dram("XT", [D, T], F32), "XB": ctx.dram("XB", [D, T], BF16),
            "P": ctx.dram("P", [11264, T], BF16), "DT": ctx.dram("DT", [32, T], F32), "FR": ctx.dram("FR", [D, T], F32),
            "XBCc": ctx.dram("XBCc", [3072, T], BF16), "SDT": ctx.dram("SDT", [64, T], F32),
            "Y": ctx.dram("Y", [D, T], F32), "CAT": ctx.dram("CAT", [4096, T], BF16), "M": ctx.dram("M", [D, T], F32),
            "H": [ctx.dram(f"H{e}", [FFN, T], BF16) for e in range(NEXP)], "GB": ctx.dram("GB", [NEXP, T], F32),
        }
        consts = make_consts(ctx, es)
        lbs = lb_setup(ctx, es, W["hg_lb_logits"])
        ctx.sp.dma([(S["XT"].t[:, :], xin.t[:, :])], S["XT"], reads=[xin], writes=[S["XT"]])
        cast_dram(ctx, S["XB"], xin)
        for li, layer in enumerate(layers):
            j = layer // 2
            last = out if li == len(layers) - 1 else None
            if layer % 2 == 0:
                w = {"w_in": W["ev_w_in"][j], "ret_norm_w": W["ev_ret_norm_w"][j], "conv_w": W["ev_conv_w"][j], "conv_b": W["ev_conv_b"][j],
                     "dt_bias": W["ev_dt_bias"][j], "a_log": W["ev_a_log"][j], "d_skip_ch": W["ev_d_skip_ch"][j], "ssd_norm_w": W["ev_ssd_norm_w"][j],
                     "w_out": W["ev_w_out"][j], "ln1_g": W["ev_ln1_g"][j], "ln1_b": W["ev_ln1_b"][j], "ffn_w1": W["ffn_w1"][j], "ffn_w3": W["ffn_w3"][j],
                     "ffn_w2": W["ffn_w2"][j], "ln2_g": W["ev_ln2_g"][j], "ln2_b": W["ev_ln2_b"][j]}
                even_layer(ctx, consts, T, S, w, j, last_out=last)
            else:
                w = {"w_in": W["od_w_in"][j], "hg_norm_w": W["od_hg_norm_w"][j], "w_out": W["od_w_out"][j], "ln1_g": W["od_ln1_g"][j], "ln1_b": W["od_ln1_b"][j],
                     "router": W["moe_router"][j], "moe_w1": W["moe_w1"][j], "moe_w3": W["moe_w3"][j], "moe_w2": W["moe_w2"][j],
                     "ln2_g": W["od_ln2_g"][j], "ln2_b": W["od_ln2_b"][j]}
                odd_layer(ctx, consts, T, S, w, j, lbs[layer], last_out=last)
        finish(ctx, [out])
    return nc


def host_inputs(inputs, xT):
    m = {"xin": np.ascontiguousarray(xT, dtype=np.float32)}
    for name, shape in WSPEC:
        if name == "ev_d_skip_ch":
            m[name] = np.ascontiguousarray(np.repeat(np.asarray(inputs["ev_d_skip"], dtype=np.float32), 64, axis=1))
        else:
            m[name] = np.ascontiguousarray(np.asarray(inputs[name], dtype=np.float32))
    return m


N_ACTIVE = 4


def kernel(**inputs):
    x = np.asarray(inputs["x"], dtype=np.float32)
    B, T, _ = x.shape
    nc = build_program(T)
    in_maps = []
    for c in range(N_ACTIVE):
        in_maps.append(host_inputs(inputs, x[c % B].T))
    res = run_bass_kernel_spmd(nc, in_maps, core_ids=list(range(N_ACTIVE)))
    outs = [np.ascontiguousarray(res.results[c]["out"].T) for c in range(B)]
    return np.stack(outs, axis=0).astype(np.float32)
```

```python
import math
from contextlib import ExitStack

import numpy as np
import concourse.bass as bass
import concourse.mybir as mybir
from concourse.alu_op_type import AluOpType as ALU
from concourse.bass_utils import run_bass_kernel_spmd

F32 = mybir.dt.float32
BF16 = mybir.dt.bfloat16
I32 = mybir.dt.int32
AF = mybir.ActivationFunctionType
AX = mybir.AxisListType

D = 2048
DEPTH = 4
LN_EPS = 1e-5
ALPHA = (2.0 * DEPTH) ** 0.25
FFN = 5632
NEXP = 8
EV_IN = 11296
SEM_LIMIT = 2000000


class Sem:
    def __init__(self, ctx, name):
        self.h = ctx.es.enter_context(ctx.nc.semaphore(name))
        self.v = 0
        ctx.sems.append(self)


class Buf:
    def __init__(self, ctx, t, name):
        self.ctx = ctx
        self.t = t
        self.name = name
        self.w = {}
        self.r = {}
        self.dsem = None

    def __getitem__(self, idx):
        return self.t[idx]

    def dma_sem(self):
        if self.dsem is None or self.dsem.v >= SEM_LIMIT:
            pool = self.ctx.sem_pool
            self.dsem = pool.pop() if pool else Sem(self.ctx, None)
            self.ctx.dma_bufs.append(self)
        return self.dsem


class Q:
    def __init__(self, ctx, name, eng, self_sync=True):
        self.ctx = ctx
        self.name = name
        self.eng = eng
        self.sem = None
        self.seen = {}
        self.self_sync = self_sync

    def _need(self, sem, val, pend):
        if not self.self_sync and sem is self.sem:
            return
        if self.seen.get(sem, 0) >= val or pend.get(sem, 0) >= val:
            return
        pend[sem] = val

    def _wait(self, sem, val):
        pend = {}
        self._need(sem, val, pend)
        self._emit(pend, None)

    def _emit(self, pend, keep_last):
        items = list(pend.items())
        ret = None
        if keep_last and items:
            ret = items.pop()
        for s, v in items:
            self.eng.wait_ge(s.h, v)
            self.seen[s] = v
        if ret is not None:
            self.seen[ret[0]] = ret[1]
        return ret

    def deps(self, reads, writes, keep_last=False):
        pend = {}
        for b in reads:
            for s, v in b.w.items():
                self._need(s, v, pend)
        for b in writes:
            for s, v in b.w.items():
                self._need(s, v, pend)
            for s, v in b.r.items():
                self._need(s, v, pend)
        return self._emit(pend, keep_last)

    def mark(self, tok, reads, writes):
        s, v = tok
        for b in reads:
            if b.r.get(s, 0) < v:
                b.r[s] = v
        for b in writes:
            if b.w.get(s, 0) < v:
                b.w[s] = v

    def op(self, ins_fn, reads=(), writes=(), single=None):
        if single is None:
            single = self.self_sync
        w = self.deps(reads, writes, keep_last=single)
        self.ctx._first = None
        ins = ins_fn()
        if w is not None:
            ins._wait_ge(w[0].h, w[1])
        if self.sem is None or self.sem.v >= SEM_LIMIT:
            self.sem = Sem(self.ctx, None)
        self.sem.v += 1
        ins.then_inc(self.sem.h, 1)
        self.mark((self.sem, self.sem.v), reads, writes)

    def dma(self, pairs, sb, reads=(), writes=(), **kw):
        w = self.deps(reads, writes, keep_last=True)
        s = sb.dma_sem()
        for out_ap, in_ap in pairs:
            s.v += 16
            ins = self.eng.dma_start(out=out_ap, in_=in_ap, **kw)
            if w is not None:
                ins._wait_ge(w[0].h, w[1])
                w = None
            ins.then_inc(s.h, 16)
        self.mark((s, s.v), reads, writes)


class Ctx:
    def __init__(self, nc, es):
        self.nc = nc
        self.es = es
        self.sems = []
        self.sem_pool = []
        self.dma_bufs = []
        self._first = None
        self.pe = Q(self, "pe", nc.tensor, self_sync=False)
        self.dve = Q(self, "dve", nc.vector)
        self.act = Q(self, "act", nc.scalar)
        self.pool = Q(self, "pool", nc.gpsimd)
        self.sp = Q(self, "sp", nc.sync)
        self.qs = [self.pe, self.dve, self.act, self.pool, self.sp]
        self.uid = 0

    def mm(self, *a, **k):
        ins = self.nc.tensor.matmul(*a, **k)
        if self._first is None:
            self._first = ins
        return ins

    def tr(self, *a, **k):
        ins = self.nc.tensor.transpose(*a, **k)
        if self._first is None:
            self._first = ins
        return ins

    def name(self, base):
        self.uid += 1
        return f"{base}_{self.uid}"

    def sbuf(self, es, shape, dt, name="sb"):
        t = es.enter_context(self.nc.sbuf_tensor(self.name(name), list(shape), dt))
        return Buf(self, t, name)

    def psum(self, es, shape, dt, name="ps"):
        t = es.enter_context(self.nc.psum_tensor(self.name(name), list(shape), dt))
        return Buf(self, t, name)

    def dram(self, name, shape, dt, kind="Internal"):
        t = self.nc.dram_tensor(name, list(shape), dt, kind=kind)
        return Buf(self, t.ap(), name)

    def barrier(self, qs=None):
        for q in (qs or self.qs):
            for s in self.sems:
                if s.v > 0:
                    q._wait(s, s.v)
        for b in self.dma_bufs:
            if b.dsem is not None and b.dsem.v < SEM_LIMIT:
                self.sem_pool.append(b.dsem)
            b.dsem = None
        self.dma_bufs = []


def gemm(ctx, A, K, T, Ws, C, epi_tile, epi_block, TS, WCB, n_par=1, acc=None, ps_bufs=None,
         a_row0=0, w_row0=0, keep_a=None):
    nc = ctx.nc
    KC = K // 128
    NT = TS // 512
    nW = len(Ws)
    with ExitStack() as es:
        a_sb = keep_a if keep_a is not None else ctx.sbuf(es, [128, KC, TS], BF16, "a_sb")
        w_sb = [[ctx.sbuf(es, [128, KC, WCB], BF16, "w_sb") for _ in range(2)] for _ in range(nW)]
        if ps_bufs is None:
            ps_bufs = [[ctx.psum(es, [128, 512], F32, "ps") for _ in range(2)] for _ in range(nW)]
        pi = 0
        wi = 0
        for st in range(T // TS):
            t0 = st * TS
            A_v = A.t[a_row0:a_row0 + K, t0:t0 + TS].rearrange("(kc p) t -> p kc t", p=128)
            g = 4 if KC % 4 == 0 else (2 if KC % 2 == 0 else 1)
            ctx.sp.dma([(a_sb[:, k0:k0 + g, :], A_v[:, k0:k0 + g, :]) for k0 in range(0, KC, g)],
                       a_sb, reads=[A], writes=[a_sb])
            for c0 in range(0, C, WCB):
                cw = min(WCB, C - c0)
                slot = wi % 2
                wi += 1
                for j, W in enumerate(Ws):
                    W_v = W[w_row0:w_row0 + K, c0:c0 + cw].rearrange("(kc p) c -> p kc c", p=128)
                    wb = w_sb[j][slot]
                    h = KC // 2 if KC % 2 == 0 else KC
                    ctx.pool.dma([(wb[:, k0:k0 + h, 0:cw], W_v[:, k0:k0 + h, :]) for k0 in range(0, KC, h)],
                                 wb, reads=[], writes=[wb])
                for m0 in range(0, cw, 128):
                    rows = min(128, cw - m0)
                    cb0 = c0 + m0
                    for nt in range(NT):
                        pss = []
                        for j in range(nW):
                            ps = ps_bufs[j][pi % 2]
                            wb = w_sb[j][slot]

                            def mm(ps=ps, wb=wb):
                                ins = None
                                for kc in range(KC):
                                    ins = ctx.mm(ps[0:rows, :], lhsT=wb[:, kc, m0:m0 + rows],
                                                           rhs=a_sb[:, kc, nt * 512:(nt + 1) * 512],
                                                           start=(kc == 0), stop=(kc == KC - 1))
                                return ins
                            ctx.pe.op(mm, reads=[wb, a_sb], writes=[ps])
                            pss.append(ps)
                        pi += 1
                        epi_tile(pss, cb0, rows, st, nt, t0)
                    if epi_block is not None:
                        epi_block(cb0, rows, st, t0)
        ctx.barrier()


class Stager:
    def __init__(self, ctx, es, TS, dt, n=3, name="stg"):
        self.ctx = ctx
        self.bufs = [ctx.sbuf(es, [128, TS], dt, name) for _ in range(n)]
        self.i = 0

    def cur(self):
        return self.bufs[self.i % len(self.bufs)]

    def next(self):
        self.i += 1


def make_plain_epi(ctx, colmap, stagers, flip=[0]):
    def epi_tile(pss, cb0, rows, st, nt, t0):
        dst, r0, func, key = colmap(cb0)
        sb = stagers[key].cur()
        ps = pss[0]
        sl = slice(nt * 512, (nt + 1) * 512)
        if func is None and flip[0] % 2 == 0:
            ctx.dve.op(lambda: ctx.nc.vector.tensor_copy(out=sb[0:rows, sl], in_=ps[0:rows, :]), reads=[ps], writes=[sb])
        else:
            f = AF.Copy if func is None else func
            ctx.act.op(lambda: ctx.nc.scalar.activation(out=sb[0:rows, sl], in_=ps[0:rows, :], func=f), reads=[ps], writes=[sb])
        flip[0] += 1

    def epi_block(cb0, rows, st, t0):
        dst, r0, func, key = colmap(cb0)
        stg = stagers[key]
        sb = stg.cur()
        TS = sb.t.shape[1]
        ctx.sp.dma([(dst.t[r0:r0 + rows, t0:t0 + TS], sb[0:rows, :])], sb, reads=[sb], writes=[dst])
        stg.next()

    return epi_tile, epi_block


def make_consts(ctx, es):
    nc = ctx.nc
    c = {}
    ones_f = ctx.sbuf(es, [128, 128], F32, "ones_f")
    ctx.pool.op(lambda: nc.gpsimd.memset(ones_f[:], 1.0), writes=[ones_f])
    ones_b = ctx.sbuf(es, [128, 128], BF16, "ones_b")
    ctx.pool.op(lambda: nc.gpsimd.memset(ones_b[:], 1.0), writes=[ones_b])
    ident_f = ctx.sbuf(es, [128, 128], F32, "ident_f")
    ctx.pool.op(lambda: nc.gpsimd.affine_select(out=ident_f[:], in_=ones_f[:], pattern=[[-1, 128]],
                                                compare_op=ALU.is_equal, fill=0.0, base=0, channel_multiplier=1),
                reads=[ones_f], writes=[ident_f])
    ident_b = ctx.sbuf(es, [128, 128], BF16, "ident_b")
    ctx.dve.op(lambda: nc.vector.tensor_copy(out=ident_b[:], in_=ident_f[:]), reads=[ident_f], writes=[ident_b])
    c["ones_f"], c["ones_b"], c["ident_f"], c["ident_b"] = ones_f, ones_b, ident_f, ident_b
    return c


def group_norm_tile(ctx, consts, wk, r, out, NB, gsz, center, nfeat, W=512):
    nc = ctx.nc
    rb, rsq, ps1, ps2, mean, rstd, tmp = wk["rb"], wk["rsq"], wk["ps1"], wk["ps2"], wk["mean"], wk["rstd"], wk["tmp"]
    ones_b = consts["ones_b"]
    inv = 1.0 / nfeat
    if center:
        ctx.act.op(lambda: nc.scalar.activation(out=rb[:, 0:NB, 0:W], in_=r[:, 0:NB, 0:W], func=AF.Copy), reads=[r], writes=[rb])
    ctx.pool.op(lambda: nc.gpsimd.tensor_tensor(out=rsq[:, 0:NB, 0:W], in0=r[:, 0:NB, 0:W], in1=r[:, 0:NB, 0:W], op=ALU.mult),
                reads=[r], writes=[rsq])
    for g0 in range(0, NB, gsz):
        if center:
            def mm1():
                ins = None
                for b in range(g0, g0 + gsz):
                    ins = ctx.mm(ps1[:, 0:W], lhsT=ones_b[:], rhs=rb[:, b, 0:W], start=(b == g0), stop=(b == g0 + gsz - 1))
                return ins
            ctx.pe.op(mm1, reads=[ones_b, rb], writes=[ps1])

        def mm2():
            ins = None
            for b in range(g0, g0 + gsz):
                ins = ctx.mm(ps2[:, 0:W], lhsT=ones_b[:], rhs=rsq[:, b, 0:W], start=(b == g0), stop=(b == g0 + gsz - 1))
            return ins
        ctx.pe.op(mm2, reads=[ones_b, rsq], writes=[ps2])
        if center:
            ctx.act.op(lambda: nc.scalar.activation(out=mean[:, 0:W], in_=ps1[:, 0:W], func=AF.Copy, scale=inv), reads=[ps1], writes=[mean])
            ctx.dve.op(lambda: nc.vector.tensor_tensor(out=tmp[:, 0:W], in0=mean[:, 0:W], in1=mean[:, 0:W], op=ALU.mult), reads=[mean], writes=[tmp])
            ctx.dve.op(lambda: nc.vector.scalar_tensor_tensor(out=tmp[:, 0:W], in0=ps2[:, 0:W], scalar=inv, in1=tmp[:, 0:W],
                                                              op0=ALU.mult, op1=ALU.subtract), reads=[ps2, tmp], writes=[tmp])
            ctx.dve.op(lambda: nc.vector.tensor_scalar(out=tmp[:, 0:W], in0=tmp[:, 0:W], scalar1=0.0, scalar2=LN_EPS, op0=ALU.max, op1=ALU.add),
                       reads=[tmp], writes=[tmp])
        else:
            ctx.dve.op(lambda: nc.vector.tensor_scalar(out=tmp[:, 0:W], in0=ps2[:, 0:W], scalar1=inv, scalar2=LN_EPS, op0=ALU.mult, op1=ALU.add),
                       reads=[ps2], writes=[tmp])
        ctx.act.op(lambda: nc.scalar.activation(out=tmp[:, 0:W], in_=tmp[:, 0:W], func=AF.Sqrt), reads=[tmp], writes=[tmp])
        ctx.dve.op(lambda: nc.vector.reciprocal(out=rstd[:, 0:W], in_=tmp[:, 0:W]), reads=[tmp], writes=[rstd])
        for b in range(g0, g0 + gsz):
            q = ctx.dve if b % 2 == 0 else ctx.pool
            e = nc.vector if b % 2 == 0 else nc.gpsimd
            if center:
                q.op(lambda e=e, b=b: e.tensor_tensor(out=out[:, b, 0:W], in0=r[:, b, 0:W], in1=mean[:, 0:W], op=ALU.subtract), reads=[r, mean], writes=[out])
                q.op(lambda e=e, b=b: e.tensor_tensor(out=out[:, b, 0:W], in0=out[:, b, 0:W], in1=rstd[:, 0:W], op=ALU.mult), reads=[out, rstd], writes=[out])
            else:
                q.op(lambda e=e, b=b: e.tensor_tensor(out=out[:, b, 0:W], in0=r[:, b, 0:W], in1=rstd[:, 0:W], op=ALU.mult), reads=[r, rstd], writes=[out])


def norm_work(ctx, es, NB, W=512):
    return {
        "rb": ctx.sbuf(es, [128, NB, W], BF16, "n_rb"),
        "rsq": ctx.sbuf(es, [128, NB, W], BF16, "n_rsq"),
        "ps1": ctx.psum(es, [128, 512], F32, "n_ps1"),
        "ps2": ctx.psum(es, [128, 512], F32, "n_ps2"),
        "mean": ctx.sbuf(es, [128, W], F32, "n_mean"),
        "rstd": ctx.sbuf(es, [128, W], F32, "n_rstd"),
        "tmp": ctx.sbuf(es, [128, W], F32, "n_tmp"),
    }


def ln_phase(ctx, consts, XT, M, XB, gvec, bvec, T, out_final=None):
    nc = ctx.nc
    NB = D // 128
    with ExitStack() as es:
        wk = norm_work(ctx, es, NB)
        gb = ctx.sbuf(es, [128, 2, NB], F32, "ln_gb")
        ctx.sp.dma([(gb[:, 0, :], gvec.rearrange("(b p) -> p b", p=128)), (gb[:, 1, :], bvec.rearrange("(b p) -> p b", p=128))],
                   gb, writes=[gb], allow_slow_non_contiguous=True)
        xs = [ctx.sbuf(es, [128, NB, 512], F32, "ln_x") for _ in range(2)]
        ms = [ctx.sbuf(es, [128, NB, 512], F32, "ln_m") for _ in range(2)]
        ob = [ctx.sbuf(es, [128, NB, 512], BF16, "ln_ob") for _ in range(2)]
        for nt in range(T // 512):
            x, m, o = xs[nt % 2], ms[nt % 2], ob[nt % 2]
            tsl = slice(nt * 512, (nt + 1) * 512)
            ctx.sp.dma([(x[:, :, :], XT.t[:, tsl].rearrange("(b p) t -> p b t", p=128))], x, reads=[XT], writes=[x])
            ctx.sp.dma([(m[:, :, :], M.t[:, tsl].rearrange("(b p) t -> p b t", p=128))], m, reads=[M], writes=[m])
            ctx.dve.op(lambda: nc.vector.scalar_tensor_tensor(out=m[:, :, :], in0=x[:, :, :], scalar=ALPHA, in1=m[:, :, :], op0=ALU.mult, op1=ALU.add),
                       reads=[x, m], writes=[m])
            group_norm_tile(ctx, consts, wk, m, x, NB, NB, True, D)
            for b in range(NB):
                q = ctx.dve if b % 2 == 0 else ctx.pool
                e = nc.vector if b % 2 == 0 else nc.gpsimd
                q.op(lambda e=e, b=b: e.tensor_scalar(out=x[:, b, :], in0=x[:, b, :], scalar1=gb[:, 0, b:b + 1], scalar2=gb[:, 1, b:b + 1],
                                                     op0=ALU.mult, op1=ALU.add), reads=[x, gb], writes=[x])
            if out_final is not None:
                ctx.sp.dma([(out_final.t[:, tsl].rearrange("(b p) t -> p b t", p=128), x[:, :, :])], x, reads=[x], writes=[out_final])
            else:
                ctx.act.op(lambda: nc.scalar.activation(out=o[:, :, :], in_=x[:, :, :], func=AF.Copy), reads=[x], writes=[o])
                ctx.sp.dma([(XT.t[:, tsl].rearrange("(b p) t -> p b t", p=128), x[:, :, :])], x, reads=[x], writes=[XT])
                ctx.sp.dma([(XB.t[:, tsl].rearrange("(b p) t -> p b t", p=128), o[:, :, :])], o, reads=[o], writes=[XB])
        ctx.barrier()


def swiglu_up(ctx, XB, T, W1s, W3s, H, GB=None):
    nc = ctx.nc
    TS = min(T, 2048)
    with ExitStack() as es:
        stg = Stager(ctx, es, TS, BF16, 3, "h_stg")
        tmps = [ctx.sbuf(es, [128, 512], F32, "sw_tmp") for _ in range(3)]
        gts = [ctx.sbuf(es, [128, TS], F32, "gate_t") for _ in range(2)] if GB is not None else None
        ti = [0]
        for e, (W1, W3) in enumerate(zip(W1s, W3s)):
            cur_g = [None]

            def epi_tile(pss, cb0, rows, st, nt, t0, e=e):
                tmp = tmps[ti[0] % 3]
                ti[0] += 1
                sb = stg.cur()
                sl = slice(nt * 512, (nt + 1) * 512)
                ctx.act.op(lambda: nc.scalar.activation(out=tmp[:, :], in_=pss[0][:, :], func=AF.Silu), reads=[pss[0]], writes=[tmp])
                if GB is not None:
                    if nt == 0 and cb0 == 0:
                        g = gts[(e + st) % 2]
                        ctx.sp.dma([(g[:, :], GB.t[e, t0:t0 + TS].partition_broadcast(128))], g, reads=[GB], writes=[g])
                        cur_g[0] = g
                    g = cur_g[0]
                    ctx.dve.op(lambda: nc.vector.tensor_tensor(out=tmp[:, :], in0=tmp[:, :], in1=pss[1][:, :], op=ALU.mult), reads=[tmp, pss[1]], writes=[tmp])
                    ctx.dve.op(lambda: nc.vector.tensor_tensor(out=sb[:, sl], in0=tmp[:, :], in1=g[:, sl], op=ALU.mult), reads=[tmp, g], writes=[sb])
                else:
                    ctx.dve.op(lambda: nc.vector.tensor_tensor(out=sb[:, sl], in0=tmp[:, :], in1=pss[1][:, :], op=ALU.mult), reads=[tmp, pss[1]], writes=[sb])

            def epi_block(cb0, rows, st, t0, e=e):
                sb = stg.cur()
                ctx.sp.dma([(H[e].t[cb0:cb0 + rows, t0:t0 + TS], sb[0:rows, :])], sb, reads=[sb], writes=[H[e]])
                stg.next()

            gemm(ctx, XB, D, T, [W1, W3], FFN, epi_tile, epi_block, TS, 256)


def cast_dram(ctx, dst, src):
    ctx.pool.dma([(dst.t[:, :], src.t[:, :])], dst, reads=[src], writes=[dst])


def ffn_down(ctx, H, T, W2, M, K=FFN):
    TS = min(T, 1024)
    with ExitStack() as es:
        stg = Stager(ctx, es, TS, F32, 3, "m_stg")
        et, eb = make_plain_epi(ctx, lambda cb0: (M, cb0, None, "m"), {"m": stg})
        gemm(ctx, H, K, T, [W2], D, et, eb, TS, 256)


def finish(ctx, bufs):
    ctx.barrier()


def all_gather(ctx, dst, src, n_cores):
    q = ctx.pool
    q.deps([src], [dst])
    if getattr(ctx, "cc_sem", None) is None or ctx.cc_sem.v >= SEM_LIMIT:
        ctx.cc_sem = Sem(ctx, None)
    s = ctx.cc_sem
    s.v += 1
    ctx.nc.gpsimd.collective_compute("AllGather", ALU.bypass, replica_groups=[list(range(n_cores))],
                                     ins=[src.t.opt()], outs=[dst.t.opt()]).then_inc(s.h)
    q.mark((s, s.v), [src], [dst])


SEG = 512
CH = 32
NCH = SEG // CH


def gla_scan(ctx, consts, T, nheads, dv, Y, seg_setup, prologue, nslots=3, scalar_decay=False, CH=32, G=2):
    nc = ctx.nc
    NCH = SEG // CH
    mid = CH // 2 - 1
    nvb = (dv + 127) // 128
    dvb = min(dv, 128)
    with ExitStack() as es:
        m0 = ctx.sbuf(es, [128, NCH, CH], F32, "m0")
        ctx.pool.op(lambda: nc.gpsimd.memset(m0[:, :, :], 1.0), writes=[m0])
        ctx.pool.op(lambda: nc.gpsimd.memset(m0[:, :, 0:1], 0.0), writes=[m0])
        cm = ctx.sbuf(es, [CH, NCH, CH], F32, "cm")
        ctx.pool.op(lambda: nc.gpsimd.memset(cm[:, :, :], 1.0), writes=[cm])
        ctx.pool.op(lambda: nc.gpsimd.affine_select(out=cm[:, :, :], in_=cm[:, :, :], pattern=[[0, NCH], [1, CH]],
                                                    compare_op=ALU.is_ge, fill=0.0, base=0, channel_multiplier=-1),
                    reads=[cm], writes=[cm])
        if scalar_decay:
            idm = ctx.sbuf(es, [CH, NCH, CH], F32, "idm")
            ctx.pool.op(lambda: nc.gpsimd.memset(idm[:, :, :], 1.0), writes=[idm])
            ctx.pool.op(lambda: nc.gpsimd.affine_select(out=idm[:, :, :], in_=idm[:, :, :], pattern=[[0, NCH], [1, CH]],
                                                        compare_op=ALU.is_equal, fill=0.0, base=0, channel_multiplier=-1), reads=[idm], writes=[idm])
            ngm = ctx.sbuf(es, [CH, NCH, CH], F32, "ngm")
            ctx.pool.op(lambda: nc.gpsimd.memset(ngm[:, :, :], 0.0), writes=[ngm])
            ctx.pool.op(lambda: nc.gpsimd.affine_select(out=ngm[:, :, :], in_=ngm[:, :, :], pattern=[[0, NCH], [1, CH]],
                                                        compare_op=ALU.is_ge, fill=-1.0e30, base=0, channel_multiplier=-1), reads=[ngm], writes=[ngm])
        S = ctx.sbuf(es, [128, nheads, dv], F32, "S")
        Sb = ctx.sbuf(es, [128, nheads, dv], BF16, "Sb")
        Sh = [Buf(ctx, S.t, "S_h") for _ in range(nheads)]
        Sbh = [Buf(ctx, Sb.t, "Sb_h") for _ in range(nheads)]
        ctx.pool.op(lambda: nc.gpsimd.memset(S[:, :, :], 0.0), writes=Sh)
        ctx.pool.op(lambda: nc.gpsimd.memset(Sb[:, :, :], 0.0), writes=Sbh)
        slots = []
        for i in range(nslots):
            sl = {
                "qf": ctx.sbuf(es, [128, SEG], F32, "qf"), "kf": ctx.sbuf(es, [128, SEG], F32, "kf"),
                "gf": ctx.sbuf(es, [128, SEG], F32, "gf"), "cum": ctx.sbuf(es, [128, NCH, CH], F32, "cum"),
                "t1": ctx.sbuf(es, [128, NCH, CH], F32, "t1"), "e": ctx.sbuf(es, [128, NCH, CH], F32, "e"),
                "qt": ctx.sbuf(es, [128, SEG], BF16, "qt"), "kt": ctx.sbuf(es, [128, SEG], BF16, "kt"),
                "qh": ctx.sbuf(es, [128, SEG], BF16, "qh"), "kd": ctx.sbuf(es, [128, SEG], BF16, "kd"),
                "dcol": ctx.sbuf(es, [128, NCH], F32, "dcol"),
                "dm": ctx.sbuf(es, [CH, NCH, CH], F32, "dm"), "col": ctx.sbuf(es, [CH, NCH], F32, "col"),
                "vT": [ctx.sbuf(es, [dvb, SEG], BF16, "vT") for _ in range(nvb)],
                "vtok": ctx.sbuf(es, [CH, NCH, dv], BF16, "vtok"), "kdtok": ctx.sbuf(es, [CH, NCH, 128], BF16, "kdtok"),
                "sc": ctx.sbuf(es, [CH, SEG], BF16, "sc"),
                "ysb": [ctx.sbuf(es, [dvb, SEG], F32, "ysb") for _ in range(nvb)],
                "aux": [ctx.sbuf(es, [128, SEG], F32, "aux") for _ in range(2)],
                "auxb": [ctx.sbuf(es, [128, SEG], BF16, "auxb") for _ in range(2)],
            }
            slots.append(sl)
        rem = 8 - G * nvb - 1
        n_sc = 2 if rem >= 4 else 1
        n_u = max(1, min(G, rem - n_sc))
        ps_sc = [ctx.psum(es, [CH, SEG], F32, "ps_sc") for _ in range(n_sc)]
        ps_y = [[ctx.psum(es, [dvb, SEG], F32, "ps_y") for _ in range(nvb)] for _ in range(G)]
        ps_tr = ctx.psum(es, [CH, 1024], BF16, "ps_tr")
        ps_u = [ctx.psum(es, [128, 512], F32, "ps_u") for _ in range(n_u)]
        ps_misc = ps_u[0]
        ident_b = consts["ident_b"]
        si = 0
        for seg in range(T // SEG):
            t0 = seg * SEG
            seg_setup(seg, t0)
            for h0 in range(0, nheads, G):
                hs = list(range(h0, min(h0 + G, nheads)))
                def head_gen(h, sl):
                    yield from prologue(h, seg, t0, sl, ps_misc)
                    qf, kf, gf, cum, t1, e = sl["qf"], sl["kf"], sl["gf"], sl["cum"], sl["t1"], sl["e"]
                    cumf = cum.t.rearrange("p c t -> p (c t)")
                    ctx.dve.op(lambda: nc.vector.tensor_tensor_scan(out=cumf, data0=m0.t.rearrange("p c t -> p (c t)"), data1=gf[:, :],
                                                                    initial=0.0, op0=ALU.mult, op1=ALU.add), reads=[m0, gf], writes=[cum])
                    yield
                    if scalar_decay:
                        dm, col = sl["dm"], sl["col"]
                        ctx.dve.op(lambda: nc.vector.tensor_tensor(out=dm[:, :, :], in0=cum[0:CH, :, :], in1=idm[:, :, :], op=ALU.mult), reads=[cum, idm], writes=[dm])
                        yield
                        ctx.dve.op(lambda: nc.vector.tensor_reduce(out=col[:, :], in_=dm[:, :, :], axis=AX.X, op=ALU.add), reads=[dm], writes=[col])
                        yield
                        ctx.dve.op(lambda: nc.vector.tensor_tensor(out=dm[:, :, :], in0=cum[0:CH, :, :], in1=col[:, :].unsqueeze(2).to_broadcast([CH, NCH, CH]),
                                                                   op=ALU.subtract), reads=[cum, col], writes=[dm])
                        yield
                        ctx.dve.op(lambda: nc.vector.tensor_tensor(out=dm[:, :, :], in0=dm[:, :, :], in1=ngm[:, :, :], op=ALU.min), reads=[dm, ngm], writes=[dm])
                        yield
                        ctx.act.op(lambda: nc.scalar.activation(out=dm[:, :, :], in_=dm[:, :, :], func=AF.Exp), reads=[dm], writes=[dm])
                        yield
                        ctx.act.op(lambda: nc.scalar.activation(out=sl["qt"][:, :], in_=qf[:, :], func=AF.Copy), reads=[qf], writes=[sl["qt"]])
                        yield
                        ctx.pool.op(lambda: nc.gpsimd.tensor_copy(out=sl["kt"][:, :], in_=kf[:, :]), reads=[kf], writes=[sl["kt"]])
                        yield
                    else:
                        ctx.dve.op(lambda: nc.vector.tensor_tensor(out=t1[:, :, :], in0=cum[:, :, :], in1=cum[:, :, mid:mid + 1].to_broadcast([128, NCH, CH]),
                                                                   op=ALU.subtract), reads=[cum], writes=[t1])
                        yield
                        ctx.act.op(lambda: nc.scalar.activation(out=e[:, :, :], in_=t1[:, :, :], func=AF.Exp), reads=[t1], writes=[e])
                        yield
                        ctx.dve.op(lambda: nc.vector.tensor_tensor(out=sl["qt"][:, :], in0=qf[:, :], in1=e.t.rearrange("p c t -> p (c t)"), op=ALU.mult),
                                   reads=[qf, e], writes=[sl["qt"]])
                        yield
                        ctx.act.op(lambda: nc.scalar.activation(out=e[:, :, :], in_=t1[:, :, :], func=AF.Exp, scale=-1.0), reads=[t1], writes=[e])
                        yield
                        ctx.pool.op(lambda: nc.gpsimd.tensor_tensor(out=sl["kt"][:, :], in0=kf[:, :], in1=e.t.rearrange("p c t -> p (c t)"), op=ALU.mult),
                                    reads=[kf, e], writes=[sl["kt"]])
                        yield
                    ctx.act.op(lambda: nc.scalar.activation(out=e[:, :, :], in_=cum[:, :, :], func=AF.Exp), reads=[cum], writes=[e])
                    yield
                    ctx.dve.op(lambda: nc.vector.tensor_tensor(out=sl["qh"][:, :], in0=qf[:, :], in1=e.t.rearrange("p c t -> p (c t)"), op=ALU.mult),
                               reads=[qf, e], writes=[sl["qh"]])
                    yield
                    ctx.act.op(lambda: nc.scalar.activation(out=sl["dcol"][:, :], in_=cum[:, :, CH - 1], func=AF.Exp), reads=[cum], writes=[sl["dcol"]])
                    yield
                    ctx.pool.op(lambda: nc.gpsimd.tensor_tensor(out=t1[:, :, :], in0=cum[:, :, :], in1=cum[:, :, CH - 1:CH].to_broadcast([128, NCH, CH]),
                                                                op=ALU.subtract), reads=[cum], writes=[t1])
                    yield
                    ctx.act.op(lambda: nc.scalar.activation(out=e[:, :, :], in_=t1[:, :, :], func=AF.Exp, scale=-1.0), reads=[t1], writes=[e])
                    yield
                    ctx.pool.op(lambda: nc.gpsimd.tensor_tensor(out=sl["kd"][:, :], in0=kf[:, :], in1=e.t.rearrange("p c t -> p (c t)"), op=ALU.mult),
                                reads=[kf, e], writes=[sl["kd"]])
                    yield
                    per = max(1, min(NCH, 1024 // dv))
                    kg = min(NCH, 8)
                    for c0 in range(0, NCH, per):
                        def trv(c0=c0):
                            ins = None
                            for c in range(c0, c0 + per):
                                for vb in range(nvb):
                                    o0 = (c - c0) * dv + vb * 128
                                    ins = ctx.tr(out=ps_tr[0:CH, o0:o0 + dvb], in_=sl["vT"][vb][0:dvb, c * CH:(c + 1) * CH],
                                                              identity=ident_b[0:dvb, 0:dvb])
                            return ins
                        ctx.pe.op(trv, reads=sl["vT"] + [ident_b], writes=[ps_tr])
                        ctx.act.op(lambda c0=c0: nc.scalar.activation(out=sl["vtok"][:, c0:c0 + per, :], in_=ps_tr.t[0:CH, 0:per * dv].rearrange("p (c d) -> p c d", d=dv),
                                                                      func=AF.Copy), reads=[ps_tr], writes=[sl["vtok"]])
                        yield
                    for c0 in range(0, NCH, kg):
                        def trk(c0=c0):
                            ins = None
                            for c in range(c0, c0 + kg):
                                ins = ctx.tr(out=ps_tr[0:CH, (c - c0) * 128:(c - c0 + 1) * 128], in_=sl["kd"][:, c * CH:(c + 1) * CH],
                                                          identity=ident_b[:, :])
                            return ins
                        ctx.pe.op(trk, reads=[sl["kd"], ident_b], writes=[ps_tr])
                        ctx.dve.op(lambda c0=c0: nc.vector.tensor_copy(out=sl["kdtok"][:, c0:c0 + kg, :], in_=ps_tr.t[0:CH, 0:kg * 128].rearrange("p (c d) -> p c d", d=128)),
                                   reads=[ps_tr], writes=[sl["kdtok"]])
                        yield
                sls = []
                gens = []
                for h in hs:
                    sl = slots[si % nslots]
                    si += 1
                    sls.append(sl)
                    gens.append(head_gen(h, sl))
                while gens:
                    for gg in list(gens):
                        try:
                            next(gg)
                        except StopIteration:
                            gens.remove(gg)
                for gi, h in enumerate(hs):
                    sl = sls[gi]

                    def scm(sl=sl, gi=gi):
                        ins = None
                        for c in range(NCH):
                            cs = slice(c * CH, (c + 1) * CH)
                            ins = ctx.mm(ps_sc[gi % n_sc][0:CH, cs], lhsT=sl["kt"][:, cs], rhs=sl["qt"][:, cs], start=True, stop=True)
                        return ins
                    ctx.pe.op(scm, reads=[sl["kt"], sl["qt"]], writes=[ps_sc[gi % n_sc]])
                    mkb = sl["dm"] if scalar_decay else cm
                    ctx.dve.op(lambda sl=sl, gi=gi, mkb=mkb: nc.vector.tensor_tensor(out=sl["sc"][:, :], in0=ps_sc[gi % n_sc][:, :], in1=mkb.t.rearrange("p c t -> p (c t)"), op=ALU.mult),
                               reads=[ps_sc[gi % n_sc], mkb], writes=[sl["sc"]])
                for c in range(NCH):
                    cs = slice(c * CH, (c + 1) * CH)
                    for gi, h in enumerate(hs):
                        sl = sls[gi]

                        def ymm(sl=sl, gi=gi, h=h):
                            ins = None
                            for vb in range(nvb):
                                vs = slice(vb * 128, vb * 128 + dvb)
                                ctx.mm(ps_y[gi][vb][0:dvb, cs], lhsT=sl["vtok"][0:CH, c, vs], rhs=sl["sc"][0:CH, cs], start=True, stop=False)
                                ins = ctx.mm(ps_y[gi][vb][0:dvb, cs], lhsT=Sb[:, h, vs], rhs=sl["qh"][:, cs], start=False, stop=True)
                            return ins
                        ctx.pe.op(ymm, reads=[sl["vtok"], sl["sc"], Sbh[h], sl["qh"]], writes=ps_y[gi])
                        pu = ps_u[gi % n_u]
                        ctx.pe.op(lambda sl=sl, pu=pu: ctx.mm(pu[:, 0:dv], lhsT=sl["kdtok"][0:CH, c, :], rhs=sl["vtok"][0:CH, c, :], start=True, stop=True),
                                  reads=[sl["kdtok"], sl["vtok"]], writes=[pu])
                        ctx.dve.op(lambda sl=sl, h=h, pu=pu: nc.vector.scalar_tensor_tensor(out=Sb[:, h, :], in0=S[:, h, :], scalar=sl["dcol"][:, c:c + 1], in1=pu[:, 0:dv],
                                                                                           op0=ALU.mult, op1=ALU.add), reads=[Sh[h], sl["dcol"], pu], writes=[Sbh[h]])
                        ctx.dve.op(lambda sl=sl, h=h, pu=pu: nc.vector.scalar_tensor_tensor(out=S[:, h, :], in0=S[:, h, :], scalar=sl["dcol"][:, c:c + 1], in1=pu[:, 0:dv],
                                                                                           op0=ALU.mult, op1=ALU.add), reads=[Sh[h], sl["dcol"], pu], writes=[Sh[h]])
                for gi, h in enumerate(hs):
                    sl = sls[gi]
                    for vb in range(nvb):
                        ctx.act.op(lambda sl=sl, gi=gi, vb=vb: nc.scalar.activation(out=sl["ysb"][vb][:, :], in_=ps_y[gi][vb][:, :], func=AF.Copy),
                                   reads=[ps_y[gi][vb]], writes=[sl["ysb"][vb]])
                        r0 = h * dv + vb * 128
                        ctx.sp.dma([(Y.t[r0:r0 + dvb, t0:t0 + SEG], sl["ysb"][vb][:, :])], sl["ysb"][vb], reads=[sl["ysb"][vb]], writes=[Y])
        ctx.barrier()


def lb_setup(ctx, es, logits_ap):
    nc = ctx.nc
    lg = ctx.sbuf(es, [128, 16, DEPTH], F32, "lb_lg")
    ctx.sp.dma([(lg[:, :, l], logits_ap[l].rearrange("(b p) -> p b", p=128)) for l in range(DEPTH)], lg, writes=[lg], allow_slow_non_contiguous=True)
    ctx.act.op(lambda: nc.scalar.activation(out=lg[:, :, :], in_=lg[:, :, :], func=AF.Exp), reads=[lg], writes=[lg])
    s = ctx.sbuf(es, [128, 16], F32, "lb_s")
    ctx.dve.op(lambda: nc.vector.tensor_reduce(out=s[:, :], in_=lg[:, :, :], axis=AX.X, op=ALU.add), reads=[lg], writes=[s])
    ctx.dve.op(lambda: nc.vector.reciprocal(out=s[:, :], in_=s[:, :]), reads=[s], writes=[s])
    res = {}
    for layer in (1, 3):
        lb = ctx.sbuf(es, [128, 16], F32, "lb")
        oml = ctx.sbuf(es, [128, 16], F32, "oml")
        noml = ctx.sbuf(es, [128, 16], F32, "noml")
        ctx.dve.op(lambda: nc.vector.tensor_copy(out=lb[:, :], in_=lg[:, :, 1]), reads=[lg], writes=[lb])
        for l in range(2, layer + 1):
            ctx.dve.op(lambda l=l: nc.vector.tensor_tensor(out=lb[:, :], in0=lb[:, :], in1=lg[:, :, l], op=ALU.add), reads=[lb, lg], writes=[lb])
        ctx.dve.op(lambda: nc.vector.tensor_tensor(out=lb[:, :], in0=lb[:, :], in1=s[:, :], op=ALU.mult), reads=[lb, s], writes=[lb])
        ctx.dve.op(lambda: nc.vector.tensor_scalar(out=oml[:, :], in0=lb[:, :], scalar1=-1.0, scalar2=1.0, op0=ALU.mult, op1=ALU.add), reads=[lb], writes=[oml])
        ctx.dve.op(lambda: nc.vector.tensor_scalar(out=noml[:, :], in0=oml[:, :], scalar1=-1.0, scalar2=None, op0=ALU.mult), reads=[oml], writes=[noml])
        res[layer] = (lb, oml, noml)
    return res


def conv_phase(ctx, T, XBC, XBCc, conv_w, conv_b):
    nc = ctx.nc
    NBK = 3072 // 128
    with ExitStack() as es:
        w = ctx.sbuf(es, [128, NBK, 4], F32, "cv_w")
        b = ctx.sbuf(es, [128, NBK], F32, "cv_b")
        ctx.sp.dma([(w[:, :, jj], conv_w[jj].rearrange("(b p) -> p b", p=128)) for jj in range(4)], w, writes=[w], allow_slow_non_contiguous=True)
        ctx.sp.dma([(b[:, :], conv_b.rearrange("(b p) -> p b", p=128))], b, writes=[b], allow_slow_non_contiguous=True)
        xps = [ctx.sbuf(es, [128, T + 3], BF16, "cv_x") for _ in range(2)]
        accs = [ctx.sbuf(es, [128, T], F32, "cv_a") for _ in range(2)]
        outs = [ctx.sbuf(es, [128, T], BF16, "cv_o") for _ in range(2)]
        for xp in xps:
            ctx.pool.op(lambda xp=xp: nc.gpsimd.memset(xp[:, 0:3], 0.0), writes=[xp])
        for blk in range(NBK):
            xp, acc, o = xps[blk % 2], accs[blk % 2], outs[blk % 2]
            ctx.sp.dma([(xp[:, 3:T + 3], XBC.t[blk * 128:(blk + 1) * 128, :])], xp, reads=[XBC], writes=[xp])
            ctx.dve.op(lambda: nc.vector.tensor_scalar(out=acc[:, :], in0=xp[:, 0:T], scalar1=w[:, blk, 0:1], scalar2=b[:, blk:blk + 1], op0=ALU.mult, op1=ALU.add),
                       reads=[xp, w, b], writes=[acc])
            for j in range(1, 4):
                ctx.dve.op(lambda j=j: nc.vector.scalar_tensor_tensor(out=acc[:, :], in0=xp[:, j:j + T], scalar=w[:, blk, j:j + 1], in1=acc[:, :], op0=ALU.mult, op1=ALU.add),
                           reads=[xp, w, acc], writes=[acc])
            ctx.act.op(lambda: nc.scalar.activation(out=o[:, :], in_=acc[:, :], func=AF.Silu), reads=[acc], writes=[o])
            ctx.sp.dma([(XBCc.t[blk * 128:(blk + 1) * 128, :], o[:, :])], o, reads=[o], writes=[XBCc])
        ctx.barrier()


def dt_phase(ctx, T, DT, SDT, dt_bias, a_log):
    nc = ctx.nc
    with ExitStack() as es:
        x = ctx.sbuf(es, [32, T], F32, "dt_x")
        y = ctx.sbuf(es, [32, T], F32, "dt_y")
        pb = ctx.sbuf(es, [32, 2], F32, "dt_pb")
        ctx.sp.dma([(pb[:, 0:1], dt_bias.rearrange("(h o) -> h o", o=1)), (pb[:, 1:2], a_log.rearrange("(h o) -> h o", o=1))], pb, writes=[pb],
                   allow_slow_non_contiguous=True)
        ctx.sp.dma([(x[:, :], DT.t[:, :])], x, reads=[DT], writes=[x])
        ctx.act.op(lambda: nc.scalar.activation(out=pb[:, 1:2], in_=pb[:, 1:2], func=AF.Exp), reads=[pb], writes=[pb])
        ctx.dve.op(lambda: nc.vector.tensor_scalar(out=pb[:, 1:2], in0=pb[:, 1:2], scalar1=-1.0, scalar2=None, op0=ALU.mult), reads=[pb], writes=[pb])
        ctx.act.op(lambda: nc.scalar.activation(out=x[:, :], in_=x[:, :], func=AF.Exp, bias=pb[:, 0:1]), reads=[x, pb], writes=[x])
        ctx.act.op(lambda: nc.scalar.activation(out=x[:, :], in_=x[:, :], func=AF.Ln, bias=1.0), reads=[x], writes=[x])
        ctx.dve.op(lambda: nc.vector.tensor_scalar(out=y[:, :], in0=x[:, :], scalar1=pb[:, 1:2], scalar2=None, op0=ALU.mult), reads=[x, pb], writes=[y])
        ctx.sp.dma([(SDT.t[0:32, :], x[:, :])], x, reads=[x], writes=[SDT])
        ctx.sp.dma([(SDT.t[32:64, :], y[:, :])], y, reads=[y], writes=[SDT])
        ctx.barrier()


def ret_scan(ctx, consts, T, P, Y):
    nc = ctx.nc
    with ExitStack() as es:
        ones_f = consts["ones_f"]
        ra = ctx.sbuf(es, [128, 128], F32, "rot_a")
        rb_ = ctx.sbuf(es, [128, 128], F32, "rot_b")
        rot = ctx.sbuf(es, [128, 128], BF16, "rot")
        ctx.pool.op(lambda: nc.gpsimd.affine_select(out=ra[:, :], in_=ones_f[:, :], pattern=[[-1, 128]], compare_op=ALU.is_equal, fill=0.0, base=64, channel_multiplier=1),
                    reads=[ones_f], writes=[ra])
        ctx.pool.op(lambda: nc.gpsimd.affine_select(out=rb_[:, :], in_=ones_f[:, :], pattern=[[-1, 128]], compare_op=ALU.is_equal, fill=0.0, base=-64, channel_multiplier=1),
                    reads=[ones_f], writes=[rb_])
        ctx.dve.op(lambda: nc.vector.tensor_tensor(out=rot[:, :], in0=ra[:, :], in1=rb_[:, :], op=ALU.subtract), reads=[ra, rb_], writes=[rot])
        pidx = ctx.sbuf(es, [128, 1], F32, "pidx")
        for half in range(2):
            ctx.pool.op(lambda half=half: nc.gpsimd.iota(pidx[half * 64:(half + 1) * 64, :], pattern=[[0, 1]], base=0, channel_multiplier=1,
                                                         allow_small_or_imprecise_dtypes=True), writes=[pidx])
        ifr = ctx.sbuf(es, [128, 1], F32, "ifr")
        ctx.act.op(lambda: nc.scalar.activation(out=ifr[:, :], in_=pidx[:, :], func=AF.Exp, scale=-math.log(10000.0) / 64.0), reads=[pidx], writes=[ifr])
        ctx.dve.op(lambda: nc.vector.tensor_scalar(out=ifr[:, :], in0=ifr[:, :], scalar1=1.0 / (2.0 * math.pi), scalar2=None, op0=ALU.mult), reads=[ifr], writes=[ifr])
        pos = ctx.sbuf(es, [128, SEG], F32, "pos")
        fr = ctx.sbuf(es, [128, SEG], F32, "fr")
        fi = ctx.sbuf(es, [128, SEG], I32, "fi")
        ff = ctx.sbuf(es, [128, SEG], F32, "ff")
        msk = ctx.sbuf(es, [128, SEG], F32, "msk")
        tabs = {k: ctx.sbuf(es, [128, SEG], F32, "tab_" + k) for k in ("cos", "sin", "cosk", "sink")}
        tb = Buf(ctx, None, "tabs")
        ks = 128.0 ** -0.5

        def wrap(src):
            ctx.dve.op(lambda: nc.vector.tensor_scalar(out=msk[:, :], in0=src[:, :], scalar1=0.5, scalar2=None, op0=ALU.is_gt), reads=[src], writes=[msk])
            ctx.dve.op(lambda: nc.vector.tensor_tensor(out=src[:, :], in0=src[:, :], in1=msk[:, :], op=ALU.subtract), reads=[src, msk], writes=[src])
            ctx.dve.op(lambda: nc.vector.tensor_scalar(out=msk[:, :], in0=src[:, :], scalar1=-0.5, scalar2=None, op0=ALU.is_lt), reads=[src], writes=[msk])
            ctx.dve.op(lambda: nc.vector.tensor_tensor(out=src[:, :], in0=src[:, :], in1=msk[:, :], op=ALU.add), reads=[src, msk], writes=[src])

        def seg_setup(seg, t0):
            ctx.pool.op(lambda: nc.gpsimd.iota(pos[:, :], pattern=[[1, SEG]], base=t0, channel_multiplier=0, allow_small_or_imprecise_dtypes=True),
                        reads=[], writes=[pos])
            ctx.dve.op(lambda: nc.vector.tensor_scalar(out=fr[:, :], in0=pos[:, :], scalar1=ifr[:, 0:1], scalar2=None, op0=ALU.mult), reads=[pos, ifr], writes=[fr])
            ctx.dve.op(lambda: nc.vector.tensor_copy(out=fi[:, :], in_=fr[:, :]), reads=[fr], writes=[fi])
            ctx.dve.op(lambda: nc.vector.tensor_copy(out=ff[:, :], in_=fi[:, :]), reads=[fi], writes=[ff])
            ctx.dve.op(lambda: nc.vector.tensor_tensor(out=fr[:, :], in0=fr[:, :], in1=ff[:, :], op=ALU.subtract), reads=[fr, ff], writes=[fr])
            wrap(fr)
            ctx.act.op(lambda: nc.scalar.activation(out=tabs["sin"][:, :], in_=fr[:, :], func=AF.Sin, scale=2.0 * math.pi), reads=[fr], writes=[tabs["sin"], tb])
            ctx.dve.op(lambda: nc.vector.tensor_scalar(out=ff[:, :], in0=fr[:, :], scalar1=0.25, scalar2=None, op0=ALU.add), reads=[fr], writes=[ff])
            wrap(ff)
            ctx.act.op(lambda: nc.scalar.activation(out=tabs["cos"][:, :], in_=ff[:, :], func=AF.Sin, scale=2.0 * math.pi), reads=[ff], writes=[tabs["cos"], tb])
            ctx.dve.op(lambda: nc.vector.tensor_scalar(out=tabs["sink"][:, :], in0=tabs["sin"][:, :], scalar1=ks, scalar2=None, op0=ALU.mult), reads=[tabs["sin"]], writes=[tabs["sink"], tb])
            ctx.dve.op(lambda: nc.vector.tensor_scalar(out=tabs["cosk"][:, :], in0=tabs["cos"][:, :], scalar1=ks, scalar2=None, op0=ALU.mult), reads=[tabs["cos"]], writes=[tabs["cosk"], tb])

        def prologue(h, seg, t0, sl, ps_misc):
            qa, ka = sl["auxb"]
            ts = slice(t0, t0 + SEG)
            ctx.sp.dma([(qa[:, :], P.t[h * 128:(h + 1) * 128, ts])], qa, reads=[P], writes=[qa])
            yield
            ctx.sp.dma([(ka[:, :], P.t[1024 + h * 128:1024 + (h + 1) * 128, ts])], ka, reads=[P], writes=[ka])
            yield
            for vb in range(2):
                r0 = 2048 + h * 256 + vb * 128
                ctx.sp.dma([(sl["vT"][vb][:, :], P.t[r0:r0 + 128, ts])], sl["vT"][vb], reads=[P], writes=[sl["vT"][vb]])
                yield
            lg = math.log1p(-2.0 ** (-5.0 - h))
            ctx.pool.op(lambda: nc.gpsimd.memset(sl["gf"][:, :], lg), writes=[sl["gf"]])
            yield
            for src, dst, c_, s_ in ((qa, sl["qf"], "cos", "sin"), (ka, sl["kf"], "cosk", "sink")):
                a0 = sl["aux"][0]
                ctx.pe.op(lambda src=src: ctx.mm(ps_misc[:, :], lhsT=rot[:, :], rhs=src[:, :], start=True, stop=True), reads=[rot, src], writes=[ps_misc])
                ctx.dve.op(lambda: nc.vector.tensor_tensor(out=a0[:, :], in0=ps_misc[:, :], in1=tabs[s_][:, :], op=ALU.mult), reads=[ps_misc, tb], writes=[a0])
                yield
                ctx.pool.op(lambda: nc.gpsimd.tensor_tensor(out=dst[:, :], in0=src[:, :], in1=tabs[c_][:, :], op=ALU.mult), reads=[src, tb], writes=[dst])
                yield
                ctx.dve.op(lambda: nc.vector.tensor_tensor(out=dst[:, :], in0=dst[:, :], in1=a0[:, :], op=ALU.add), reads=[dst, a0], writes=[dst])
                yield

        gla_scan(ctx, consts, T, 8, 256, Y, seg_setup, prologue, CH=128, G=2, nslots=4)


def ssd_scan(ctx, consts, T, XBCc, SDT, Y):
    nc = ctx.nc

    def seg_setup(seg, t0):
        pass

    def prologue(h, seg, t0, sl, ps_misc):
        g = h // 8
        ts = slice(t0, t0 + SEG)
        ba, ca = sl["auxb"]
        dtb = sl["aux"][0]
        ctx.sp.dma([(ba[:, :], XBCc.t[2048 + g * 128:2048 + (g + 1) * 128, ts])], ba, reads=[XBCc], writes=[ba])
        yield
        ctx.sp.dma([(ca[:, :], XBCc.t[2560 + g * 128:2560 + (g + 1) * 128, ts])], ca, reads=[XBCc], writes=[ca])
        yield
        ctx.sp.dma([(sl["vT"][0][:, :], XBCc.t[h * 64:(h + 1) * 64, ts])], sl["vT"][0], reads=[XBCc], writes=[sl["vT"][0]])
        yield
        ctx.sp.dma([(dtb[:, :], SDT.t[h, ts].partition_broadcast(128))], dtb, reads=[SDT], writes=[dtb])
        yield
        ctx.sp.dma([(sl["gf"][:, :], SDT.t[32 + h, ts].partition_broadcast(128))], sl["gf"], reads=[SDT], writes=[sl["gf"]])
        yield
        ctx.pool.op(lambda: nc.gpsimd.tensor_tensor(out=sl["kf"][:, :], in0=ba[:, :], in1=dtb[:, :], op=ALU.mult), reads=[ba, dtb], writes=[sl["kf"]])
        yield
        ctx.act.op(lambda: nc.scalar.activation(out=sl["qf"][:, :], in_=ca[:, :], func=AF.Copy), reads=[ca], writes=[sl["qf"]])
        yield

    gla_scan(ctx, consts, T, 32, 64, Y, seg_setup, prologue, scalar_decay=True, CH=128, G=4, nslots=5)


def hgrn_scan(ctx, consts, T, P, FR, lbs, Y):
    nc = ctx.nc
    lb, oml, noml = lbs

    def seg_setup(seg, t0):
        pass

    def prologue(h, seg, t0, sl, ps_misc):
        ts = slice(t0, t0 + SEG)
        qa = sl["auxb"][0]
        fr, sg = sl["aux"]
        ctx.sp.dma([(qa[:, :], P.t[h * 128:(h + 1) * 128, ts])], qa, reads=[P], writes=[qa])
        yield
        ctx.sp.dma([(sl["vT"][0][:, :], P.t[2048 + h * 128:2048 + (h + 1) * 128, ts])], sl["vT"][0], reads=[P], writes=[sl["vT"][0]])
        yield
        ctx.sp.dma([(fr[:, :], FR.t[h * 128:(h + 1) * 128, ts])], fr, reads=[FR], writes=[fr])
        yield
        ctx.act.op(lambda: nc.scalar.activation(out=sl["qf"][:, :], in_=qa[:, :], func=AF.Copy), reads=[qa], writes=[sl["qf"]])
        yield
        ctx.act.op(lambda: nc.scalar.activation(out=sg[:, :], in_=fr[:, :], func=AF.Sigmoid), reads=[fr], writes=[sg])
        yield
        ctx.dve.op(lambda: nc.vector.tensor_scalar(out=fr[:, :], in0=sg[:, :], scalar1=oml[:, h:h + 1], scalar2=lb[:, h:h + 1], op0=ALU.mult, op1=ALU.add),
                   reads=[sg, oml, lb], writes=[fr])
        yield
        ctx.act.op(lambda: nc.scalar.activation(out=sl["gf"][:, :], in_=fr[:, :], func=AF.Ln), reads=[fr], writes=[sl["gf"]])
        yield
        ctx.dve.op(lambda: nc.vector.tensor_scalar(out=sl["kf"][:, :], in0=sg[:, :], scalar1=noml[:, h:h + 1], scalar2=oml[:, h:h + 1], op0=ALU.mult, op1=ALU.add),
                   reads=[sg, noml, oml], writes=[sl["kf"]])
        yield

    gla_scan(ctx, consts, T, 16, 128, Y, seg_setup, prologue, CH=32, G=4, nslots=5)


def post_phase(ctx, consts, T, kind, Y, CAT, cat_row0, wvec, gate_src, gate_row0, xs_src=None, dskip=None):
    nc = ctx.nc
    NBH = 8
    gsz = {"ret": 2, "ssd": 4, "hgrn": 1}[kind]
    with ExitStack() as es:
        wk = norm_work(ctx, es, NBH)
        wv = ctx.sbuf(es, [128, 16], F32, "po_w")
        ctx.sp.dma([(wv[:, :], wvec.rearrange("(b p) -> p b", p=128))], wv, writes=[wv], allow_slow_non_contiguous=True)
        if dskip is not None:
            dk = ctx.sbuf(es, [128, 16], F32, "po_dk")
            ctx.sp.dma([(dk[:, :], dskip.rearrange("(b p) -> p b", p=128))], dk, writes=[dk], allow_slow_non_contiguous=True)
        ys = [ctx.sbuf(es, [128, NBH, 512], F32, "po_y") for _ in range(2)]
        gs = [ctx.sbuf(es, [128, NBH, 512], BF16, "po_g") for _ in range(2)]
        xs_ = [ctx.sbuf(es, [128, NBH, 512], BF16, "po_x") for _ in range(2)] if xs_src is not None else None
        os_ = [ctx.sbuf(es, [128, NBH, 512], BF16, "po_o") for _ in range(2)]
        it = 0
        for nt in range(T // 512):
            tsl = slice(nt * 512, (nt + 1) * 512)
            for half in range(2):
                b0 = half * NBH
                y, g, o = ys[it % 2], gs[it % 2], os_[it % 2]
                rs = slice(b0 * 128, (b0 + NBH) * 128)
                ctx.sp.dma([(y[:, :, :], Y.t[rs, tsl].rearrange("(b p) t -> p b t", p=128))], y, reads=[Y], writes=[y])
                grs = slice(gate_row0 + b0 * 128, gate_row0 + (b0 + NBH) * 128)
                ctx.sp.dma([(g[:, :, :], gate_src.t[grs, tsl].rearrange("(b p) t -> p b t", p=128))], g, reads=[gate_src], writes=[g])
                if kind == "ssd":
                    x = xs_[it % 2]
                    ctx.sp.dma([(x[:, :, :], xs_src.t[rs, tsl].rearrange("(b p) t -> p b t", p=128))], x, reads=[xs_src], writes=[x])
                    for b in range(NBH):
                        ctx.dve.op(lambda b=b: nc.vector.scalar_tensor_tensor(out=y[:, b, :], in0=x[:, b, :], scalar=dk[:, b0 + b:b0 + b + 1], in1=y[:, b, :],
                                                                             op0=ALU.mult, op1=ALU.add), reads=[x, dk, y], writes=[y])
                    ctx.pool.op(lambda: nc.gpsimd.tensor_tensor(out=y[:, :, :], in0=y[:, :, :], in1=g[:, :, :], op=ALU.mult), reads=[y, g], writes=[y])
                group_norm_tile(ctx, consts, wk, y, y, NBH, gsz, kind == "ret", gsz * 128)
                for b in range(NBH):
                    if kind == "ssd":
                        ctx.act.op(lambda b=b: nc.scalar.activation(out=o[:, b, :], in_=y[:, b, :], func=AF.Copy, scale=wv[:, b0 + b:b0 + b + 1]), reads=[y, wv], writes=[o])
                    else:
                        ctx.dve.op(lambda b=b: nc.vector.scalar_tensor_tensor(out=o[:, b, :], in0=y[:, b, :], scalar=wv[:, b0 + b:b0 + b + 1], in1=g[:, b, :],
                                                                             op0=ALU.mult, op1=ALU.mult), reads=[y, wv, g], writes=[o])
                ors = slice(cat_row0 + b0 * 128, cat_row0 + (b0 + NBH) * 128)
                ctx.sp.dma([(CAT.t[ors, tsl].rearrange("(b p) t -> p b t", p=128), o[:, :, :])], o, reads=[o], writes=[CAT])
                it += 1
        ctx.barrier()


def router_phase(ctx, consts, T, XT, w_router, GB):
    nc = ctx.nc
    with ExitStack() as es:
        wr = ctx.sbuf(es, [128, 16, NEXP], F32, "rt_w")
        ctx.sp.dma([(wr[:, :, :], w_router.rearrange("(kc p) e -> p kc e", p=128))], wr, writes=[wr])
        xs = [ctx.sbuf(es, [128, 16, 512], F32, "rt_x") for _ in range(2)]
        gall = ctx.sbuf(es, [NEXP, T], F32, "rt_gall")
        ps_l = ctx.psum(es, [128, NEXP], F32, "rt_psl")
        ps_t = ctx.psum(es, [NEXP, 128], F32, "rt_pst")
        lg = ctx.sbuf(es, [128, NEXP], F32, "rt_lg")
        m8 = ctx.sbuf(es, [128, 8], F32, "rt_m8")
        nm = ctx.sbuf(es, [128, 1], F32, "rt_nm")
        mk = ctx.sbuf(es, [128, NEXP], F32, "rt_mk")
        ex = ctx.sbuf(es, [128, NEXP], F32, "rt_ex")
        sm = ctx.sbuf(es, [128, 1], F32, "rt_sm")
        ident_f = consts["ident_f"]
        for nt in range(T // 512):
            x = xs[nt % 2]
            ctx.sp.dma([(x[:, :, :], XT.t[:, nt * 512:(nt + 1) * 512].rearrange("(kc p) t -> p kc t", p=128))], x, reads=[XT], writes=[x])
            for j in range(4):
                tk = slice(j * 128, (j + 1) * 128)

                def mm():
                    ins = None
                    for kc in range(16):
                        ins = ctx.mm(ps_l[:, :], lhsT=x[:, kc, tk], rhs=wr[:, kc, :], start=(kc == 0), stop=(kc == 15))
                    return ins
                ctx.pe.op(mm, reads=[x, wr], writes=[ps_l])
                ctx.dve.op(lambda: nc.vector.tensor_copy(out=lg[:, :], in_=ps_l[:, :]), reads=[ps_l], writes=[lg])
                ctx.dve.op(lambda: nc.vector.max(out=m8[:, :], in_=lg[:, :]), reads=[lg], writes=[m8])
                ctx.dve.op(lambda: nc.vector.tensor_scalar(out=mk[:, :], in0=lg[:, :], scalar1=m8[:, 1:2], scalar2=None, op0=ALU.is_ge), reads=[lg, m8], writes=[mk])
                ctx.dve.op(lambda: nc.vector.tensor_scalar(out=nm[:, :], in0=m8[:, 0:1], scalar1=-1.0, scalar2=None, op0=ALU.mult), reads=[m8], writes=[nm])
                ctx.act.op(lambda: nc.scalar.activation(out=ex[:, :], in_=lg[:, :], func=AF.Exp, bias=nm[:, 0:1]), reads=[lg, nm], writes=[ex])
                ctx.dve.op(lambda: nc.vector.tensor_tensor(out=ex[:, :], in0=ex[:, :], in1=mk[:, :], op=ALU.mult), reads=[ex, mk], writes=[ex])
                ctx.dve.op(lambda: nc.vector.tensor_reduce(out=sm[:, :], in_=ex[:, :], axis=AX.X, op=ALU.add), reads=[ex], writes=[sm])
                ctx.dve.op(lambda: nc.vector.reciprocal(out=sm[:, :], in_=sm[:, :]), reads=[sm], writes=[sm])
                ctx.dve.op(lambda: nc.vector.tensor_scalar(out=ex[:, :], in0=ex[:, :], scalar1=sm[:, 0:1], scalar2=None, op0=ALU.mult), reads=[ex, sm], writes=[ex])
                ctx.pe.op(lambda: ctx.tr(out=ps_t[:, :], in_=ex[:, :], identity=ident_f[:, :]), reads=[ex, ident_f], writes=[ps_t])
                c0 = nt * 512 + j * 128
                ctx.act.op(lambda: nc.scalar.activation(out=gall[:, c0:c0 + 128], in_=ps_t[:, :], func=AF.Copy), reads=[ps_t], writes=[gall])
        ctx.sp.dma([(GB.t[:, :], gall[:, :])], gall, reads=[gall], writes=[GB])
        ctx.barrier()


def moe_down(ctx, H, T, W2s, M):
    nc = ctx.nc
    TS = min(T, 1024)
    NT = TS // 512
    KH = 22
    WCB = 256
    with ExitStack() as es:
        acc = ctx.sbuf(es, [128, 16, TS], F32, "md_acc")
        a_sb = [ctx.sbuf(es, [128, KH, TS], BF16, "md_a") for _ in range(2)]
        w_sb = [ctx.sbuf(es, [128, KH, WCB], BF16, "md_w") for _ in range(2)]
        pss = [ctx.psum(es, [128, 512], F32, "md_ps") for _ in range(3)]
        wi = 0
        pi = 0
        ai = 0
        for st in range(T // TS):
            t0 = st * TS
            first = True
            for e, W2 in enumerate(W2s):
                for half in range(2):
                    r0 = half * KH * 128
                    a = a_sb[ai % 2]
                    ai += 1
                    A_v = H[e].t[r0:r0 + KH * 128, t0:t0 + TS].rearrange("(kc p) t -> p kc t", p=128)
                    ctx.sp.dma([(a[:, k0:k0 + 11, :], A_v[:, k0:k0 + 11, :]) for k0 in range(0, KH, 11)], a, reads=[H[e]], writes=[a])
                    for c0 in range(0, D, WCB):
                        wb = w_sb[wi % 2]
                        wi += 1
                        W_v = W2[r0:r0 + KH * 128, c0:c0 + WCB].rearrange("(kc p) c -> p kc c", p=128)
                        ctx.pool.dma([(wb[:, k0:k0 + 11, :], W_v[:, k0:k0 + 11, :]) for k0 in range(0, KH, 11)], wb, writes=[wb])
                        for m0 in range(0, WCB, 128):
                            cb = (c0 + m0) // 128
                            for nt in range(NT):
                                ps = pss[pi % 3]
                                pi += 1
                                ns = slice(nt * 512, (nt + 1) * 512)

                                def mm(ps=ps, wb=wb, a=a, m0=m0, ns=ns):
                                    ins = None
                                    for kc in range(KH):
                                        ins = ctx.mm(ps[:, :], lhsT=wb[:, kc, m0:m0 + 128], rhs=a[:, kc, ns], start=(kc == 0), stop=(kc == KH - 1))
                                    return ins
                                ctx.pe.op(mm, reads=[wb, a], writes=[ps])
                                if first:
                                    ctx.act.op(lambda ps=ps, cb=cb, ns=ns: nc.scalar.activation(out=acc[:, cb, ns], in_=ps[:, :], func=AF.Copy), reads=[ps], writes=[acc])
                                else:
                                    ctx.dve.op(lambda ps=ps, cb=cb, ns=ns: nc.vector.tensor_tensor(out=acc[:, cb, ns], in0=acc[:, cb, ns], in1=ps[:, :], op=ALU.add),
                                               reads=[ps, acc], writes=[acc])
                    first = False
            ctx.sp.dma([(M.t[:, t0:t0 + TS].rearrange("(b p) t -> p b t", p=128), acc[:, :, :])], acc, reads=[acc], writes=[M])
        ctx.barrier()


def in_proj(ctx, XB, T, W, C, colmap):
    TS = min(T, 2048)
    with ExitStack() as es:
        stagers = {"b": Stager(ctx, es, TS, BF16, 3, "ip_b"), "f": Stager(ctx, es, TS, F32, 2, "ip_f")}
        et, eb = make_plain_epi(ctx, colmap, stagers)
        gemm(ctx, XB, D, T, [W], C, et, eb, TS, 512)


def out_proj(ctx, CAT, K, T, W, M):
    TS = min(T, 1024)
    with ExitStack() as es:
        stg = Stager(ctx, es, TS, F32, 3, "op_stg")
        et, eb = make_plain_epi(ctx, lambda cb0: (M, cb0, None, "m"), {"m": stg})
        gemm(ctx, CAT, K, T, [W], D, et, eb, TS, 256)


def even_layer(ctx, consts, T, S, w, j, last_out=None):
    P, DT, XBCc, SDT, Y, CAT, M, H, XT, XB = (S[k] for k in ("P", "DT", "XBCc", "SDT", "Y", "CAT", "M", "H", "XT", "XB"))

    def colmap(cb0):
        if cb0 < 4096:
            return (P, cb0, None, "b")
        if cb0 < 8192:
            return (P, cb0, AF.Silu, "b")
        if cb0 < 11264:
            return (P, cb0, None, "b")
        return (DT, 0, None, "f")
    in_proj(ctx, XB, T, w["w_in"], EV_IN, colmap)
    ret_scan(ctx, consts, T, P, Y)
    post_phase(ctx, consts, T, "ret", Y, CAT, 0, w["ret_norm_w"], P, 4096)
    XBC = Buf(ctx, P.t[8192:11264, :], "XBC")
    XBC.w, XBC.r = P.w, P.r
    conv_phase(ctx, T, XBC, XBCc, w["conv_w"], w["conv_b"])
    dt_phase(ctx, T, DT, SDT, w["dt_bias"], w["a_log"])
    ssd_scan(ctx, consts, T, XBCc, SDT, Y)
    post_phase(ctx, consts, T, "ssd", Y, CAT, 2048, w["ssd_norm_w"], P, 6144, xs_src=XBCc, dskip=w["d_skip_ch"])
    out_proj(ctx, CAT, 4096, T, w["w_out"], M)
    ln_phase(ctx, consts, XT, M, XB, w["ln1_g"], w["ln1_b"], T)
    swiglu_up(ctx, XB, T, [w["ffn_w1"]], [w["ffn_w3"]], H)
    ffn_down(ctx, H[0], T, w["ffn_w2"], M)
    ln_phase(ctx, consts, XT, M, XB, w["ln2_g"], w["ln2_b"], T, out_final=last_out)


def odd_layer(ctx, consts, T, S, w, j, lbs, last_out=None):
    P, FR, Y, CAT, M, H, XT, XB, GB = (S[k] for k in ("P", "FR", "Y", "CAT", "M", "H", "XT", "XB", "GB"))

    def colmap(cb0):
        if cb0 < 2048:
            return (P, cb0, AF.Silu, "b")
        if cb0 < 4096:
            return (FR, cb0 - 2048, None, "f")
        if cb0 < 6144:
            return (P, cb0 - 2048, None, "b")
        return (P, cb0 - 2048, AF.Silu, "b")
    in_proj(ctx, XB, T, w["w_in"], 8192, colmap)
    hgrn_scan(ctx, consts, T, P, FR, lbs, Y)
    post_phase(ctx, consts, T, "hgrn", Y, CAT, 0, w["hg_norm_w"], P, 4096)
    out_proj(ctx, CAT, 2048, T, w["w_out"], M)
    ln_phase(ctx, consts, XT, M, XB, w["ln1_g"], w["ln1_b"], T)
    router_phase(ctx, consts, T, XT, w["router"], GB)
    swiglu_up(ctx, XB, T, [w["moe_w1"][e] for e in range(NEXP)], [w["moe_w3"][e] for e in range(NEXP)], H, GB=GB)
    moe_down(ctx, H, T, [w["moe_w2"][e] for e in range(NEXP)], M)
    ln_phase(ctx, consts, XT, M, XB, w["ln2_g"], w["ln2_b"], T, out_final=last_out)


WSPEC = [
    ("ev_w_in", [2, D, EV_IN]), ("ev_ret_norm_w", [2, D]), ("ev_conv_w", [2, 4, 3072]), ("ev_conv_b", [2, 3072]),
    ("ev_dt_bias", [2, 32]), ("ev_a_log", [2, 32]), ("ev_d_skip_ch", [2, D]), ("ev_ssd_norm_w", [2, D]),
    ("ev_w_out", [2, 4096, D]), ("ev_ln1_g", [2, D]), ("ev_ln1_b", [2, D]),
    ("ffn_w1", [2, D, FFN]), ("ffn_w3", [2, D, FFN]), ("ffn_w2", [2, FFN, D]), ("ev_ln2_g", [2, D]), ("ev_ln2_b", [2, D]),
    ("od_w_in", [2, D, 8192]), ("hg_lb_logits", [DEPTH, D]), ("od_hg_norm_w", [2, D]), ("od_w_out", [2, D, D]),
    ("od_ln1_g", [2, D]), ("od_ln1_b", [2, D]), ("moe_router", [2, D, NEXP]),
    ("moe_w1", [2, NEXP, D, FFN]), ("moe_w3", [2, NEXP, D, FFN]), ("moe_w2", [2, NEXP, FFN, D]),
    ("od_ln2_g", [2, D]), ("od_ln2_b", [2, D]),
]


def build_program(T, layers=(0, 1, 2, 3), wfilter=None, nl=2):
    nc = bass.Bass("TRN2", target_bir_lowering=False)
    with ExitStack() as es:
        ctx = Ctx(nc, es)
        xin = ctx.dram("xin", [D, T], F32, kind="ExternalInput")
        out = ctx.dram("out", [D, T], F32, kind="ExternalOutput")
        W = {name: nc.dram_tensor(name, ([nl] + shape[1:]) if name != "hg_lb_logits" else shape, F32, kind="ExternalInput").ap()
             for name, shape in WSPEC if (wfilter is None or wfilter(name))}
        S = {
            "XT": ctx.dram("XT", [D, T], F32), "XB": ctx.dram("XB", [D, T], BF16),
            "P": ctx.dram("P", [11264, T], BF16), "DT": ctx.dram("DT", [32, T], F32), "FR": ctx.dram("FR", [D, T], F32),
            "XBCc": ctx.dram("XBCc", [3072, T], BF16), "SDT": ctx.dram("SDT", [64, T], F32),
            "Y": ctx.dram("Y", [D, T], F32), "CAT": ctx.dram("CAT", [4096, T], BF16), "M": ctx.dram("M", [D, T], F32),
            "H": [ctx.dram(f"H{e}", [FFN, T], BF16) for e in range(NEXP)], "GB": ctx.dram("GB", [NEXP, T], F32),
        }
        consts = make_consts(ctx, es)
        lbs = lb_setup(ctx, es, W["hg_lb_logits"])
        ctx.sp.dma([(S["XT"].t[:, :], xin.t[:, :])], S["XT"], reads=[xin], writes=[S["XT"]])
        cast_dram(ctx, S["XB"], xin)
        for li, layer in enumerate(layers):
            j = layer // 2
            last = out if li == len(layers) - 1 else None
            if layer % 2 == 0:
                w = {"w_in": W["ev_w_in"][j], "ret_norm_w": W["ev_ret_norm_w"][j], "conv_w": W["ev_conv_w"][j], "conv_b": W["ev_conv_b"][j],
                     "dt_bias": W["ev_dt_bias"][j], "a_log": W["ev_a_log"][j], "d_skip_ch": W["ev_d_skip_ch"][j], "ssd_norm_w": W["ev_ssd_norm_w"][j],
                     "w_out": W["ev_w_out"][j], "ln1_g": W["ev_ln1_g"][j], "ln1_b": W["ev_ln1_b"][j], "ffn_w1": W["ffn_w1"][j], "ffn_w3": W["ffn_w3"][j],
                     "ffn_w2": W["ffn_w2"][j], "ln2_g": W["ev_ln2_g"][j], "ln2_b": W["ev_ln2_b"][j]}
                even_layer(ctx, consts, T, S, w, j, last_out=last)
            else:
                w = {"w_in": W["od_w_in"][j], "hg_norm_w": W["od_hg_norm_w"][j], "w_out": W["od_w_out"][j], "ln1_g": W["od_ln1_g"][j], "ln1_b": W["od_ln1_b"][j],
                     "router": W["moe_router"][j], "moe_w1": W["moe_w1"][j], "moe_w3": W["moe_w3"][j], "moe_w2": W["moe_w2"][j],
                     "ln2_g": W["od_ln2_g"][j], "ln2_b": W["od_ln2_b"][j]}
                odd_layer(ctx, consts, T, S, w, j, lbs[layer], last_out=last)
        finish(ctx, [out])
    return nc


def host_inputs(inputs, xT):
    m = {"xin": np.ascontiguousarray(xT, dtype=np.float32)}
    for name, shape in WSPEC:
        if name == "ev_d_skip_ch":
            m[name] = np.ascontiguousarray(np.repeat(np.asarray(inputs["ev_d_skip"], dtype=np.float32), 64, axis=1))
        else:
            m[name] = np.ascontiguousarray(np.asarray(inputs[name], dtype=np.float32))
    return m


N_ACTIVE = 4


def kernel(**inputs):
    x = np.asarray(inputs["x"], dtype=np.float32)
    B, T, _ = x.shape
    nc = build_program(T)
    in_maps = []
    for c in range(N_ACTIVE):
        in_maps.append(host_inputs(inputs, x[c % B].T))
    res = run_bass_kernel_spmd(nc, in_maps, core_ids=list(range(N_ACTIVE)))
    outs = [np.ascontiguousarray(res.results[c]["out"].T) for c in range(B)]
    return np.stack(outs, axis=0).astype(np.float32)
```
